# Optimizing a Trainium2 kernel written in Bass

```python
import jax, jax.numpy as jnp
from jax import lax
import numpy as np

D_MODEL = 4096
BATCH = 2
SEQ = 4096
DEPTH = 2

GRID_W = 64
CTX_LEN = 256
HEAD_DIM = 128
BLOCK = 128
A_HEADS = D_MODEL // (2 * HEAD_DIM)
A_KV_HEADS = A_HEADS // 4
B_HEADS = D_MODEL // 512
B_QK_DIM = HEAD_DIM
B_V_DIM = (D_MODEL // 2) // B_HEADS
C_HEADS = D_MODEL // HEAD_DIM
WIN_R = 8
WIN_C = 16
PEER_HEADS = 8
PEER_NKEYS = 128
PEER_EXPERTS = PEER_NKEYS * PEER_NKEYS
PEER_DKEY = 256
PEER_TOPK = 16
PEER_BLOCK = 64
ROPE_THETA = 10000.0
EPS = 1e-6
N_EVEN = (DEPTH + 1) // 2
N_ODD = DEPTH // 2
EVEN_SPLITS = (A_HEADS * HEAD_DIM, A_KV_HEADS * HEAD_DIM, A_KV_HEADS * HEAD_DIM,
               B_HEADS * B_QK_DIM, B_HEADS * B_QK_DIM, B_HEADS * B_V_DIM, B_HEADS * B_V_DIM,
               4 * B_HEADS)
EVEN_COLS = sum(EVEN_SPLITS)
ODD_COLS = 3 * C_HEADS * HEAD_DIM

kernel_name = 'hybrid_dit_gqa_mlstm_natten_peer'


def rmsnorm(x, g):
    xf = x.astype(jnp.float32)
    y = xf * lax.rsqrt(jnp.mean(xf * xf, axis=-1, keepdims=True) + EPS)
    return (y * g.astype(jnp.float32)).astype(x.dtype)


def split_cols(p, sizes):
    return jnp.split(p, np.cumsum(sizes)[:-1].tolist(), axis=-1)


def modulation(cvec, w_mod, b_mod):
    m = jax.nn.silu(cvec) @ w_mod + b_mod
    return jnp.split(m, 6, axis=-1)


def rope_2d(x):
    n = x.shape[1]
    t = jnp.arange(n)
    row = (t // GRID_W).astype(jnp.float32)
    col = (t % GRID_W).astype(jnp.float32)
    nf = HEAD_DIM // 4
    inv = ROPE_THETA ** (-jnp.arange(nf, dtype=jnp.float32) / nf)
    ang = jnp.concatenate([row[:, None] * inv, col[:, None] * inv], axis=-1)
    cos = jnp.cos(ang)[None, :, None, :]
    sin = jnp.sin(ang)[None, :, None, :]
    x1, x2 = jnp.split(x.astype(jnp.float32), 2, axis=-1)
    return jnp.concatenate([x1 * cos - x2 * sin, x1 * sin + x2 * cos], axis=-1).astype(x.dtype)


def block_attention(q, k, v):
    Bn, S, H, D = q.shape
    KV = k.shape[2]
    G = H // KV
    nb = S // BLOCK
    qb = q.reshape(Bn, nb, BLOCK, KV, G, D).transpose(1, 0, 2, 3, 4, 5)
    scale = D ** -0.5

    def one(qblk):
        s = jnp.einsum('bqhgd,bkhd->bhgqk', qblk, k).astype(jnp.float32) * scale
        p = jax.nn.softmax(s, axis=-1).astype(v.dtype)
        return jnp.einsum('bhgqk,bkhd->bqhgd', p, v)

    o = lax.map(one, qb)
    return o.transpose(1, 0, 2, 3, 4, 5).reshape(Bn, S, H * D)


def mlstm_chunkwise(q, k, v, i_pre, f_pre):
    Bn, T, H, dk = q.shape
    dv = v.shape[-1]
    L = BLOCK
    nc = T // L
    f32 = jnp.float32
    qs = q.astype(f32).reshape(Bn, nc, L, H, dk).transpose(1, 0, 3, 2, 4)
    ks = (k.astype(f32) * dk ** -0.5).reshape(Bn, nc, L, H, dk).transpose(1, 0, 3, 2, 4)
    vs = v.astype(f32).reshape(Bn, nc, L, H, dv).transpose(1, 0, 3, 2, 4)
    li = i_pre.astype(f32).reshape(Bn, nc, L, H).transpose(1, 0, 3, 2)
    lf = jax.nn.log_sigmoid(f_pre.astype(f32)).reshape(Bn, nc, L, H).transpose(1, 0, 3, 2)
    tri = jnp.tril(jnp.ones((L, L), dtype=bool))

    def step(carry, inp):
        C, n, m = carry
        qc, kc, vc, ic, fc = inp
        b = jnp.cumsum(fc, axis=-1)
        dmat = jnp.where(tri, b[..., :, None] - b[..., None, :] + ic[..., None, :], -jnp.inf)
        inter = b + m[..., None]
        m_t = jnp.maximum(inter, jnp.max(dmat, axis=-1))
        w_intra = jnp.exp(dmat - m_t[..., None])
        w_inter = jnp.exp(inter - m_t)
        s = jnp.einsum('bhtd,bhsd->bhts', qc, kc) * w_intra
        num = w_inter[..., None] * jnp.einsum('bhtd,bhde->bhte', qc, C) + jnp.einsum('bhts,bhse->bhte', s, vc)
        den = w_inter * jnp.einsum('bhtd,bhd->bht', qc, n) + jnp.sum(s, axis=-1)
        h = num / jnp.maximum(jnp.abs(den), jnp.exp(-m_t))[..., None]
        bL = b[..., -1]
        g = bL[..., None] - b + ic
        m_new = jnp.maximum(bL + m, jnp.max(g, axis=-1))
        wk = jnp.exp(g - m_new[..., None])
        decay = jnp.exp(bL + m - m_new)
        C_new = decay[..., None, None] * C + jnp.einsum('bhs,bhsd,bhse->bhde', wk, kc, vc)
        n_new = decay[..., None] * n + jnp.einsum('bhs,bhsd->bhd', wk, kc)
        return (C_new, n_new, m_new), h

    init = (jnp.zeros((Bn, H, dk, dv), f32), jnp.zeros((Bn, H, dk), f32), jnp.zeros((Bn, H), f32))
    _, hs = lax.scan(step, init, (qs, ks, vs, li, lf))
    return hs.transpose(1, 0, 3, 2, 4).reshape(Bn, T, H, dv).astype(q.dtype)


def neighbourhood_attention(q, k, v, kc, vc, rpb):
    Bn, S, H, D = q.shape
    rows = S // GRID_W
    wr = min(WIN_R, rows)
    rs = np.clip(np.arange(rows) - wr // 2, 0, rows - wr).astype(np.int32)
    cols = np.arange(GRID_W)
    cs = np.clip(cols - WIN_C // 2, 0, GRID_W - WIN_C)
    col_idx = cs[:, None] + np.arange(WIN_C)
    onehot = jnp.asarray(col_idx[..., None] == cols, dtype=q.dtype)
    coff = col_idx - cols[:, None] + WIN_C - 1
    bias_c = rpb[:, :, coff]
    kg = k.reshape(Bn, rows, GRID_W, H, D)
    vg = v.reshape(Bn, rows, GRID_W, H, D)
    qg = q.reshape(Bn, rows, GRID_W, H, D).transpose(1, 0, 2, 3, 4)
    scale = D ** -0.5
    nwin = wr * WIN_C

    def one(args):
        qrow, r, r0 = args
        krows = lax.dynamic_slice_in_dim(kg, r0, wr, axis=1)
        vrows = lax.dynamic_slice_in_dim(vg, r0, wr, axis=1)
        roff = r0 + jnp.arange(wr) - r + WIN_R - 1
        bias = bias_c[:, roff].transpose(0, 2, 1, 3)[None]
        s_band = jnp.einsum('bqhd,brkhd->bhqrk', qrow, krows)
        s_win = jnp.einsum('bhqrk,qjk->bhqrj', s_band, onehot).astype(jnp.float32) * scale + bias
        s_ctx = jnp.einsum('bqhd,bkhd->bhqk', qrow, kc).astype(jnp.float32) * scale
        p = jax.nn.softmax(jnp.concatenate([s_win.reshape(Bn, H, GRID_W, nwin), s_ctx], axis=-1), axis=-1)
        p = p.astype(v.dtype)
        p_win = p[..., :nwin].reshape(Bn, H, GRID_W, wr, WIN_C)
        p_band = jnp.einsum('bhqrj,qjk->bhqrk', p_win, onehot)
        return jnp.einsum('bhqrk,brkhd->bqhd', p_band, vrows) + jnp.einsum('bhqk,bkhd->bqhd', p[..., nwin:], vc)

    o = lax.map(one, (qg, jnp.arange(rows, dtype=jnp.int32), jnp.asarray(rs)))
    return o.transpose(1, 0, 2, 3, 4).reshape(Bn, S, H * D)


def peer(h, wq, sub_keys, u, v):
    shp = h.shape
    xt = h.reshape(-1, shp[-1])
    xb = xt.reshape(xt.shape[0] // PEER_BLOCK, PEER_BLOCK, shp[-1])
    half = PEER_DKEY // 2
    ncand = PEER_TOPK * PEER_TOPK

    def one(xblk):
        q = (xblk @ wq).reshape(PEER_BLOCK, PEER_HEADS, 2, half)
        s1 = jnp.einsum('thd,kd->thk', q[:, :, 0], sub_keys[0]).astype(jnp.float32)
        s2 = jnp.einsum('thd,kd->thk', q[:, :, 1], sub_keys[1]).astype(jnp.float32)
        v1, i1 = lax.top_k(s1, PEER_TOPK)
        v2, i2 = lax.top_k(s2, PEER_TOPK)
        cand = (v1[..., :, None] + v2[..., None, :]).reshape(PEER_BLOCK, PEER_HEADS, ncand)
        cidx = (i1[..., :, None] * PEER_NKEYS + i2[..., None, :]).reshape(PEER_BLOCK, PEER_HEADS, ncand)
        sc, pos = lax.top_k(cand, PEER_TOPK)
        eidx = jnp.take_along_axis(cidx, pos, axis=-1)
        gate = jax.nn.softmax(sc, axis=-1)
        ue = jnp.take(u, eidx, axis=0)
        ve = jnp.take(v, eidx, axis=0)
        act = jax.nn.gelu(jnp.einsum('td,thkd->thk', xblk, ue).astype(jnp.float32))
        return jnp.einsum('thk,thkd->td', (gate * act).astype(ve.dtype), ve)

    return lax.map(one, xb).reshape(shp)


def _even_heads(p, gate_b):
    lead = p.shape[:-1]
    qa, ka, va, qb, kb, vb, ob, g = split_cols(p, EVEN_SPLITS)
    return (qa.reshape(*lead, A_HEADS, HEAD_DIM), ka.reshape(*lead, A_KV_HEADS, HEAD_DIM),
            va.reshape(*lead, A_KV_HEADS, HEAD_DIM), qb.reshape(*lead, B_HEADS, B_QK_DIM),
            kb.reshape(*lead, B_HEADS, B_QK_DIM), vb.reshape(*lead, B_HEADS, B_V_DIM),
            ob, jnp.split(g + gate_b, 4, axis=-1))


def _cat(a, b):
    return jnp.concatenate([a, b], axis=1)


def _rev(a):
    return jnp.flip(a, axis=1)


def even_mixer(pl, pc, gate_b, qk_g, hnorm_g, need_ctx):
    Bn, S, _ = pl.shape
    Lc = pc.shape[1]
    qa, ka, va, qb, kb, vb, ob, (i_f, f_f, i_b, f_b) = _even_heads(pl, gate_b)
    qac, kac, vac, qbc, kbc, vbc, obc, (ic_f, fc_f, ic_b, fc_b) = _even_heads(pc, gate_b)
    qa = rope_2d(rmsnorm(qa, qk_g[0]))
    ka = rope_2d(rmsnorm(ka, qk_g[1]))
    qac = rmsnorm(qac, qk_g[0])
    kac = rmsnorm(kac, qk_g[1])
    ya = block_attention(qa, _cat(kac, ka), _cat(vac, va))
    h_fwd = mlstm_chunkwise(_cat(qbc, qb), _cat(kbc, kb), _cat(vbc, vb), _cat(ic_f, i_f), _cat(fc_f, f_f))
    h_bwd = mlstm_chunkwise(_cat(_rev(qbc), _rev(qb)), _cat(_rev(kbc), _rev(kb)), _cat(_rev(vbc), _rev(vb)),
                            _cat(_rev(ic_b), _rev(i_b)), _cat(_rev(fc_b), _rev(f_b)))
    g_h = hnorm_g.reshape(B_HEADS, B_V_DIM)
    h_lat = h_fwd[:, Lc:] + _rev(h_bwd[:, Lc:])
    yb = jax.nn.sigmoid(ob) * rmsnorm(h_lat, g_h).reshape(Bn, S, B_HEADS * B_V_DIM)
    yl = jnp.concatenate([ya, yb], axis=-1)
    yc = None
    if need_ctx:
        yac = block_attention(qac, kac, vac)
        h_ctx = h_fwd[:, :Lc] + _rev(h_bwd[:, :Lc])
        ybc = jax.nn.sigmoid(obc) * rmsnorm(h_ctx, g_h).reshape(Bn, Lc, B_HEADS * B_V_DIM)
        yc = jnp.concatenate([yac, ybc], axis=-1)
    return yl, yc


def odd_mixer(pl, pc, rpb, need_ctx):
    Bn, S, _ = pl.shape
    Lc = pc.shape[1]
    q, k, v = [a.reshape(Bn, S, C_HEADS, HEAD_DIM) for a in jnp.split(pl, 3, axis=-1)]
    qc, kc, vc = [a.reshape(Bn, Lc, C_HEADS, HEAD_DIM) for a in jnp.split(pc, 3, axis=-1)]
    yl = neighbourhood_attention(q, k, v, kc, vc, rpb)
    yc = block_attention(qc, kc, vc) if need_ctx else None
    return yl, yc


def setup_inputs(seed: int = 0) -> dict:
    key = jax.random.key(seed)
    ks = jax.random.split(key, 24)
    D = D_MODEL
    f32 = jnp.float32

    def nrm(k, shape, scale):
        return jax.random.normal(k, shape, f32) * scale

    gk = jax.random.split(ks[8], 4)
    gate_b = jnp.concatenate([
        nrm(gk[0], (N_EVEN, B_HEADS), 0.1),
        3.0 + 3.0 * jax.random.uniform(gk[1], (N_EVEN, B_HEADS), f32),
        nrm(gk[2], (N_EVEN, B_HEADS), 0.1),
        3.0 + 3.0 * jax.random.uniform(gk[3], (N_EVEN, B_HEADS), f32)], axis=-1)
    return {
        'x': nrm(ks[0], (BATCH, SEQ, D), 1.0),
        'c': nrm(ks[1], (BATCH, D), 1.0),
        'ctx': nrm(ks[2], (BATCH, CTX_LEN, D), 1.0),
        'c_ctx': nrm(ks[3], (D,), 1.0),
        'mod_w': nrm(ks[4], (DEPTH, D, 6 * D), 0.5 * D ** -0.5),
        'mod_b': nrm(ks[5], (DEPTH, 6 * D), 0.02),
        'norm_g': 1.0 + nrm(ks[6], (DEPTH, 2, D), 0.02),
        'ev_w_in': nrm(ks[7], (N_EVEN, D, EVEN_COLS), D ** -0.5),
        'ev_gate_b': gate_b,
        'ev_qk_g': 1.0 + nrm(ks[9], (N_EVEN, 2, HEAD_DIM), 0.02),
        'ev_hnorm_g': 1.0 + nrm(ks[10], (N_EVEN, B_HEADS * B_V_DIM), 0.02),
        'ev_w_out': nrm(ks[11], (N_EVEN, D, D), D ** -0.5),
        'od_w_in': nrm(ks[12], (N_ODD, D, ODD_COLS), D ** -0.5),
        'od_rpb': nrm(ks[13], (N_ODD, C_HEADS, 2 * WIN_R - 1, 2 * WIN_C - 1), 0.5),
        'od_w_out': nrm(ks[14], (N_ODD, D, D), D ** -0.5),
        'peer_wq': nrm(ks[15], (DEPTH, D, PEER_HEADS * PEER_DKEY), D ** -0.5),
        'peer_subkeys': nrm(ks[16], (DEPTH, 2, PEER_NKEYS, PEER_DKEY // 2), (PEER_DKEY // 2) ** -0.5),
        'peer_u': nrm(ks[17], (DEPTH, PEER_EXPERTS, D), D ** -0.5),
        'peer_v': nrm(ks[18], (DEPTH, PEER_EXPERTS, D), 0.3),
        'final_g': 1.0 + nrm(ks[19], (D,), 0.02),
    }


def reference(x, c, ctx, c_ctx, mod_w, mod_b, norm_g, ev_w_in, ev_gate_b, ev_qk_g, ev_hnorm_g, ev_w_out,
              od_w_in, od_rpb, od_w_out, peer_wq, peer_subkeys, peer_u, peer_v, final_g):
    xc = ctx
    for layer in range(DEPTH):
        last = layer == DEPTH - 1
        j = layer // 2
        sh1, sc1, g1, sh2, sc2, g2 = modulation(c, mod_w[layer], mod_b[layer])
        csh1, csc1, cg1, csh2, csc2, cg2 = modulation(c_ctx, mod_w[layer], mod_b[layer])
        hl = rmsnorm(x, norm_g[layer, 0]) * (1.0 + sc1[:, None]) + sh1[:, None]
        hc = rmsnorm(xc, norm_g[layer, 0]) * (1.0 + csc1) + csh1
        if layer % 2 == 0:
            yl, yc = even_mixer(hl @ ev_w_in[j], hc @ ev_w_in[j], ev_gate_b[j], ev_qk_g[j], ev_hnorm_g[j], not last)
            w_out = ev_w_out[j]
        else:
            yl, yc = odd_mixer(hl @ od_w_in[j], hc @ od_w_in[j], od_rpb[j], not last)
            w_out = od_w_out[j]
        x = x + g1[:, None] * (yl @ w_out)
        h2 = rmsnorm(x, norm_g[layer, 1]) * (1.0 + sc2[:, None]) + sh2[:, None]
        x = x + g2[:, None] * peer(h2, peer_wq[layer], peer_subkeys[layer], peer_u[layer], peer_v[layer])
        if not last:
            xc = xc + cg1 * (yc @ w_out)
            hc2 = rmsnorm(xc, norm_g[layer, 1]) * (1.0 + csc2) + csh2
            xc = xc + cg2 * peer(hc2, peer_wq[layer], peer_subkeys[layer], peer_u[layer], peer_v[layer])
    return rmsnorm(x, final_g)
```

```python
import numpy as np
import concourse.bass as bass
import concourse.mybir as mybir

F32 = mybir.dt.float32
BF16 = mybir.dt.bfloat16
AF = mybir.ActivationFunctionType
ALU = mybir.AluOpType
AX = mybir.AxisListType

SEM_LIMIT = 30000
import os
N_DMA_SEMS = int(os.environ.get("NDS", "48"))


class KB:
    def __init__(self, nc, stack):
        self.nc = nc
        self.stack = stack
        self.sem_stack = stack
        self.phase_stack = None
        self.eng = {"pe": nc.tensor, "dve": nc.vector, "act": nc.scalar,
                    "pool": nc.gpsimd, "sp": nc.sync}
        self.cnt = {}
        self.semkey = {}
        self.sems = {}
        self.nsem = 0
        for e in ("pe", "dve", "act", "pool"):
            self._new_epoch(e, 0)
        self.epoch = {e: 0 for e in ("pe", "dve", "act", "pool")}
        self.known = {e: {} for e in self.eng}
        self.last_w = {}
        self.readers = {}
        self.dma_sems = []
        for i in range(N_DMA_SEMS):
            s = self.sem_stack.enter_context(nc.semaphore(f"dq{i}"))
            key = ("dma", i)
            self.sems[key] = s
            self.dma_sems.append(key)
        self.dma_uses = {k: 0 for k in self.dma_sems}
        self.dma_rr = 0
        self.ninstr = 0
        self.out_tokens = []
        self.prog = {e: [] for e in self.eng}
        self.psum_keys = set()

    def _new_epoch(self, e, ep):
        key = (e, ep)
        s = self.sem_stack.enter_context(self.nc.semaphore(f"c_{e}_{ep}"))
        self.sems[key] = s
        self.semkey[e] = key
        self.cnt[e] = 0

    def _deps(self, R, W):
        deps = set()
        for b in R:
            t = self.last_w.get(b)
            if t is not None:
                deps.add(t)
        for b in W:
            t = self.last_w.get(b)
            if t is not None:
                deps.add(t)
            for t in self.readers.get(b, ()):
                deps.add(t)
        return deps

    def _record(self, tok, R, W):
        for b in R:
            self.readers.setdefault(b, []).append(tok)
        for b in W:
            self.last_w[b] = tok
            self.readers[b] = []

    def _emit_waits(self, x, deps):
        kn = self.known[x]
        best = {}
        for (key, val) in deps:
            if kn.get(key, 0) >= val:
                continue
            if key == self.semkey.get(x) and val > self.cnt[x]:
                continue
            if best.get(key, 0) < val:
                best[key] = val
        for key, val in best.items():
            self.prog[x].append(("w", self.sems[key], val))
            kn[key] = val
            self.ninstr += 1

    def op(self, x, meth, R=(), W=(), inc=True, **kw):
        import os
        if x == "pool" and os.environ.get("NOPOOL"):
            x = "dve"
        fn = (lambda e, meth=meth, kw=kw: getattr(e, meth)(**kw))
        pk = [b for b in R if b in self.psum_keys]
        if pk:
            R = [b for b in R if b not in self.psum_keys]
            W = list(W) + pk
        deps = self._deps(R, W)
        self._emit_waits(x, deps)
        if self.cnt[x] + 1 > SEM_LIMIT:
            self.epoch[x] += 1
            self._new_epoch(x, self.epoch[x])
        self.ninstr += 1
        key = self.semkey[x]
        if inc:
            self.cnt[x] += 1
            self.prog[x].append(("o", fn, self.sems[key], 1))
            tok = (key, self.cnt[x])
        else:
            self.prog[x].append(("o", fn, None, 0))
            tok = (key, self.cnt[x] + 1)
        self._record(tok, R, W)
        return tok

    def dma(self, out, in_, R=(), W=(), q="sp", is_output=False, **kw):
        key = self.dma_sems[self.dma_rr % N_DMA_SEMS]
        self.dma_rr += 1
        deps = self._deps(R, W)
        prev = self.dma_uses[key]
        if prev:
            deps.add((key, 16 * prev))
        self._emit_waits(q, deps)
        self.prog[q].append(("o", (lambda e, out=out, in_=in_, kw=kw: e.dma_start(out=out, in_=in_, **kw)),
                             self.sems[key], 16))
        self.ninstr += 1
        self.dma_uses[key] = prev + 1
        tok = (key, 16 * (prev + 1))
        self._record(tok, R, W)
        if is_output:
            self.out_tokens.append(tok)
        return tok

    def finish(self, q="sp"):
        self._emit_waits(q, set(self.out_tokens))
        deps = set()
        for e in ("pe", "dve", "act", "pool"):
            if self.cnt[e] > 0:
                deps.add((self.semkey[e], self.cnt[e]))
        for key, n in self.dma_uses.items():
            if n:
                deps.add((key, 16 * n))
        self._emit_waits(q, deps)
        self.emit()

    def begin_phase(self):
        from contextlib import ExitStack
        ps = ExitStack()
        ps.__enter__()
        if not hasattr(self, "_stk"):
            self._stk = []
        self._stk.append(self.stack)
        self.stack = ps

    def end_phase(self, q="sp"):
        deps = set()
        for key, n in self.dma_uses.items():
            if n:
                deps.add((key, 16 * n))
        for e in ("pe", "dve", "act", "pool"):
            if self.cnt[e] > 0:
                deps.add((self.semkey[e], self.cnt[e]))
        self._emit_waits(q, deps)
        self.emit()
        self.prog = {e: [] for e in self.eng}
        ps = self.stack
        self.stack = self._stk.pop()
        ps.__exit__(None, None, None)
        self.last_w = {b: t for b, t in self.last_w.items() if isinstance(b, str) and b.startswith("D:")}
        self.readers = {b: t for b, t in self.readers.items() if isinstance(b, str) and b.startswith("D:")}

    def emit(self):
        def runner(lst):
            def f(eng):
                for it in lst:
                    if it[0] == "w":
                        eng.wait_ge(it[1], it[2])
                    else:
                        ins = it[1](eng)
                        if it[2] is not None:
                            ins.then_inc(it[2], it[3])
            return f
        with self.nc.Block() as block:
            block.sync(runner(self.prog["sp"]))
            block.tensor(runner(self.prog["pe"]))
            block.vector(runner(self.prog["dve"]))
            block.scalar(runner(self.prog["act"]))
            block.gpsimd(runner(self.prog["pool"]))

    def sb(self, name, shape, dtype=F32):
        self.nsem += 1
        return self.stack.enter_context(self.nc.sbuf_tensor(f"{name}_{self.nsem}", list(shape), dtype))

    def ps(self, name, shape, dtype=F32):
        self.nsem += 1
        self.psum_keys.add(name)
        return self.stack.enter_context(self.nc.psum_tensor(f"{name}_{self.nsem}", list(shape), dtype))


import numpy as np
from contextlib import ExitStack
import concourse.bass as bass
import concourse.mybir as mybir
from concourse.bass_utils import run_bass_kernel_spmd
import ml_dtypes

BF = ml_dtypes.bfloat16
EPS = 1e-6
D = 4096


def new_nc():
    return bass.Bass("TRN2", target_bir_lowering=False)


def din(nc, name, shape, dt=F32):
    return nc.dram_tensor(name, list(shape), dt, kind="ExternalInput").ap()


def dout(nc, name, shape, dt=F32):
    return nc.dram_tensor(name, list(shape), dt, kind="ExternalOutput").ap()


def dscr(nc, name, shape, dt=F32):
    return nc.dram_tensor(name, list(shape), dt, kind="Internal").ap()


class NormT:
    def __init__(self, k, ident, nbuf=2, pfx="n"):
        self.k = k
        self.ident = ident
        self.pfx = pfx
        self.xt = [k.sb(f"{pfx}xt{i}", [128, D]) for i in range(nbuf)]
        self.tmp = k.sb(f"{pfx}tmp", [128, D])
        self.hb = [k.sb(f"{pfx}hb{i}", [128, D], BF16) for i in range(nbuf)]
        self.ss = [k.sb(f"{pfx}ss{i}", [128, 2]) for i in range(nbuf)]
        self.pst = [k.ps(f"{pfx}pst{i}", [128, 1024], BF16) for i in range(2)]
        self.n = 0
        self.nps = 0

    def tile(self, x_rows, xkey_R, scale_bc, shift_bc, scale_key, hT, hT_key):
        k = self.k
        i = self.n % len(self.xt)
        self.n += 1
        p = self.pfx
        xt, hb, ss = self.xt[i], self.hb[i], self.ss[i]
        k.dma(xt[:], x_rows, R=xkey_R, W=[f"{p}xt{i}"])
        k.op("act", "activation", out=self.tmp[:], in_=xt[:], func=AF.Square, accum_out=ss[:, 0:1],
             R=[f"{p}xt{i}"], W=[f"{p}tmp", f"{p}ss{i}"])
        k.op("dve", "tensor_scalar", out=ss[:, 1:2], in0=ss[:, 0:1], scalar1=1.0 / D, scalar2=EPS,
             op0=ALU.mult, op1=ALU.add, R=[f"{p}ss{i}"], W=[f"{p}ss{i}"])
        k.op("act", "activation", out=ss[:, 1:2], in_=ss[:, 1:2], func=AF.Sqrt, R=[f"{p}ss{i}"], W=[f"{p}ss{i}"])
        k.op("dve", "reciprocal", out=ss[:, 1:2], in_=ss[:, 1:2], R=[f"{p}ss{i}"], W=[f"{p}ss{i}"])
        k.op("dve", "scalar_tensor_tensor", out=self.tmp[:], in0=xt[:], scalar=ss[:, 1:2], in1=scale_bc,
             op0=ALU.mult, op1=ALU.mult, R=[f"{p}xt{i}", f"{p}ss{i}", scale_key], W=[f"{p}tmp"])
        k.op("pool", "tensor_tensor", out=hb[:], in0=self.tmp[:], in1=shift_bc, op=ALU.add,
             R=[f"{p}tmp", scale_key], W=[f"{p}hb{i}"])
        for b in range(4):
            j = self.nps % 2
            self.nps += 1
            for q in range(8):
                kk = b * 8 + q
                k.op("pe", "transpose", out=self.pst[j][:, q * 128:(q + 1) * 128],
                     in_=hb[:, kk * 128:(kk + 1) * 128], identity=self.ident,
                     R=[f"{p}hb{i}", "ident"], W=[f"{p}pst{j}"], inc=(q == 7))
            eng = "act" if b % 2 == 0 else "dve"
            meth = "copy" if eng == "act" else "tensor_copy"
            k.op(eng, meth, out=hT[:, b * 8:(b + 1) * 8, :],
                 in_=self.pst[j][:].rearrange("p (a b) -> p a b", a=8),
                 R=[f"{p}pst{j}"], W=[hT_key])


def load_bc(k, dst, vec_ap, key):
    k.dma(dst, vec_ap.partition_broadcast(128), W=[key])


def build_B(NT=9):
    nc = new_nc()
    x = din(nc, "x", [NT * 128, D])
    mv = din(nc, "mv", [5, D])
    idn = din(nc, "ident", [128, 128], BF16)
    hT = dout(nc, "hT", [D, NT * 128], BF16)
    with ExitStack() as st:
        k = KB(nc, st)
        ident = k.sb("ident_sb", [128, 128], BF16)
        k.dma(ident[:], idn, W=["ident"])
        mods = emit_mods(k, mv)
        nt = NormT(k, ident[:])
        hts = [k.sb(f"hT{i}", [128, 32, 128], BF16) for i in range(2)]
        for t in range(NT):
            sc, sh = mods[0] if t == 0 else mods[1]
            j = t % 2
            nt.tile(x[t * 128:(t + 1) * 128, :], [], sc[:], sh[:], "mods", hts[j], f"hT{j}")
            k.dma(hT[:, t * 128:(t + 1) * 128].rearrange("(a p) t -> p a t", p=128), hts[j][:],
                  R=[f"hT{j}"], W=["hTout"], is_output=True)
        k.finish()
    return nc


def emit_mods(k, mv, pfx="m"):
    g = k.sb(pfx + "g", [128, D])
    tiles = [k.sb(f"{pfx}{i}", [128, D]) for i in range(4)]
    load_bc(k, g[:], mv[0:1, :], "mods_g")
    for i in range(4):
        load_bc(k, tiles[i][:], mv[1 + i:2 + i, :], f"mods_r{i}")
    for i in (0, 2):
        k.op("dve", "scalar_tensor_tensor", out=tiles[i][:], in0=tiles[i][:], scalar=1.0, in1=g[:],
             op0=ALU.add, op1=ALU.mult, R=["mods_g", f"mods_r{i}"], W=[f"mods_r{i}", "mods"])
    k.op("dve", "tensor_copy", out=g[:, 0:1], in_=tiles[1][:, 0:1], R=["mods_r1", "mods_r3", "mods_g"], W=["mods", "mods_g"])
    return [(tiles[0], tiles[1]), (tiles[2], tiles[3])]


def ref_B(x, mv):
    xf = x.astype(np.float64)
    y = xf / np.sqrt((xf * xf).mean(-1, keepdims=True) + EPS) * mv[0]
    out = np.empty_like(y)
    out[:128] = y[:128] * (1 + mv[1]) + mv[2]
    out[128:] = y[128:] * (1 + mv[3]) + mv[4]
    return out.T


C_Q, C_K, C_V, C_QB, C_KB, C_VB, C_OB, C_GT, C_N = 0, 512, 640, 768, 1024, 1280, 1792, 2304, 2312
ATT_SCALE = 128 ** -0.5


def emit_linear_tm(k, hT, W, out, T, N, out_key, pfx="l"):
    wb = k.sb(pfx + "wb", [128, 32, 512], BF16)
    stg = [k.sb(f"{pfx}stg{i}", [128, 8, 512]) for i in range(2)]
    hb = [k.sb(f"{pfx}hb{i}", [128, 32, 512], BF16) for i in range(2)]
    osb = [k.sb(f"{pfx}o{i}", [128, 512]) for i in range(3)]
    ps = [k.ps(f"{pfx}ps{i}", [128, 512]) for i in range(3)]
    ns = nh = no = 0
    for c0 in range(0, N, 512):
        nw = min(512, N - c0)
        for q in range(4):
            j = ns % 2
            ns += 1
            k.dma(stg[j][:, :, :nw], W[q * 1024:(q + 1) * 1024, c0:c0 + nw].rearrange("(a p) n -> p a n", p=128),
                  W=[f"{pfx}stg{j}"])
            if q % 2 == 0:
                k.op("pool", "tensor_copy", out=wb[:, q * 8:(q + 1) * 8, :nw], in_=stg[j][:, :, :nw],
                     R=[f"{pfx}stg{j}"], W=[pfx + "wb"])
            else:
                k.op("act", "copy", out=wb[:, q * 8:(q + 1) * 8, :nw], in_=stg[j][:, :, :nw],
                     R=[f"{pfx}stg{j}"], W=[pfx + "wb"])
        for t0 in range(0, T, 512):
            tw = min(512, T - t0)
            jh = nh % 2
            nh += 1
            k.dma(hb[jh][:, :, :tw], hT[:, t0:t0 + tw].rearrange("(a p) t -> p a t", p=128), W=[f"{pfx}hb{jh}"])
            for tt in range(0, tw, 128):
                jo = no % 3
                no += 1
                for kk in range(32):
                    k.op("pe", "matmul", out=ps[jo][:, :nw], lhsT=hb[jh][:, kk, tt:tt + 128], rhs=wb[:, kk, :nw],
                         start=(kk == 0), stop=(kk == 31), R=[f"{pfx}hb{jh}", pfx + "wb"], W=[f"{pfx}ps{jo}"],
                         inc=(kk == 31))
                if jo % 2 == 0:
                    k.op("act", "copy", out=osb[jo][:, :nw], in_=ps[jo][:, :nw], R=[f"{pfx}ps{jo}"], W=[f"{pfx}o{jo}"])
                else:
                    k.op("dve", "tensor_copy", out=osb[jo][:, :nw], in_=ps[jo][:, :nw], R=[f"{pfx}ps{jo}"],
                         W=[f"{pfx}o{jo}"])
                k.dma(out[t0 + tt:t0 + tt + 128, c0:c0 + nw], osb[jo][:, :nw], R=[f"{pfx}o{jo}"], W=[out_key])


def build_C0(CT=2, LT=32, stop_after=3, part="all"):
    NTt = CT + LT
    T = NTt * 128
    nc = new_nc()
    hT = din(nc, "hT", [D, T], BF16)
    W = din(nc, "W", [D, C_N])
    cst = din(nc, "cst", [128, 5, 128])
    idn = din(nc, "ident", [128, 128], BF16)
    qkg = din(nc, "qkg", [2, 128])
    gb = din(nc, "gb", [1, 8])
    hng = din(nc, "hng", [2, 256])
    cs_t = din(nc, "rope", [LT * 128, 128])
    yT = dout(nc, "yT", [1024, T], BF16)
    if part == "a":
        Pm = dout(nc, "Pm", [T, C_N])
    elif part == "b":
        Pm = din(nc, "Pm", [T, C_N])
    else:
        Pm = dscr(nc, "Pm", [T, C_N])
    with ExitStack() as st:
        k = KB(nc, st)
        import os
        if part != "b":
            k.begin_phase()
            emit_linear_tm(k, hT, W, Pm, T, C_N, "D:Pm")
            k.end_phase()
        if stop_after < 1.5 or part == "a":
            return nc
        k.begin_phase()
        ident = k.sb("ident_sb", [128, 128], BF16)
        k.dma(ident[:], idn, W=["ident"])
        ones = k.sb("ones", [128, 128], BF16)
        k.op("dve", "memset", ap=ones[:], constant=1.0, W=["ones"])
        g5 = k.sb("g5", [128, 5, 128])
        for j in range(5):
            k.dma(g5[:, j, :], qkg[(0 if j < 4 else 1):(1 if j < 4 else 2), :].partition_broadcast(128), W=["g5"])
        QT = k.sb("QT", [128, 4, T], BF16)
        KT = k.sb("KT", [128, T], BF16)
        Vs = k.sb("Vs", [128, NTt, 128], BF16)
        xin = [k.sb(f"xin{i}", [128, 768]) for i in range(2)]
        cs = [k.sb(f"cs{i}", [128, 128]) for i in range(2)]
        sq = k.sb("sq", [128, 5, 128])
        xn = k.sb("xn", [128, 5, 128])
        ta = k.sb("ta", [128, 5, 64])
        tb = k.sb("tb", [128, 5, 64])
        xo = [k.sb(f"xo{i}", [128, 5, 128], BF16) for i in range(2)]
        st5 = k.sb("st5", [128, 10])
        pst = [k.ps(f"pst{i}", [128, 1024], BF16) for i in range(2)]
        for t in range(NTt):
            i = t % 2
            k.dma(xin[i][:], Pm[t * 128:(t + 1) * 128, 0:768], R=["D:Pm"], W=[f"xin{i}"])
            xv = xin[i][:, 0:640].rearrange("p (h d) -> p h d", h=5)
            if int(os.environ.get("PREP_CUT", "9")) < 1:
                continue
            k.op("pool", "tensor_tensor", out=sq[:], in0=xv, in1=xv, op=ALU.mult, R=[f"xin{i}"], W=["sq"])
            k.op("dve", "tensor_reduce", out=st5[:, 0:5], in_=sq[:], op=ALU.add, axis=AX.X, R=["sq"], W=["st5"])
            k.op("dve", "tensor_scalar", out=st5[:, 5:10], in0=st5[:, 0:5], scalar1=1.0 / 128, scalar2=EPS,
                 op0=ALU.mult, op1=ALU.add, R=["st5"], W=["st5"])
            k.op("act", "activation", out=st5[:, 5:10], in_=st5[:, 5:10], func=AF.Sqrt, R=["st5"], W=["st5"])
            k.op("dve", "reciprocal", out=st5[:, 5:10], in_=st5[:, 5:10], R=["st5"], W=["st5"])
            k.op("dve", "tensor_tensor", out=xn[:], in0=xv, in1=st5[:, 5:10].unsqueeze(2).to_broadcast([128, 5, 128]),
                 op=ALU.mult, R=[f"xin{i}", "st5"], W=["xn"])
            if t >= CT:
                k.dma(cs[i][:], cs_t[(t - CT) * 128:(t - CT + 1) * 128, :], W=[f"cs{i}"])
                k.op("pool", "tensor_tensor", out=xn[:], in0=xn[:], in1=g5[:], op=ALU.mult, R=["xn", "g5"], W=["xn"])
                cosb = cs[i][:, 0:64].unsqueeze(1).to_broadcast([128, 5, 64])
                sinb = cs[i][:, 64:128].unsqueeze(1).to_broadcast([128, 5, 64])
                x1, x2 = xn[:, :, 0:64], xn[:, :, 64:128]
                k.op("dve", "tensor_tensor", out=ta[:], in0=x1, in1=cosb, op=ALU.mult, R=["xn", f"cs{i}"], W=["ta"])
                k.op("dve", "tensor_tensor", out=tb[:], in0=x2, in1=sinb, op=ALU.mult, R=["xn", f"cs{i}"], W=["tb"])
                k.op("dve", "tensor_tensor", out=xo[i][:, :, 0:64], in0=ta[:], in1=tb[:], op=ALU.subtract,
                     R=["ta", "tb"], W=[f"xo{i}"])
                k.op("dve", "tensor_tensor", out=ta[:], in0=x1, in1=sinb, op=ALU.mult, R=["xn", f"cs{i}"], W=["ta"])
                k.op("dve", "tensor_tensor", out=tb[:], in0=x2, in1=cosb, op=ALU.mult, R=["xn", f"cs{i}"], W=["tb"])
                k.op("pool", "tensor_tensor", out=xo[i][:, :, 64:128], in0=ta[:], in1=tb[:], op=ALU.add,
                     R=["ta", "tb"], W=[f"xo{i}"])
            else:
                k.op("pool", "tensor_tensor", out=xo[i][:], in0=xn[:], in1=g5[:], op=ALU.mult, R=["xn", "g5"],
                     W=[f"xo{i}"])
            if int(os.environ.get("PREP_CUT", "9")) < 3:
                continue
            for j in range(5):
                k.op("pe", "transpose", out=pst[i][:, j * 128:(j + 1) * 128], in_=xo[i][:, j, :], identity=ident[:],
                     R=[f"xo{i}", "ident"], W=[f"pst{i}"], inc=(j == 4))
            if int(os.environ.get("PREP_CUT", "9")) < 5:
                continue
            k.op("act", "copy", out=QT[:, :, t * 128:(t + 1) * 128],
                 in_=pst[i][:, 0:512].rearrange("p (h d) -> p h d", h=4), R=[f"pst{i}"], W=["QT"])
            k.op("dve", "tensor_copy", out=KT[:, t * 128:(t + 1) * 128], in_=pst[i][:, 512:640], R=[f"pst{i}"], W=["KT"])
            k.op("act", "copy", out=Vs[:, t, :], in_=xin[i][:, 640:768], R=[f"xin{i}"], W=["Vs"])
        if stop_after < 1.7:
            k.end_phase()
            return nc
        E = [k.sb(f"E{i}", [128, 512], BF16) for i in range(3)]
        pS = [k.ps(f"pS{i}", [128, 512]) for i in range(2)]
        pO = [k.ps(f"pO{i}", [128, 512]) for i in range(2)]
        pD = [k.ps(f"pD{i}", [128, 512]) for i in range(2)]
        rc = k.sb("rc", [128, 512])
        yo = [k.sb(f"yo{i}", [128, 512], BF16) for i in range(2)]
        nE = nS = nb = 0
        for j in range(4):
            blocks = [(0, CT * 128, range(0, CT))]
            for q0 in range(CT * 128, T, 512):
                blocks.append((q0, min(512, T - q0), range(0, NTt)))
            for (q0, qw, kts) in blocks:
                ib = nb % 2
                nb += 1
                kts = list(kts)
                for n_, kt in enumerate(kts):
                    iS = nS % 2
                    nS += 1
                    iE = nE % 3
                    nE += 1
                    k.op("pe", "matmul", out=pS[iS][:, :qw], lhsT=KT[:, kt * 128:(kt + 1) * 128],
                         rhs=QT[:, j, q0:q0 + qw], start=True, stop=True, R=["KT", "QT"], W=[f"pS{iS}"])
                    k.op("act", "activation", out=E[iE][:, :qw], in_=pS[iS][:, :qw], func=AF.Exp, scale=ATT_SCALE,
                         R=[f"pS{iS}"], W=[f"E{iE}"])
                    last = (n_ == len(kts) - 1)
                    k.op("pe", "matmul", out=pO[ib][:, :qw], lhsT=Vs[:, kt, :], rhs=E[iE][:, :qw], start=(n_ == 0),
                         stop=last, R=["Vs", f"E{iE}"], W=[f"pO{ib}"], inc=last)
                    k.op("pe", "matmul", out=pD[ib][:, :qw], lhsT=ones[:], rhs=E[iE][:, :qw], start=(n_ == 0),
                         stop=last, R=["ones", f"E{iE}"], W=[f"pD{ib}"], inc=last)
                k.op("dve", "reciprocal", out=rc[:, :qw], in_=pD[ib][:, :qw], R=[f"pD{ib}"], W=["rc"])
                k.op("dve", "tensor_tensor", out=yo[ib][:, :qw], in0=pO[ib][:, :qw], in1=rc[:, :qw], op=ALU.mult,
                     R=[f"pO{ib}", "rc"], W=[f"yo{ib}"])
                k.dma(yT[j * 128:(j + 1) * 128, q0:q0 + qw], yo[ib][:, :qw], R=[f"yo{ib}"], W=["D:yT"], is_output=True)
        k.end_phase()
        if stop_after < 3:
            return nc
        k.begin_phase()
        ident = k.sb("ident_sb", [128, 128], BF16)
        k.dma(ident[:], idn, W=["ident"])
        cst_sb = k.sb("cst_sb", [128, 5, 128])
        k.dma(cst_sb[:], cst, W=["cst"])
        hgb = k.sb("hgb", [128, 2, 256])
        for h in range(2):
            k.dma(hgb[:, h, :], hng[h:h + 1, :].partition_broadcast(128), W=["hgb"])
        gbb = k.sb("gbb", [128, 8])
        k.dma(gbb[:], gb.partition_broadcast(128), W=["gbb"])
        Gt = k.sb("Gt", [128, NTt, 8])
        k.dma(Gt[:], Pm[:, C_GT:C_GT + 8].rearrange("(n p) c -> p n c", p=128), R=["D:Pm"], W=["Gt"])
        k.op("dve", "tensor_tensor", out=Gt[:], in0=Gt[:], in1=gbb[:].unsqueeze(1).to_broadcast([128, NTt, 8]),
             op=ALU.add, R=["Gt", "gbb"], W=["Gt"])
        LF = k.sb("LF", [128, 2, NTt, 2])
        for d_ in range(2):
            zc = Gt[:, :, 2 + 4 * d_:4 + 4 * d_]
            k.op("act", "activation", out=LF[:, d_, :, :], in_=zc, func=AF.Exp, scale=-1.0, R=["Gt"], W=["LF"])
        k.op("act", "activation", out=LF[:], in_=LF[:], func=AF.Ln, bias=1.0, R=["LF"], W=["LF"])
        k.op("dve", "tensor_scalar", out=LF[:], in0=LF[:], scalar1=-1.0, scalar2=None, op0=ALU.mult, R=["LF"], W=["LF"])
        pg = k.ps("pg", [128, 512])
        NG = NTt * 2
        for d_ in range(2):
            rhs = LF[:, d_, :, :].rearrange("p n h -> p (n h)")
            k.op("pe", "matmul", out=pg[:, d_ * NG:(d_ + 1) * NG], lhsT=cst_sb[:, d_, :], rhs=rhs, start=True, stop=True,
                 R=["cst", "LF"], W=["pg"])
            k.op("pe", "matmul", out=pg[:, (2 + d_) * NG:(3 + d_) * NG], lhsT=cst_sb[:, 2, :], rhs=rhs, start=True,
                 stop=True, R=["cst", "LF"], W=["pg"])
        Aa = k.sb("Aa", [128, 2, NTt, 2])
        Cc = k.sb("Cc", [128, 2, NTt, 2])
        EB = k.sb("EB", [128, 2, NTt, 2])
        k.op("act", "activation", out=Aa[:].rearrange("p d n h -> p (d n h)"), in_=pg[:, 0:2 * NG], func=AF.Exp,
             R=["pg"], W=["Aa"])
        k.op("act", "activation", out=EB[:].rearrange("p d n h -> p (d n h)"), in_=pg[:, 2 * NG:4 * NG], func=AF.Exp,
             R=["pg"], W=["EB"])
        for d_ in range(2):
            k.op("dve", "tensor_tensor", out=Cc[:, d_, :, :], in0=Gt[:, :, 4 * d_:4 * d_ + 2],
                 in1=pg[:, d_ * NG:(d_ + 1) * NG].rearrange("p (n h) -> p n h", h=2), op=ALU.subtract,
                 R=["Gt", "pg"], W=["Cc"])
        lnsc = k.sb("lnsc", [128, 1])
        k.op("dve", "memset", ap=lnsc[:], constant=float(-0.5 * np.log(128.0)), W=["lnsc"])
        k.op("act", "activation", out=Cc[:], in_=Cc[:], func=AF.Exp, bias=lnsc[:, 0:1], R=["Cc", "lnsc"], W=["Cc"])
        stg = k.sb("stg", [128, NTt, 256])
        Hs = k.sb("Hs", [128, NTt, 256])
        hsc = k.sb("hsc", [128, 4])
        pst = [k.ps(f"pst{i}", [128, 1024], BF16) for i in range(2)]
        pP = [k.ps(f"pP{i}", [128, 512]) for i in range(2)]
        pN = [k.ps(f"pN{i}", [128, 512]) for i in range(2)]
        pC = k.ps("pC", [128, 512])
        npst = 0

        def transpose_all(src, dst, src_key, dst_key, ncol_tiles=1):
            nonlocal npst
            for a in range(ncol_tiles):
                for c0 in range(0, NTt, 8):
                    cn = min(8, NTt - c0)
                    i = npst % 2
                    npst += 1
                    for c in range(cn):
                        k.op("pe", "transpose", out=pst[i][:, c * 128:(c + 1) * 128],
                             in_=src[:, c0 + c, a * 128:(a + 1) * 128], identity=ident[:],
                             R=[src_key, "ident"], W=[f"pst{i}"], inc=(c == cn - 1))
                    dv = dst[:, c0 * 128:(c0 + cn) * 128] if ncol_tiles == 1 else dst[:, a, c0 * 128:(c0 + cn) * 128]
                    if (c0 // 8) % 2 == 0:
                        k.op("act", "copy", out=dv, in_=pst[i][:, :cn * 128], R=[f"pst{i}"], W=[dst_key])
                    else:
                        k.op("dve", "tensor_copy", out=dv, in_=pst[i][:, :cn * 128], R=[f"pst{i}"], W=[dst_key])

        for hh in range(2):
            k.begin_phase()
            qb = k.sb("qb", [128, NTt, 128], BF16)
            kf = [k.sb(f"kf{d_}", [128, NTt, 128], BF16) for d_ in range(2)]
            qT = k.sb("qT", [128, T], BF16)
            kT = [k.sb(f"kT{d_}", [128, T], BF16) for d_ in range(2)]
            vt = k.sb("vt", [128, NTt, 257], BF16)
            Cst = k.sb("Cst", [128, 257])
            Cb = k.sb("Cb", [128, 257], BF16)
            PTm = [k.sb(f"PTm{i}", [128, 128], BF16) for i in range(2)]
            k.dma(stg[:, :, 0:128], Pm[:, C_QB + 128 * hh:C_QB + 128 * hh + 128].rearrange("(n p) c -> p n c", p=128),
                  R=["D:Pm"], W=["stg"])
            k.op("act", "copy", out=qb[:], in_=stg[:, :, 0:128], R=["stg"], W=["qb"])
            transpose_all(qb, qT, "qb", "qT")
            k.dma(stg[:, :, 0:128], Pm[:, C_KB + 128 * hh:C_KB + 128 * hh + 128].rearrange("(n p) c -> p n c", p=128),
                  R=["D:Pm"], W=["stg"])
            for d_ in range(2):
                k.op("dve", "tensor_tensor", out=kf[d_][:], in0=stg[:, :, 0:128],
                     in1=Cc[:, d_, :, hh:hh + 1].to_broadcast([128, NTt, 128]), op=ALU.mult,
                     R=["stg", "Cc"], W=[f"kf{d_}"])
                transpose_all(kf[d_], kT[d_], f"kf{d_}", f"kT{d_}")
            k.dma(stg[:], Pm[:, C_VB + 256 * hh:C_VB + 256 * hh + 256].rearrange("(n p) c -> p n c", p=128),
                  R=["D:Pm"], W=["stg"])
            k.op("act", "copy", out=vt[:, :, 0:256], in_=stg[:], R=["stg"], W=["vt"])
            k.op("dve", "memset", ap=vt[:, :, 256:257], constant=1.0, W=["vt"])
            nP = 0
            for d_ in range(2):
                order = list(range(NTt)) if d_ == 0 else (list(range(CT - 1, -1, -1)) + list(range(NTt - 1, CT - 1, -1)))
                k.op("dve", "memset", ap=Cst[:], constant=0.0, W=["Cst"])
                k.op("dve", "memset", ap=Cb[:], constant=0.0, W=["Cb"])
                for c in order:
                    i = nP % 2
                    nP += 1
                    cols = slice(c * 128, (c + 1) * 128)
                    k.op("pe", "matmul", out=pP[i][:, 0:128], lhsT=kT[d_][:, cols], rhs=qT[:, cols], start=True, stop=True,
                         R=[f"kT{d_}", "qT"], W=[f"pP{i}"])
                    k.op("dve", "tensor_tensor", out=PTm[i][:], in0=pP[i][:, 0:128], in1=cst_sb[:, d_, :], op=ALU.mult,
                         R=[f"pP{i}", "cst"], W=[f"PTm{i}"])
                    k.op("pe", "matmul", out=pN[i][:, 0:257], lhsT=qT[:, cols], rhs=Cb[:], start=True, stop=False,
                         R=["qT", "Cb"], W=[f"pN{i}"], inc=False)
                    k.op("pe", "matmul", out=pN[i][:, 0:257], lhsT=PTm[i][:], rhs=vt[:, c, :], start=False, stop=True,
                         R=[f"PTm{i}", "vt"], W=[f"pN{i}"])
                    k.op("pe", "matmul", out=pC[:, 0:257], lhsT=kf[d_][:, c, :], rhs=vt[:, c, :], start=True, stop=True,
                         R=[f"kf{d_}", "vt"], W=["pC"])
                    eb = EB[:, d_, c, hh:hh + 1]
                    k.op("dve", "tensor_scalar", out=Cst[:], in0=Cst[:], scalar1=eb, scalar2=None, op0=ALU.mult,
                         R=["Cst", "EB"], W=["Cst"])
                    k.op("dve", "scalar_tensor_tensor", out=Cst[:], in0=pC[:, 0:257], scalar=eb, in1=Cst[:],
                         op0=ALU.mult, op1=ALU.add, R=["pC", "EB", "Cst"], W=["Cst"])
                    k.op("act", "copy", out=Cb[:], in_=Cst[:], R=["Cst"], W=["Cb"])
                    a_ = Aa[:, d_, c, hh:hh + 1]
                    k.op("dve", "tensor_scalar", out=hsc[:, 0:1], in0=pN[i][:, 256:257], scalar1=a_, scalar2=None,
                         op0=ALU.mult, R=[f"pN{i}", "Aa"], W=["hsc"])
                    k.op("dve", "tensor_scalar", out=hsc[:, 1:2], in0=hsc[:, 0:1], scalar1=-1.0, scalar2=None,
                         op0=ALU.mult, R=["hsc"], W=["hsc"])
                    k.op("dve", "scalar_tensor_tensor", out=hsc[:, 1:2], in0=hsc[:, 1:2], scalar=1.0, in1=hsc[:, 0:1],
                         op0=ALU.max, op1=ALU.max, R=["hsc"], W=["hsc"])
                    k.op("dve", "reciprocal", out=hsc[:, 2:3], in_=hsc[:, 1:2], R=["hsc"], W=["hsc"])
                    k.op("dve", "tensor_tensor", out=hsc[:, 3:4], in0=hsc[:, 2:3], in1=a_, op=ALU.mult,
                         R=["hsc", "Aa"], W=["hsc"])
                    if d_ == 0:
                        k.op("dve", "tensor_scalar", out=Hs[:, c, :], in0=pN[i][:, 0:256], scalar1=hsc[:, 3:4],
                             scalar2=None, op0=ALU.mult, R=[f"pN{i}", "hsc"], W=["Hs"])
                    else:
                        k.op("dve", "scalar_tensor_tensor", out=Hs[:, c, :], in0=pN[i][:, 0:256], scalar=hsc[:, 3:4],
                             in1=Hs[:, c, :], op0=ALU.mult, op1=ALU.add, R=[f"pN{i}", "hsc", "Hs"], W=["Hs"])
            k.end_phase()
            k.begin_phase()
            Yb = k.sb("Yb", [128, NTt, 256], BF16)
            yTo = k.sb("yTo", [128, 2, T], BF16)
            rst = k.sb("rst", [128, 2, NTt])
            k.dma(stg[:], Pm[:, C_OB + 256 * hh:C_OB + 256 * hh + 256].rearrange("(n p) c -> p n c", p=128),
                  R=["D:Pm"], W=["stg"])
            k.op("act", "activation", out=stg[:], in_=stg[:], func=AF.Sigmoid, R=["stg"], W=["stg"])
            k.op("pool", "tensor_tensor", out=Yb[:], in0=Hs[:], in1=Hs[:], op=ALU.mult, R=["Hs"], W=["Yb"])
            k.op("dve", "tensor_reduce", out=rst[:, 0, :], in_=Yb[:], op=ALU.add, axis=AX.X, R=["Yb"], W=["rst"])
            k.op("dve", "tensor_scalar", out=rst[:, 1, :], in0=rst[:, 0, :], scalar1=1.0 / 256, scalar2=EPS,
                 op0=ALU.mult, op1=ALU.add, R=["rst"], W=["rst"])
            k.op("act", "activation", out=rst[:, 1, :], in_=rst[:, 1, :], func=AF.Sqrt, R=["rst"], W=["rst"])
            k.op("dve", "reciprocal", out=rst[:, 1, :], in_=rst[:, 1, :], R=["rst"], W=["rst"])
            k.op("dve", "tensor_tensor", out=Hs[:], in0=Hs[:], in1=rst[:, 1, :].unsqueeze(2).to_broadcast([128, NTt, 256]),
                 op=ALU.mult, R=["Hs", "rst"], W=["Hs"])
            k.op("dve", "tensor_tensor", out=Hs[:], in0=Hs[:], in1=hgb[:, hh, :].unsqueeze(1).to_broadcast([128, NTt, 256]),
                 op=ALU.mult, R=["Hs", "hgb"], W=["Hs"])
            k.op("dve", "tensor_tensor", out=Yb[:], in0=Hs[:], in1=stg[:], op=ALU.mult, R=["Hs", "stg"], W=["Yb"])
            transpose_all(Yb, yTo, "Yb", "yTo", ncol_tiles=2)
            for a in range(2):
                k.dma(yT[512 + hh * 256 + a * 128:512 + hh * 256 + (a + 1) * 128, :], yTo[:, a, :], R=["yTo"],
                      W=["D:yT"], is_output=True)
            k.end_phase()
        k.end_phase()
    return nc


NH1 = 8


def na_bias_table(rpb_h, rows):
    out = np.full((128, 8, 4, 64), -30000.0, np.float32)
    q = np.arange(64)
    cs = np.clip(q - 8, 0, 48)
    for di in range(8):
        for a in range(4):
            for rr2 in range(2):
                rr = 2 * a + rr2
                roff = 7 - di + rr
                for kc in range(64):
                    valid = (kc >= cs) & (kc < cs + 16)
                    coff = np.clip(kc - q + 15, 0, 30)
                    out[rr2 * 64 + kc, di, a, :] = np.where(valid, rpb_h[roff, coff], -30000.0)
    return out


def build_C1(CT=2, LT=32, nheads=NH1):
    NTt = CT + LT
    T = NTt * 128
    L0 = CT * 128
    rows = LT * 2
    NW = 3 * 128 * nheads
    nc = new_nc()
    hT = din(nc, "hT", [D, T], BF16)
    W = din(nc, "W", [D, NW])
    idn = din(nc, "ident", [128, 128], BF16)
    Bt = din(nc, "Bt", [nheads, 128, 8 * 4 * 64])
    yT = dout(nc, "yT", [nheads * 128, LT * 128], BF16)
    Pm = dscr(nc, "Pm", [T, NW])
    with ExitStack() as st:
        k = KB(nc, st)
        k.begin_phase()
        emit_linear_tm(k, hT, W, Pm, T, NW, "D:Pm")
        k.end_phase()
        k.begin_phase()
        ident = k.sb("ident_sb", [128, 128], BF16)
        k.dma(ident[:], idn, W=["ident"])
        ones = k.sb("ones", [128, 128], BF16)
        k.op("dve", "memset", ap=ones[:], constant=1.0, W=["ones"])
        stg = k.sb("stg", [128, NTt, 128])
        xb = k.sb("xb", [128, NTt, 128], BF16)
        QT = k.sb("QT", [128, T], BF16)
        KT = k.sb("KT", [128, T], BF16)
        Va = k.sb("Va", [128, NTt, 128], BF16)
        Vb = k.sb("Vb", [128, NTt, 128], BF16)
        Mt = k.sb("Mt", [128, 8, 256])
        E = [k.sb(f"E{i}", [128, 64 * (4 + CT)], BF16) for i in range(3)]
        rc = k.sb("rc", [128, 512])
        yo = [k.sb(f"yo{i}", [128, 512], BF16) for i in range(2)]
        pst = [k.ps(f"pst{i}", [128, 1024], BF16) for i in range(2)]
        pS = [k.ps(f"pS{i}", [128, 512]) for i in range(2)]
        pO = [k.ps(f"pO{i}", [128, 512]) for i in range(2)]
        pD = [k.ps(f"pD{i}", [128, 512]) for i in range(2)]
        npst = [0]

        def transpose_all(src, dst, src_key, dst_key):
            for c0 in range(0, NTt, 8):
                cn = min(8, NTt - c0)
                i = npst[0] % 2
                npst[0] += 1
                for c in range(cn):
                    k.op("pe", "transpose", out=pst[i][:, c * 128:(c + 1) * 128], in_=src[:, c0 + c, :],
                         identity=ident[:], R=[src_key, "ident"], W=[f"pst{i}"], inc=(c == cn - 1))
                dv = dst[:, c0 * 128:(c0 + cn) * 128]
                if (c0 // 8) % 2 == 0:
                    k.op("act", "copy", out=dv, in_=pst[i][:, :cn * 128], R=[f"pst{i}"], W=[dst_key])
                else:
                    k.op("dve", "tensor_copy", out=dv, in_=pst[i][:, :cn * 128], R=[f"pst{i}"], W=[dst_key])

        nE = nS = nG = 0
        NA_ = 4 + CT
        for h in range(nheads):
            for (col, dst, dkey) in ((h * 128, QT, "QT"), ((nheads + h) * 128, KT, "KT")):
                k.dma(stg[:], Pm[:, col:col + 128].rearrange("(n p) c -> p n c", p=128), R=["D:Pm"], W=["stg"])
                k.op("act", "copy", out=xb[:], in_=stg[:], R=["stg"], W=["xb"])
                transpose_all(xb, dst, "xb", dkey)
            vcol = (2 * nheads + h) * 128
            k.dma(stg[:], Pm[:, vcol:vcol + 128].rearrange("(n p) c -> p n c", p=128), R=["D:Pm"], W=["stg"])
            k.op("act", "copy", out=Va[:], in_=stg[:], R=["stg"], W=["Va"])
            k.dma(stg[:, 0:NTt - 1, :], Pm[64:64 + (NTt - 1) * 128, vcol:vcol + 128].rearrange("(n p) c -> p n c", p=128),
                  R=["D:Pm"], W=["stg"])
            k.op("dve", "tensor_copy", out=Vb[:, 0:NTt - 1, :], in_=stg[:, 0:NTt - 1, :], R=["stg"], W=["Vb"])
            k.dma(Mt[:].rearrange("p d c -> p (d c)"), Bt[h], W=["Mt"])
            k.op("act", "activation", out=Mt[:], in_=Mt[:], func=AF.Exp, R=["Mt"], W=["Mt"])
            for r in range(rows):
                r0 = min(max(r - 4, 0), rows - 8)
                di = r - r0
                g8, rg = r // 8, r % 8
                ig = nG % 2
                iS = nS % 2
                nS += 1
                iE = nE % 3
                nE += 1
                qs = slice(L0 + 64 * r, L0 + 64 * r + 64)
                for a in range(NA_):
                    ks = (L0 + 64 * r0 + 128 * a) if a < 4 else 128 * (a - 4)
                    k.op("pe", "matmul", out=pS[iS][:, a * 64:(a + 1) * 64], lhsT=KT[:, ks:ks + 128], rhs=QT[:, qs],
                         start=True, stop=True, R=["KT", "QT"], W=[f"pS{iS}"], inc=(a == NA_ - 1))
                k.op("act", "activation", out=E[iE][:], in_=pS[iS][:, 0:64 * NA_], func=AF.Exp, scale=ATT_SCALE,
                     R=[f"pS{iS}"], W=[f"E{iE}"])
                k.op("dve", "tensor_tensor", out=E[iE][:, 0:256], in0=E[iE][:, 0:256], in1=Mt[:, di, :], op=ALU.mult,
                     R=[f"E{iE}", "Mt"], W=[f"E{iE}"])
                for a in range(NA_):
                    if a < 4:
                        t64 = CT * 2 + r0 + 2 * a
                        vt = Va[:, t64 // 2, :] if t64 % 2 == 0 else Vb[:, (t64 - 1) // 2, :]
                        vkey = "Va" if t64 % 2 == 0 else "Vb"
                    else:
                        vt, vkey = Va[:, a - 4, :], "Va"
                    last = (a == NA_ - 1)
                    k.op("pe", "matmul", out=pO[ig][:, rg * 64:(rg + 1) * 64], lhsT=vt, rhs=E[iE][:, a * 64:(a + 1) * 64],
                         start=(a == 0), stop=last, R=[vkey, f"E{iE}"], W=[f"pO{ig}"], inc=last)
                    k.op("pe", "matmul", out=pD[ig][:, rg * 64:(rg + 1) * 64], lhsT=ones[:], rhs=E[iE][:, a * 64:(a + 1) * 64],
                         start=(a == 0), stop=last, R=["ones", f"E{iE}"], W=[f"pD{ig}"], inc=last)
                if rg == 7:
                    nG += 1
                    k.op("dve", "reciprocal", out=rc[:], in_=pD[ig][:], R=[f"pD{ig}"], W=["rc"])
                    k.op("dve", "tensor_tensor", out=yo[ig][:], in0=pO[ig][:], in1=rc[:], op=ALU.mult,
                         R=[f"pO{ig}", "rc"], W=[f"yo{ig}"])
                    k.dma(yT[h * 128:(h + 1) * 128, g8 * 512:(g8 + 1) * 512], yo[ig][:], R=[f"yo{ig}"], W=["D:yT"],
                          is_output=True)
        k.end_phase()
    return nc


NEG_INF = -1.0e30


def build_D(NT=9, ctx_tiles=1, final=False, stop_after=9, neg=64, cut=9, part="all"):
    R = NT * 128
    if part == "a":
        stop_after = 5
    nc = new_nc()
    x = din(nc, "x", [R, D])
    yT = din(nc, "yT", [D, R], BF16)
    wout = din(nc, "wout", [D, D])
    mv = din(nc, "mv", [10, D])
    wq = din(nc, "wq", [D, 2048])
    skT = din(nc, "skT", [128, 2, 128])
    if stop_after >= 6:
        UTb = din(nc, "UTb", [neg, 128, 32, 256], BF16)
        Vb = din(nc, "Vb", [neg * 256, D], BF16)
    idn = din(nc, "ident", [128, 128], BF16)
    xo = dout(nc, "xo", [R, D]) if part != "a" else None
    if part == "a":
        stop_after = 5
        x1 = dout(nc, "x1", [R, D])
        h2T = dout(nc, "h2T", [D, R], BF16)
        G = dout(nc, "G", [R, 16384], BF16)
    else:
        x1 = dscr(nc, "x1", [R, D])
        h2T = dscr(nc, "h2T", [D, R], BF16)
        G = dscr(nc, "G", [R, 16384], BF16)
    S = dscr(nc, "S", [R, 2048])
    x2 = dscr(nc, "x2", [R, D]) if final else xo
    typ = lambda t: 0 if t < ctx_tiles else 1
    with ExitStack() as st:
        k = KB(nc, st)

        def load_wblock(wb, stg, Wd, c0, nw, ns):
            for q in range(4):
                j = ns[0] % 2
                ns[0] += 1
                k.dma(stg[j][:, :, :nw], Wd[q * 1024:(q + 1) * 1024, c0:c0 + nw].rearrange("(a p) n -> p a n", p=128),
                      W=[f"stg{j}"])
                if q % 2 == 0:
                    k.op("pool", "tensor_copy", out=wb[:, q * 8:(q + 1) * 8, :nw], in_=stg[j][:, :, :nw],
                         R=[f"stg{j}"], W=["wb"])
                else:
                    k.op("act", "copy", out=wb[:, q * 8:(q + 1) * 8, :nw], in_=stg[j][:, :, :nw],
                         R=[f"stg{j}"], W=["wb"])

        k.begin_phase()
        yTs = k.sb("yTs", [128, 32, R], BF16)
        k.dma(yTs[:], yT.rearrange("(a p) t -> p a t", p=128), W=["yTs"])
        wb = k.sb("wb", [128, 32, 512], BF16)
        stg = [k.sb(f"stg{i}", [128, 8, 512]) for i in range(2)]
        g1s = k.sb("g1s", [128, 2, 512])
        xs = [k.sb(f"xs{i}", [128, 512]) for i in range(3)]
        tmp = [k.sb(f"tmp{i}", [128, 512]) for i in range(3)]
        ps = [k.ps(f"ps{i}", [128, 512]) for i in range(3)]
        ns = [0]
        n = 0
        for nb in range(8):
            c0 = nb * 512
            load_wblock(wb, stg, wout, c0, 512, ns)
            for ty in range(2):
                k.dma(g1s[:, ty, :], mv[ty:ty + 1, c0:c0 + 512].partition_broadcast(128), W=["g1s"])
            for t in range(NT):
                i = n % 3
                n += 1
                for kk in range(32):
                    k.op("pe", "matmul", out=ps[i][:], lhsT=yTs[:, kk, t * 128:(t + 1) * 128], rhs=wb[:, kk, :],
                         start=(kk == 0), stop=(kk == 31), R=["yTs", "wb"], W=[f"ps{i}"], inc=(kk == 31))
                k.dma(xs[i][:], x[t * 128:(t + 1) * 128, c0:c0 + 512], W=[f"xs{i}"])
                k.op("dve", "tensor_tensor", out=tmp[i][:], in0=ps[i][:], in1=g1s[:, typ(t), :], op=ALU.mult,
                     R=[f"ps{i}", "g1s"], W=[f"tmp{i}"])
                k.op("pool", "tensor_tensor", out=tmp[i][:], in0=tmp[i][:], in1=xs[i][:], op=ALU.add,
                     R=[f"tmp{i}", f"xs{i}"], W=[f"tmp{i}"])
                k.dma(x1[t * 128:(t + 1) * 128, c0:c0 + 512], tmp[i][:], R=[f"tmp{i}"], W=["D:x1"])
        k.end_phase()
        if stop_after < 2:
            return nc
        k.begin_phase()
        ident = k.sb("ident_sb", [128, 128], BF16)
        k.dma(ident[:], idn, W=["ident"])
        mods = emit_mods(k, mv[2:7, :])
        ntm = NormT(k, ident[:])
        hts = [k.sb(f"hT{i}", [128, 32, 128], BF16) for i in range(2)]
        for t in range(NT):
            sc, sh = mods[typ(t)]
            j = t % 2
            ntm.tile(x1[t * 128:(t + 1) * 128, :], ["D:x1"], sc[:], sh[:], "mods", hts[j], f"hT{j}")
            k.dma(h2T[:, t * 128:(t + 1) * 128].rearrange("(a p) t -> p a t", p=128), hts[j][:],
                  R=[f"hT{j}"], W=["D:h2T"])
        k.end_phase()
        if stop_after < 3:
            return nc
        k.begin_phase()
        h2s = k.sb("h2s", [128, 32, R], BF16)
        k.dma(h2s[:], h2T.rearrange("(a p) t -> p a t", p=128), R=["D:h2T"], W=["h2s"])
        sk = k.sb("sk", [128, 2, 128])
        k.dma(sk[:], skT, W=["sk"])
        wb = k.sb("wb", [128, 32, 512], BF16)
        stg = [k.sb(f"stg{i}", [128, 8, 512]) for i in range(2)]
        qn = k.sb("qn", [128, 4, R])
        so = [k.sb(f"so{i}", [128, 512]) for i in range(2)]
        ps = [k.ps(f"ps{i}", [128, 512]) for i in range(3)]
        p2 = [k.ps(f"p2{i}", [128, 512]) for i in range(2)]
        n = n2 = 0
        for nb in range(4):
            load_wblock(wb, stg, wq, nb * 512, 512, ns)
            for j in range(4):
                for t0 in range(0, R, 512):
                    tw = min(512, R - t0)
                    i = n % 3
                    n += 1
                    for kk in range(32):
                        k.op("pe", "matmul", out=ps[i][:, :tw], lhsT=wb[:, kk, j * 128:(j + 1) * 128],
                             rhs=h2s[:, kk, t0:t0 + tw], start=(kk == 0), stop=(kk == 31), R=["wb", "h2s"],
                             W=[f"ps{i}"], inc=(kk == 31))
                    if n % 2 == 0:
                        k.op("act", "copy", out=qn[:, j, t0:t0 + tw], in_=ps[i][:, :tw], R=[f"ps{i}"], W=["qn"])
                    else:
                        k.op("dve", "tensor_copy", out=qn[:, j, t0:t0 + tw], in_=ps[i][:, :tw], R=[f"ps{i}"], W=["qn"])
            for t in range(NT):
                i = n2 % 2
                n2 += 1
                for j in range(4):
                    k.op("pe", "matmul", out=p2[i][:, j * 128:(j + 1) * 128], lhsT=qn[:, j, t * 128:(t + 1) * 128],
                         rhs=sk[:, j % 2, :], start=True, stop=True, R=["qn", "sk"], W=[f"p2{i}"], inc=(j == 3))
                k.op("dve", "tensor_copy", out=so[i][:], in_=p2[i][:], R=[f"p2{i}"], W=[f"so{i}"])
                k.dma(S[t * 128:(t + 1) * 128, nb * 512:(nb + 1) * 512], so[i][:], R=[f"so{i}"], W=["D:S"])
        k.end_phase()
        if stop_after < 4:
            return nc
        k.begin_phase()
        St = [k.sb(f"St{i}", [128, 16, 128]) for i in range(2)]
        V16 = k.sb("V16", [128, 16, 16])
        scr = k.sb("scr", [128, 256])
        cand = k.sb("cand", [128, 8, 256])
        T16 = k.sb("T16", [128, 8, 16])
        E16 = k.sb("E16", [128, 8, 16])
        zz = k.sb("zz", [128, 16])
        Dm = [k.sb(f"Dm{i}", [128, 32, 128]) for i in range(2)]
        Eg = [k.sb(f"Eg{i}", [128, 32, 128]) for i in range(2)]
        Ff = [k.sb(f"Ff{i}", [128, 32, 128]) for i in range(2)]
        Gq = k.sb("Gq", [128, 32, 128])
        Gb = [k.sb(f"Gb{i}", [128, 4096], BF16) for i in range(2)]
        nd = ngb = 0
        for t in range(NT):
            s_ = St[t % 2]
            sk_ = f"St{t % 2}"
            k.dma(s_[:], S[t * 128:(t + 1) * 128, :].rearrange("p (b c) -> p b c", b=16), R=["D:S"], W=[sk_])
            for b in range(16):
                k.op("dve", "max", out=V16[:, b, 0:8], in_=s_[:, b, :], R=[sk_], W=["V16"])
                k.op("dve", "match_replace", out=scr[:, 0:128], in_to_replace=V16[:, b, 0:8], in_values=s_[:, b, :],
                     imm_value=NEG_INF, R=[sk_, "V16"], W=["scr"])
                k.op("dve", "max", out=V16[:, b, 8:16], in_=scr[:, 0:128], R=["scr"], W=["V16"])
            Vv = V16[:].rearrange("p (h two) r -> p h two r", two=2)
            k.op("dve", "tensor_tensor", out=cand[:].rearrange("p h (a b) -> p h a b", a=16),
                 in0=Vv[:, :, 0, :].unsqueeze(3).to_broadcast([128, 8, 16, 16]),
                 in1=Vv[:, :, 1, :].unsqueeze(2).to_broadcast([128, 8, 16, 16]), op=ALU.add, R=["V16"], W=["cand"])
            for h in range(8):
                k.op("dve", "max", out=T16[:, h, 0:8], in_=cand[:, h, :], R=["cand"], W=["T16"])
                k.op("dve", "match_replace", out=scr[:], in_to_replace=T16[:, h, 0:8], in_values=cand[:, h, :],
                     imm_value=NEG_INF, R=["cand", "T16"], W=["scr"])
                k.op("dve", "max", out=T16[:, h, 8:16], in_=scr[:], R=["scr"], W=["T16"])
            k.op("act", "activation", out=E16[:], in_=T16[:], func=AF.Exp, R=["T16"], W=["E16"])
            k.op("dve", "tensor_reduce", out=zz[:, 0:8], in_=E16[:], op=ALU.add, axis=AX.X, R=["E16"], W=["zz"])
            k.op("act", "activation", out=zz[:, 8:16], in_=zz[:, 0:8], func=AF.Ln, R=["zz"], W=["zz"])
            k.op("dve", "tensor_scalar", out=zz[:, 8:16], in0=zz[:, 8:16], scalar1=-1.0, scalar2=None, op0=ALU.mult,
                 R=["zz"], W=["zz"])
            Sv = s_[:].rearrange("p (h two) c -> p h two c", two=2)
            for qi in range(4):
                for h in range(8):
                    i = nd % 2
                    nd += 1
                    k.op("pool", "tensor_tensor", out=Dm[i][:],
                         in0=Sv[:, h, 0, qi * 32:(qi + 1) * 32].unsqueeze(2).to_broadcast([128, 32, 128]),
                         in1=Sv[:, h, 1, :].unsqueeze(1).to_broadcast([128, 32, 128]), op=ALU.add,
                         R=[sk_], W=[f"Dm{i}"])
                    k.op("act", "activation", out=Eg[i][:], in_=Dm[i][:], func=AF.Exp, bias=zz[:, 8 + h:9 + h],
                         R=[f"Dm{i}", "zz"], W=[f"Eg{i}"])
                    dst = Gq if h == 0 else Ff[i]
                    dkey = "Gq" if h == 0 else f"Ff{i}"
                    k.op("dve", "scalar_tensor_tensor", out=dst[:], in0=Dm[i][:], scalar=T16[:, h, 15:16],
                         in1=Eg[i][:], op0=ALU.is_ge, op1=ALU.mult, R=[f"Dm{i}", f"Eg{i}", "T16"], W=[dkey])
                    if h > 0:
                        k.op("pool", "tensor_tensor", out=Gq[:], in0=Gq[:], in1=Ff[i][:], op=ALU.add,
                             R=["Gq", f"Ff{i}"], W=["Gq"])
                ig = ngb % 2
                ngb += 1
                k.op("act", "copy", out=Gb[ig][:], in_=Gq[:].rearrange("p a b -> p (a b)"), R=["Gq"], W=[f"Gb{ig}"])
                k.dma(G[t * 128:(t + 1) * 128, qi * 4096:(qi + 1) * 4096], Gb[ig][:], R=[f"Gb{ig}"], W=["D:G"])
        k.end_phase()
        if stop_after < 6:
            return nc
        k.begin_phase()
        ident = k.sb("ident_sb", [128, 128], BF16)
        k.dma(ident[:], idn, W=["ident"])
        h2b = k.sb("h2b", [128, 32, 384], BF16)
        acc = k.sb("acc", [128, 3, D])
        UTs = [k.sb(f"UTs{i}", [128, 32, 256], BF16) for i in range(2)]
        Vs = [k.sb(f"Vs{i}", [128, 2, D], BF16) for i in range(2)]
        Gs = [k.sb(f"Gs{i}", [128, 3, 256], BF16) for i in range(2)]
        Wg = [k.sb(f"Wg{i}", [128, 256]) for i in range(2)]
        Wb = [k.sb(f"Wb{i}", [128, 256], BF16) for i in range(2)]
        WT = [k.sb(f"WT{i}", [128, 2, 128], BF16) for i in range(2)]
        xs = [k.sb(f"xs{i}", [128, 512]) for i in range(2)]
        g2s = [k.sb(f"g2s{i}", [128, 512]) for i in range(2)]
        tm = [k.sb(f"tm{i}", [128, 512]) for i in range(2)]
        pA = [k.ps(f"pA{i}", [128, 512]) for i in range(2)]
        pT = k.ps("pT", [128, 1024], BF16)
        po = [k.ps(f"po{i}", [128, 1024]) for i in range(2)]
        nA = npo = ne = 0
        for tb0 in range(0, NT, 3):
            tiles = list(range(tb0, min(tb0 + 3, NT)))
            nt_ = len(tiles)
            r0, r1 = tb0 * 128, (tb0 + nt_) * 128
            k.dma(h2b[:, :, :nt_ * 128], h2T[:, r0:r1].rearrange("(a p) t -> p a t", p=128), R=["D:h2T"], W=["h2b"])
            for eg in range(neg):
                j = eg % 2
                k.dma(UTs[j][:], UTb[eg], W=[f"UTs{j}"])
                k.dma(Vs[j][:], Vb[eg * 256:(eg + 1) * 256, :].rearrange("(c p) d -> p c d", p=128), W=[f"Vs{j}"])
                k.dma(Gs[j][:, :nt_, :], G[r0:r1, eg * 256:(eg + 1) * 256].rearrange("(n p) c -> p n c", p=128),
                      R=["D:G"], W=[f"Gs{j}"])
                for ti in range(nt_):
                    i = nA % 2
                    nA += 1
                    for kk in range(32):
                        k.op("pe", "matmul", out=pA[i][:, 0:256], lhsT=h2b[:, kk, ti * 128:(ti + 1) * 128],
                             rhs=UTs[j][:, kk, :], start=(kk == 0), stop=(kk == 31), R=["h2b", f"UTs{j}"],
                             W=[f"pA{i}"], inc=(kk == 31))
                    k.op("act", "activation", out=Wg[i][:], in_=pA[i][:, 0:256], func=AF.Gelu_apprx_tanh,
                         R=[f"pA{i}"], W=[f"Wg{i}"])
                    if cut < 2:
                        continue
                    k.op("pool", "tensor_tensor", out=Wb[i][:], in0=Wg[i][:], in1=Gs[j][:, ti, :], op=ALU.mult,
                         R=[f"Wg{i}", f"Gs{j}"], W=[f"Wb{i}"])
                    if cut < 3:
                        continue
                    for c in range(2):
                        k.op("pe", "transpose", out=pT[:, c * 128:(c + 1) * 128], in_=Wb[i][:, c * 128:(c + 1) * 128],
                             identity=ident[:], R=[f"Wb{i}", "ident"], W=["pT"], inc=(c == 1))
                    k.op("act", "copy", out=WT[i][:], in_=pT[:, 0:256].rearrange("p (c t) -> p c t", c=2),
                         R=["pT"], W=[f"WT{i}"])
                    if cut < 4:
                        continue
                    for ch in range(4):
                        ip = npo % 2
                        npo += 1
                        for db in range(2):
                            for c in range(2):
                                d0 = ch * 1024 + db * 512
                                k.op("pe", "matmul", out=po[ip][:, db * 512:(db + 1) * 512], lhsT=WT[i][:, c, :],
                                     rhs=Vs[j][:, c, d0:d0 + 512], start=(c == 0), stop=(c == 1),
                                     R=[f"WT{i}", f"Vs{j}"], W=[f"po{ip}"], inc=(db == 1 and c == 1))
                        a_ = acc[:, ti, ch * 1024:(ch + 1) * 1024]
                        if cut < 5:
                            continue
                        if eg == 0:
                            k.op("dve", "tensor_copy", out=a_, in_=po[ip][:], R=[f"po{ip}"], W=["acc"])
                        else:
                            k.op("dve", "tensor_tensor", out=a_, in0=po[ip][:], in1=a_, op=ALU.add,
                                 R=[f"po{ip}", "acc"], W=["acc"])
            for ti, t in enumerate(tiles):
                for db in range(8):
                    i = ne % 2
                    ne += 1
                    c0 = db * 512
                    k.dma(xs[i][:], x1[t * 128:(t + 1) * 128, c0:c0 + 512], R=["D:x1"], W=[f"xs{i}"])
                    k.dma(g2s[i][:], mv[7 + typ(t):8 + typ(t), c0:c0 + 512].partition_broadcast(128), W=[f"g2s{i}"])
                    k.op("dve", "tensor_tensor", out=tm[i][:], in0=acc[:, ti, c0:c0 + 512], in1=g2s[i][:], op=ALU.mult,
                         R=["acc", f"g2s{i}"], W=[f"tm{i}"])
                    k.op("pool", "tensor_tensor", out=tm[i][:], in0=tm[i][:], in1=xs[i][:], op=ALU.add,
                         R=[f"tm{i}", f"xs{i}"], W=[f"tm{i}"])
                    k.dma(x2[t * 128:(t + 1) * 128, c0:c0 + 512], tm[i][:], R=[f"tm{i}"], W=["D:x2"],
                          is_output=(not final))
        k.end_phase()
        if final:
            k.begin_phase()
            fg = k.sb("fg", [128, D])
            k.dma(fg[:], mv[9:10, :].partition_broadcast(128), W=["fg"])
            xt = [k.sb(f"xt{i}", [128, D]) for i in range(2)]
            jk = k.sb("jk", [128, D])
            ss = [k.sb(f"ss{i}", [128, 2]) for i in range(2)]
            for t in range(NT):
                i = t % 2
                k.dma(xt[i][:], x2[t * 128:(t + 1) * 128, :], R=["D:x2"], W=[f"xt{i}"])
                k.op("act", "activation", out=jk[:], in_=xt[i][:], func=AF.Square, accum_out=ss[i][:, 0:1],
                     R=[f"xt{i}"], W=["jk", f"ss{i}"])
                k.op("dve", "tensor_scalar", out=ss[i][:, 1:2], in0=ss[i][:, 0:1], scalar1=1.0 / D, scalar2=EPS,
                     op0=ALU.mult, op1=ALU.add, R=[f"ss{i}"], W=[f"ss{i}"])
                k.op("act", "activation", out=ss[i][:, 1:2], in_=ss[i][:, 1:2], func=AF.Sqrt, R=[f"ss{i}"], W=[f"ss{i}"])
                k.op("dve", "reciprocal", out=ss[i][:, 1:2], in_=ss[i][:, 1:2], R=[f"ss{i}"], W=[f"ss{i}"])
                k.op("dve", "scalar_tensor_tensor", out=xt[i][:], in0=xt[i][:], scalar=ss[i][:, 1:2], in1=fg[:],
                     op0=ALU.mult, op1=ALU.mult, R=[f"xt{i}", f"ss{i}", "fg"], W=[f"xt{i}"])
                k.dma(xo[t * 128:(t + 1) * 128, :], xt[i][:], R=[f"xt{i}"], W=["D:xo"], is_output=True)
            k.end_phase()
    return nc


def build_Db(NT=9, ctx_tiles=1, final=False, neg=16, first=True, last=True):
    R = NT * 128
    nc = new_nc()
    h2T = din(nc, "h2T", [D, R], BF16)
    G = din(nc, "G", [R, neg * 256], BF16)
    UTb = din(nc, "UTb", [neg, 128, 32, 256], BF16)
    Vb = din(nc, "Vb", [neg * 256, D], BF16)
    idn = din(nc, "ident", [128, 128], BF16)
    acc_in = None if first else din(nc, "acc_in", [R, D])
    if last:
        x1 = din(nc, "x1", [R, D])
        mv = din(nc, "mv", [10, D])
        xo = dout(nc, "xo", [R, D])
        x2 = dscr(nc, "x2", [R, D]) if final else xo
    else:
        acc_out = dout(nc, "acc_out", [R, D])
    typ = lambda t: 0 if t < ctx_tiles else 1
    with ExitStack() as st:
        k = KB(nc, st)
        k.begin_phase()
        ident = k.sb("ident_sb", [128, 128], BF16)
        k.dma(ident[:], idn, W=["ident"])
        h2b = k.sb("h2b", [128, 32, 384], BF16)
        acc = k.sb("acc", [128, 3, D])
        UTs = [k.sb(f"UTs{i}", [128, 32, 256], BF16) for i in range(2)]
        Vs = [k.sb(f"Vs{i}", [128, 2, D], BF16) for i in range(2)]
        Gs = [k.sb(f"Gs{i}", [128, 3, 256], BF16) for i in range(2)]
        Wg = [k.sb(f"Wg{i}", [128, 256]) for i in range(2)]
        Wb = [k.sb(f"Wb{i}", [128, 256], BF16) for i in range(2)]
        WT = [k.sb(f"WT{i}", [128, 2, 128], BF16) for i in range(2)]
        xs = [k.sb(f"xs{i}", [128, 512]) for i in range(2)]
        g2s = [k.sb(f"g2s{i}", [128, 512]) for i in range(2)]
        tm = [k.sb(f"tm{i}", [128, 512]) for i in range(2)]
        pA = [k.ps(f"pA{i}", [128, 512]) for i in range(2)]
        pT = k.ps("pT", [128, 1024], BF16)
        po = [k.ps(f"po{i}", [128, 1024]) for i in range(2)]
        nA = npo = ne = 0
        for tb0 in range(0, NT, 3):
            tiles = list(range(tb0, min(tb0 + 3, NT)))
            nt_ = len(tiles)
            r0, r1 = tb0 * 128, (tb0 + nt_) * 128
            k.dma(h2b[:, :, :nt_ * 128], h2T[:, r0:r1].rearrange("(a p) t -> p a t", p=128), W=["h2b"])
            if not first:
                k.dma(acc[:, :nt_, :], acc_in[r0:r1, :].rearrange("(n p) d -> p n d", p=128), W=["acc"])
            for eg in range(neg):
                j = eg % 2
                k.dma(UTs[j][:], UTb[eg], W=[f"UTs{j}"])
                k.dma(Vs[j][:], Vb[eg * 256:(eg + 1) * 256, :].rearrange("(c p) d -> p c d", p=128), W=[f"Vs{j}"])
                k.dma(Gs[j][:, :nt_, :], G[r0:r1, eg * 256:(eg + 1) * 256].rearrange("(n p) c -> p n c", p=128),
                      W=[f"Gs{j}"])
                for ti in range(nt_):
                    i = nA % 2
                    nA += 1
                    for kk in range(32):
                        k.op("pe", "matmul", out=pA[i][:, 0:256], lhsT=h2b[:, kk, ti * 128:(ti + 1) * 128],
                             rhs=UTs[j][:, kk, :], start=(kk == 0), stop=(kk == 31), R=["h2b", f"UTs{j}"],
                             W=[f"pA{i}"], inc=(kk == 31))
                    k.op("act", "activation", out=Wg[i][:], in_=pA[i][:, 0:256], func=AF.Gelu_apprx_tanh,
                         R=[f"pA{i}"], W=[f"Wg{i}"])
                    k.op("pool", "tensor_tensor", out=Wb[i][:], in0=Wg[i][:], in1=Gs[j][:, ti, :], op=ALU.mult,
                         R=[f"Wg{i}", f"Gs{j}"], W=[f"Wb{i}"])
                    for c in range(2):
                        k.op("pe", "transpose", out=pT[:, c * 128:(c + 1) * 128], in_=Wb[i][:, c * 128:(c + 1) * 128],
                             identity=ident[:], R=[f"Wb{i}", "ident"], W=["pT"], inc=(c == 1))
                    k.op("act", "copy", out=WT[i][:], in_=pT[:, 0:256].rearrange("p (c t) -> p c t", c=2),
                         R=["pT"], W=[f"WT{i}"])
                    for ch in range(4):
                        ip = npo % 2
                        npo += 1
                        for db in range(2):
                            for c in range(2):
                                d0 = ch * 1024 + db * 512
                                k.op("pe", "matmul", out=po[ip][:, db * 512:(db + 1) * 512], lhsT=WT[i][:, c, :],
                                     rhs=Vs[j][:, c, d0:d0 + 512], start=(c == 0), stop=(c == 1),
                                     R=[f"WT{i}", f"Vs{j}"], W=[f"po{ip}"], inc=(db == 1 and c == 1))
                        a_ = acc[:, ti, ch * 1024:(ch + 1) * 1024]
                        if eg == 0 and first:
                            k.op("dve", "tensor_copy", out=a_, in_=po[ip][:], R=[f"po{ip}"], W=["acc"])
                        else:
                            k.op("dve", "tensor_tensor", out=a_, in0=po[ip][:], in1=a_, op=ALU.add,
                                 R=[f"po{ip}", "acc"], W=["acc"])
            if not last:
                k.dma(acc_out[r0:r1, :].rearrange("(n p) d -> p n d", p=128), acc[:, :nt_, :], R=["acc"],
                      W=["D:acc_out"], is_output=True)
                continue
            for ti, t in enumerate(tiles):
                for db in range(8):
                    i = ne % 2
                    ne += 1
                    c0 = db * 512
                    k.dma(xs[i][:], x1[t * 128:(t + 1) * 128, c0:c0 + 512], W=[f"xs{i}"])
                    k.dma(g2s[i][:], mv[7 + typ(t):8 + typ(t), c0:c0 + 512].partition_broadcast(128), W=[f"g2s{i}"])
                    k.op("dve", "tensor_tensor", out=tm[i][:], in0=acc[:, ti, c0:c0 + 512], in1=g2s[i][:], op=ALU.mult,
                         R=["acc", f"g2s{i}"], W=[f"tm{i}"])
                    k.op("pool", "tensor_tensor", out=tm[i][:], in0=tm[i][:], in1=xs[i][:], op=ALU.add,
                         R=[f"tm{i}", f"xs{i}"], W=[f"tm{i}"])
                    k.dma(x2[t * 128:(t + 1) * 128, c0:c0 + 512], tm[i][:], R=[f"tm{i}"], W=["D:x2"],
                          is_output=(not final))
        k.end_phase()
        if last and final:
            k.begin_phase()
            fg = k.sb("fg", [128, D])
            k.dma(fg[:], mv[9:10, :].partition_broadcast(128), W=["fg"])
            xt = [k.sb(f"xt{i}", [128, D]) for i in range(2)]
            jk = k.sb("jk", [128, D])
            ss = [k.sb(f"ss{i}", [128, 2]) for i in range(2)]
            for t in range(NT):
                i = t % 2
                k.dma(xt[i][:], x2[t * 128:(t + 1) * 128, :], R=["D:x2"], W=[f"xt{i}"])
                k.op("act", "activation", out=jk[:], in_=xt[i][:], func=AF.Square, accum_out=ss[i][:, 0:1],
                     R=[f"xt{i}"], W=["jk", f"ss{i}"])
                k.op("dve", "tensor_scalar", out=ss[i][:, 1:2], in0=ss[i][:, 0:1], scalar1=1.0 / D, scalar2=EPS,
                     op0=ALU.mult, op1=ALU.add, R=[f"ss{i}"], W=[f"ss{i}"])
                k.op("act", "activation", out=ss[i][:, 1:2], in_=ss[i][:, 1:2], func=AF.Sqrt, R=[f"ss{i}"], W=[f"ss{i}"])
                k.op("dve", "reciprocal", out=ss[i][:, 1:2], in_=ss[i][:, 1:2], R=[f"ss{i}"], W=[f"ss{i}"])
                k.op("dve", "scalar_tensor_tensor", out=xt[i][:], in0=xt[i][:], scalar=ss[i][:, 1:2], in1=fg[:],
                     op0=ALU.mult, op1=ALU.mult, R=[f"xt{i}", f"ss{i}", "fg"], W=[f"xt{i}"])
                k.dma(xo[t * 128:(t + 1) * 128, :], xt[i][:], R=[f"xt{i}"], W=["D:xo"], is_output=True)
            k.end_phase()
    return nc


def build_E(NEG=8):
    nc = new_nc()
    U = din(nc, "U", [NEG * 256, D])
    V = din(nc, "V", [NEG * 256, D])
    idf = din(nc, "identf", [128, 128])
    UTb = dout(nc, "UTb", [NEG, 128, 32, 256], BF16)
    Vb = dout(nc, "Vb", [NEG * 256, D], BF16)
    with ExitStack() as st:
        k = KB(nc, st)
        k.begin_phase()
        ident = k.sb("identf_sb", [128, 128])
        k.dma(ident[:], idf, W=["ident"])
        ut = [k.sb(f"ut{i}", [128, D]) for i in range(3)]
        vb = [k.sb(f"vb{i}", [128, D], BF16) for i in range(2)]
        uo = [k.sb(f"uo{i}", [128, 32, 256], BF16) for i in range(2)]
        pt = [k.ps(f"pt{i}", [128, 512]) for i in range(4)]
        nu = nv = npt = 0
        for g in range(NEG):
            io = g % 2
            for c in range(2):
                iu = nu % 3
                nu += 1
                r0 = g * 256 + c * 128
                k.dma(ut[iu][:], U[r0:r0 + 128, :], W=[f"ut{iu}"])
                for k4 in range(8):
                    ip = npt % 4
                    npt += 1
                    for q in range(4):
                        kk = k4 * 4 + q
                        k.op("pe", "transpose", out=pt[ip][:, q * 128:(q + 1) * 128], in_=ut[iu][:, kk * 128:(kk + 1) * 128],
                             identity=ident[:], R=[f"ut{iu}", "ident"], W=[f"pt{ip}"], inc=(q == 3))
                    src = pt[ip][:].rearrange("p (a b) -> p a b", a=4)
                    dst = uo[io][:, k4 * 4:(k4 + 1) * 4, c * 128:(c + 1) * 128]
                    if k4 % 2 == 0:
                        k.op("act", "copy", out=dst, in_=src, R=[f"pt{ip}"], W=[f"uo{io}"])
                    else:
                        k.op("dve", "tensor_copy", out=dst, in_=src, R=[f"pt{ip}"], W=[f"uo{io}"])
                iu2 = nu % 3
                nu += 1
                iv = nv % 2
                nv += 1
                k.dma(ut[iu2][:], V[r0:r0 + 128, :], W=[f"ut{iu2}"])
                k.op("pool", "tensor_copy", out=vb[iv][:], in_=ut[iu2][:], R=[f"ut{iu2}"], W=[f"vb{iv}"])
                k.dma(Vb[r0:r0 + 128, :], vb[iv][:], R=[f"vb{iv}"], W=["D:Vb"], is_output=True)
            k.dma(UTb[g], uo[io][:], R=[f"uo{io}"], W=["D:UTb"], is_output=True)
        k.end_phase()
    return nc


def build_mod(NL=2, NC=3072):
    nc = bass.Bass("TRN2", target_bir_lowering=False)
    cT = nc.dram_tensor("cT", [128, 32, 3], F32, kind="ExternalInput").ap()
    w = nc.dram_tensor("w", [NL, 4096, NC], F32, kind="ExternalInput").ap()
    b = nc.dram_tensor("b", [NL, 1, NC], F32, kind="ExternalInput").ap()
    m = nc.dram_tensor("m", [NL, 3, NC], F32, kind="ExternalOutput").ap()
    with ExitStack() as st:
        k = KB(nc, st)
        c_sb = k.sb("c_sb", [128, 32, 3])
        s_sb = k.sb("s_sb", [128, 32, 3])
        wb = [k.sb(f"wb{i}", [128, 32, 512]) for i in range(2)]
        bias = k.sb("bias", [3, NL, NC])
        o_sb = [k.sb(f"o{i}", [3, 512]) for i in range(2)]
        ps = [k.ps(f"ps{i}", [128, 512]) for i in range(2)]
        k.dma(c_sb[:], cT, W=["c"])
        for l in range(NL):
            k.dma(bias[:, l, :], b[l].partition_broadcast(3), W=["bias"])
        k.op("act", "activation", out=s_sb[:], in_=c_sb[:], func=AF.Silu, R=["c"], W=["s"])
        it = 0
        for l in range(NL):
            for cb in range(NC // 512):
                j = it % 2
                k.dma(wb[j][:], w[l, :, cb * 512:(cb + 1) * 512].rearrange("(k p) n -> p k n", p=128),
                      W=[f"wb{j}"])
                for kk in range(32):
                    k.op("pe", "matmul", out=ps[j][0:3, :], lhsT=s_sb[:, kk, :], rhs=wb[j][:, kk, :],
                         start=(kk == 0), stop=(kk == 31),
                         R=["s", f"wb{j}"], W=[f"ps{j}"], inc=(kk == 31))
                k.op("dve", "tensor_tensor", out=o_sb[j][:], in0=ps[j][0:3, :],
                     in1=bias[:, l, cb * 512:(cb + 1) * 512], op=ALU.add,
                     R=[f"ps{j}", "bias"], W=[f"o{j}"])
                k.dma(m[l, :, cb * 512:(cb + 1) * 512], o_sb[j][:], R=[f"o{j}"], W=["m"], is_output=True)
                it += 1
        k.finish()
        print("instrs", k.ninstr)
    return nc


_PROGS = {}


def _prog(key, fn):
    if key not in _PROGS:
        _PROGS[key] = fn()
    return _PROGS[key]


def _run(nc, in_maps):
    return run_bass_kernel_spmd(nc, in_maps, core_ids=list(range(8))).results


def _c(a):
    return np.ascontiguousarray(a)


def kernel(x, c, ctx, c_ctx, mod_w, mod_b, norm_g, ev_w_in, ev_gate_b, ev_qk_g, ev_hnorm_g, ev_w_out,
           od_w_in, od_rpb, od_w_out, peer_wq, peer_subkeys, peer_u, peer_v, final_g):
    f32 = np.float32
    x, c, ctx, c_ctx = (np.asarray(a, f32) for a in (x, c, ctx, c_ctx))
    mod_w, mod_b, norm_g = (np.asarray(a, f32) for a in (mod_w, mod_b, norm_g))
    ev_w_in, ev_gate_b, ev_qk_g, ev_hnorm_g, ev_w_out = (np.asarray(a, f32) for a in
                                                         (ev_w_in, ev_gate_b, ev_qk_g, ev_hnorm_g, ev_w_out))
    od_w_in, od_rpb, od_w_out = (np.asarray(a, f32) for a in (od_w_in, od_rpb, od_w_out))
    peer_wq, peer_subkeys, peer_u, peer_v, final_g = (np.asarray(a, f32) for a in
                                                      (peer_wq, peer_subkeys, peer_u, peer_v, final_g))
    ident = np.eye(128, dtype=f32).astype(BF)
    identf = np.eye(128, dtype=f32)
    cst = np.zeros((128, 5, 128), f32)
    s_, t_ = np.meshgrid(np.arange(128), np.arange(128), indexing="ij")
    cst[:, 0, :] = (s_ <= t_)
    cst[:, 1, :] = (s_ >= t_)
    cst[:, 2, :] = 1.0
    tt = np.arange(4096)
    inv = (10000.0 ** (-np.arange(32, dtype=f32) / 32)).astype(f32)
    ang = np.concatenate([(tt // 64).astype(f32)[:, None] * inv, (tt % 64).astype(f32)[:, None] * inv], -1)
    rope = np.concatenate([np.cos(ang), np.sin(ang)], -1).astype(f32)

    cvec = np.stack([c[0], c[1], c_ctx])
    cT = _c(cvec.reshape(3, 32, 128).transpose(2, 1, 0))
    ncA = _prog("A", lambda: build_mod(2, 3072))
    rA = _run(ncA, [{"cT": cT, "w": _c(mod_w[:, :, i * 3072:(i + 1) * 3072]),
                     "b": _c(mod_b[:, None, i * 3072:(i + 1) * 3072])} for i in range(8)])
    mod = np.concatenate([r["m"] for r in rA], axis=-1).reshape(2, 3, 6, 4096)

    ncE = _prog("E", lambda: build_E(8))
    UTb, Vbf = [], []
    for l in range(2):
        rE = _run(ncE, [{"U": _c(peer_u[l, i * 2048:(i + 1) * 2048]), "V": _c(peer_v[l, i * 2048:(i + 1) * 2048]),
                         "identf": identf} for i in range(8)])
        UTb.append(np.concatenate([r["UTb"] for r in rE], axis=0))
        Vbf.append(np.concatenate([r["Vb"] for r in rE], axis=0))

    xl = x
    xc = ctx
    zpad = np.zeros((64, 4096), f32)
    out = np.empty((2, 4096, 4096), f32)
    for layer in range(2):
        last = layer == 1
        sh1, sc1, g1, sh2, sc2, g2 = (mod[layer, :, i] for i in range(6))
        ncB = _prog("B", lambda: build_B(9))
        rows = {}
        maps = []
        for b in range(2):
            for q in range(4):
                r_ = np.concatenate([xc[b, 64 * q:64 * q + 64], zpad, xl[b, 1024 * q:1024 * (q + 1)]], axis=0)
                rows[(b, q)] = r_
                mv5 = np.stack([norm_g[layer, 0], sc1[2], sh1[2], sc1[b], sh1[b]])
                maps.append({"x": r_, "mv": mv5, "ident": ident})
        rB = _run(ncB, maps)
        hTb = []
        for b in range(2):
            parts = [rB[b * 4 + q]["hT"][:, 0:64] for q in range(4)] + [rB[b * 4 + q]["hT"][:, 128:] for q in range(4)]
            hTb.append(_c(np.concatenate(parts, axis=1)))
        if layer == 0:
            ncC = _prog("C0", lambda: build_C0(2, 32))
            w = ev_w_in[0]
            maps = []
            for b in range(2):
                for g in range(4):
                    gcols = [9216 + o + 2 * g + hh for o in (0, 8, 16, 24) for hh in (0, 1)]
                    Wc = np.concatenate([w[:, 512 * g:512 * g + 512], w[:, 2048 + 128 * g:2048 + 128 * g + 128],
                                         w[:, 2560 + 128 * g:2560 + 128 * g + 128],
                                         w[:, 3072 + 256 * g:3072 + 256 * g + 256],
                                         w[:, 4096 + 256 * g:4096 + 256 * g + 256],
                                         w[:, 5120 + 512 * g:5120 + 512 * g + 512],
                                         w[:, 7168 + 512 * g:7168 + 512 * g + 512], w[:, gcols]], axis=1)
                    gbv = ev_gate_b[0][[o + 2 * g + hh for o in (0, 8, 16, 24) for hh in (0, 1)]][None]
                    maps.append({"hT": hTb[b], "W": _c(Wc), "cst": cst, "ident": ident, "qkg": _c(ev_qk_g[0]),
                                 "gb": _c(gbv), "hng": _c(ev_hnorm_g[0].reshape(8, 256)[2 * g:2 * g + 2]),
                                 "rope": rope})
            rC = _run(ncC, maps)
            yTb = []
            for b in range(2):
                y = np.empty((4096, 4352), BF)
                for g in range(4):
                    p = rC[b * 4 + g]["yT"]
                    y[512 * g:512 * g + 512] = p[0:512]
                    y[2048 + 512 * g:2048 + 512 * g + 512] = p[512:1024]
                yTb.append(y)
        else:
            ncC = _prog("C1", lambda: build_C1(2, 32, 8))
            w = od_w_in[0]
            maps = []
            for b in range(2):
                for g in range(4):
                    Wc = np.concatenate([w[:, 1024 * g:1024 * (g + 1)], w[:, 4096 + 1024 * g:4096 + 1024 * (g + 1)],
                                         w[:, 8192 + 1024 * g:8192 + 1024 * (g + 1)]], axis=1)
                    Bt = np.stack([na_bias_table(od_rpb[0][8 * g + h], 64).reshape(128, -1) for h in range(8)])
                    maps.append({"hT": hTb[b], "W": _c(Wc), "ident": ident, "Bt": _c(Bt)})
            rC = _run(ncC, maps)
            yTb = []
            for b in range(2):
                y = np.empty((4096, 4352), BF)
                y[:, :256] = 0
                for g in range(4):
                    y[1024 * g:1024 * (g + 1), 256:] = rC[b * 4 + g]["yT"]
                yTb.append(y)
        NT = 8 if last else 9
        CTXT = 0 if last else 1
        w_out = (od_w_out if last else ev_w_out)[0]
        skT = _c(peer_subkeys[layer].transpose(2, 0, 1))
        ncDa = _prog(("Da", NT), lambda: build_D(NT, CTXT, part="a"))
        maps = []
        mvs = {}
        for b in range(2):
            for q in range(4):
                lat = yTb[b][:, 256 + 1024 * q:256 + 1024 * (q + 1)]
                if last:
                    xr = rows[(b, q)][128:]
                    yc = _c(lat)
                else:
                    xr = rows[(b, q)]
                    yc = _c(np.concatenate([yTb[b][:, 64 * q:64 * q + 64], np.zeros((4096, 64), BF), lat], axis=1))
                mv10 = np.stack([g1[2], g1[b], norm_g[layer, 1], sc2[2], sh2[2], sc2[b], sh2[b], g2[2], g2[b], final_g])
                mvs[(b, q)] = mv10
                maps.append({"x": _c(xr), "yT": yc, "wout": w_out, "mv": mv10, "wq": peer_wq[layer], "skT": skT,
                             "ident": ident})
        rDa = _run(ncDa, maps)
        acc = [None] * 8
        for ch in range(4):
            first, lastc = ch == 0, ch == 3
            ncDb = _prog(("Db", NT, first, lastc), lambda: build_Db(NT, CTXT, last, 16, first, lastc))
            maps = []
            for ci in range(8):
                m = {"h2T": rDa[ci]["h2T"], "G": _c(rDa[ci]["G"][:, ch * 4096:(ch + 1) * 4096]),
                     "UTb": _c(UTb[layer][ch * 16:(ch + 1) * 16]), "Vb": _c(Vbf[layer][ch * 4096:(ch + 1) * 4096]),
                     "ident": ident}
                if not first:
                    m["acc_in"] = acc[ci]
                if lastc:
                    m["x1"] = rDa[ci]["x1"]
                    m["mv"] = mvs[(ci // 4, ci % 4)]
                maps.append(m)
            rDb = _run(ncDb, maps)
            if not lastc:
                acc = [r["acc_out"] for r in rDb]
        if last:
            for b in range(2):
                for q in range(4):
                    out[b, 1024 * q:1024 * (q + 1)] = rDb[b * 4 + q]["xo"]
        else:
            xl = np.empty_like(x)
            xc = np.empty_like(ctx)
            for b in range(2):
                for q in range(4):
                    r_ = rDb[b * 4 + q]["xo"]
                    xc[b, 64 * q:64 * q + 64] = r_[0:64]
                    xl[b, 1024 * q:1024 * (q + 1)] = r_[128:]
    return out
```

```python
import numpy as np
import concourse.bass as bass
import concourse.mybir as mybir

F32 = mybir.dt.float32
BF16 = mybir.dt.bfloat16
AF = mybir.ActivationFunctionType
ALU = mybir.AluOpType
AX = mybir.AxisListType

SEM_LIMIT = 30000
import os
N_DMA_SEMS = int(os.environ.get("NDS", "48"))


class KB:
    def __init__(self, nc, stack):
        self.nc = nc
        self.stack = stack
        self.sem_stack = stack
        self.phase_stack = None
        self.eng = {"pe": nc.tensor, "dve": nc.vector, "act": nc.scalar,
                    "pool": nc.gpsimd, "sp": nc.sync}
        self.cnt = {}
        self.semkey = {}
        self.sems = {}
        self.nsem = 0
        for e in ("pe", "dve", "act", "pool"):
            self._new_epoch(e, 0)
        self.epoch = {e: 0 for e in ("pe", "dve", "act", "pool")}
        self.known = {e: {} for e in self.eng}
        self.last_w = {}
        self.readers = {}
        self.dma_sems = []
        for i in range(N_DMA_SEMS):
            s = self.sem_stack.enter_context(nc.semaphore(f"dq{i}"))
            key = ("dma", i)
            self.sems[key] = s
            self.dma_sems.append(key)
        self.dma_uses = {k: 0 for k in self.dma_sems}
        self.dma_rr = 0
        self.ninstr = 0
        self.out_tokens = []
        self.prog = {e: [] for e in self.eng}
        self.psum_keys = set()

    def _new_epoch(self, e, ep):
        key = (e, ep)
        s = self.sem_stack.enter_context(self.nc.semaphore(f"c_{e}_{ep}"))
        self.sems[key] = s
        self.semkey[e] = key
        self.cnt[e] = 0

    def _deps(self, R, W):
        deps = set()
        for b in R:
            t = self.last_w.get(b)
            if t is not None:
                deps.add(t)
        for b in W:
            t = self.last_w.get(b)
            if t is not None:
                deps.add(t)
            for t in self.readers.get(b, ()):
                deps.add(t)
        return deps

    def _record(self, tok, R, W):
        for b in R:
            self.readers.setdefault(b, []).append(tok)
        for b in W:
            self.last_w[b] = tok
            self.readers[b] = []

    def _emit_waits(self, x, deps):
        kn = self.known[x]
        best = {}
        for (key, val) in deps:
            if kn.get(key, 0) >= val:
                continue
            if key == self.semkey.get(x) and val > self.cnt[x]:
                continue
            if best.get(key, 0) < val:
                best[key] = val
        for key, val in best.items():
            self.prog[x].append(("w", self.sems[key], val))
            kn[key] = val
            self.ninstr += 1

    def op(self, x, meth, R=(), W=(), inc=True, **kw):
        import os
        if x == "pool" and os.environ.get("NOPOOL"):
            x = "dve"
        fn = (lambda e, meth=meth, kw=kw: getattr(e, meth)(**kw))
        pk = [b for b in R if b in self.psum_keys]
        if pk:
            R = [b for b in R if b not in self.psum_keys]
            W = list(W) + pk
        deps = self._deps(R, W)
        self._emit_waits(x, deps)
        if self.cnt[x] + 1 > SEM_LIMIT:
            self.epoch[x] += 1
            self._new_epoch(x, self.epoch[x])
        self.ninstr += 1
        key = self.semkey[x]
        if inc:
            self.cnt[x] += 1
            self.prog[x].append(("o", fn, self.sems[key], 1))
            tok = (key, self.cnt[x])
        else:
            self.prog[x].append(("o", fn, None, 0))
            tok = (key, self.cnt[x] + 1)
        self._record(tok, R, W)
        return tok

    def dma(self, out, in_, R=(), W=(), q="sp", is_output=False, **kw):
        key = self.dma_sems[self.dma_rr % N_DMA_SEMS]
        self.dma_rr += 1
        deps = self._deps(R, W)
        prev = self.dma_uses[key]
        if prev:
            deps.add((key, 16 * prev))
        self._emit_waits(q, deps)
        self.prog[q].append(("o", (lambda e, out=out, in_=in_, kw=kw: e.dma_start(out=out, in_=in_, **kw)),
                             self.sems[key], 16))
        self.ninstr += 1
        self.dma_uses[key] = prev + 1
        tok = (key, 16 * (prev + 1))
        self._record(tok, R, W)
        if is_output:
            self.out_tokens.append(tok)
        return tok

    def finish(self, q="sp"):
        self._emit_waits(q, set(self.out_tokens))
        deps = set()
        for e in ("pe", "dve", "act", "pool"):
            if self.cnt[e] > 0:
                deps.add((self.semkey[e], self.cnt[e]))
        for key, n in self.dma_uses.items():
            if n:
                deps.add((key, 16 * n))
        self._emit_waits(q, deps)
        self.emit()

    def begin_phase(self):
        from contextlib import ExitStack
        ps = ExitStack()
        ps.__enter__()
        if not hasattr(self, "_stk"):
            self._stk = []
        self._stk.append(self.stack)
        self.stack = ps

    def end_phase(self, q="sp"):
        deps = set()
        for key, n in self.dma_uses.items():
            if n:
                deps.add((key, 16 * n))
        for e in ("pe", "dve", "act", "pool"):
            if self.cnt[e] > 0:
                deps.add((self.semkey[e], self.cnt[e]))
        self._emit_waits(q, deps)
        self.emit()
        self.prog = {e: [] for e in self.eng}
        ps = self.stack
        self.stack = self._stk.pop()
        ps.__exit__(None, None, None)
        self.last_w = {b: t for b, t in self.last_w.items() if isinstance(b, str) and b.startswith("D:")}
        self.readers = {b: t for b, t in self.readers.items() if isinstance(b, str) and b.startswith("D:")}

    def emit(self):
        def runner(lst):
            def f(eng):
                for it in lst:
                    if it[0] == "w":
                        eng.wait_ge(it[1], it[2])
                    else:
                        ins = it[1](eng)
                        if it[2] is not None:
                            ins.then_inc(it[2], it[3])
            return f
        with self.nc.Block() as block:
            block.sync(runner(self.prog["sp"]))
            block.tensor(runner(self.prog["pe"]))
            block.vector(runner(self.prog["dve"]))
            block.scalar(runner(self.prog["act"]))
            block.gpsimd(runner(self.prog["pool"]))

    def sb(self, name, shape, dtype=F32):
        self.nsem += 1
        return self.stack.enter_context(self.nc.sbuf_tensor(f"{name}_{self.nsem}", list(shape), dtype))

    def ps(self, name, shape, dtype=F32):
        self.nsem += 1
        self.psum_keys.add(name)
        return self.stack.enter_context(self.nc.psum_tensor(f"{name}_{self.nsem}", list(shape), dtype))


import numpy as np
from contextlib import ExitStack
import concourse.bass as bass
import concourse.mybir as mybir
from concourse.bass_utils import run_bass_kernel_spmd
import ml_dtypes

BF = ml_dtypes.bfloat16
EPS = 1e-6
D = 4096


def new_nc():
    return bass.Bass("TRN2", target_bir_lowering=False)


def din(nc, name, shape, dt=F32):
    return nc.dram_tensor(name, list(shape), dt, kind="ExternalInput").ap()


def dout(nc, name, shape, dt=F32):
    return nc.dram_tensor(name, list(shape), dt, kind="ExternalOutput").ap()


def dscr(nc, name, shape, dt=F32):
    return nc.dram_tensor(name, list(shape), dt, kind="Internal").ap()


class NormT:
    def __init__(self, k, ident, nbuf=2, pfx="n"):
        self.k = k
        self.ident = ident
        self.pfx = pfx
        self.xt = [k.sb(f"{pfx}xt{i}", [128, D]) for i in range(nbuf)]
        self.tmp = k.sb(f"{pfx}tmp", [128, D])
        self.hb = [k.sb(f"{pfx}hb{i}", [128, D], BF16) for i in range(nbuf)]
        self.ss = [k.sb(f"{pfx}ss{i}", [128, 2]) for i in range(nbuf)]
        self.pst = [k.ps(f"{pfx}pst{i}", [128, 1024], BF16) for i in range(2)]
        self.n = 0
        self.nps = 0

    def tile(self, x_rows, xkey_R, scale_bc, shift_bc, scale_key, hT, hT_key):
        k = self.k
        i = self.n % len(self.xt)
        self.n += 1
        p = self.pfx
        xt, hb, ss = self.xt[i], self.hb[i], self.ss[i]
        k.dma(xt[:], x_rows, R=xkey_R, W=[f"{p}xt{i}"])
        k.op("act", "activation", out=self.tmp[:], in_=xt[:], func=AF.Square, accum_out=ss[:, 0:1],
             R=[f"{p}xt{i}"], W=[f"{p}tmp", f"{p}ss{i}"])
        k.op("dve", "tensor_scalar", out=ss[:, 1:2], in0=ss[:, 0:1], scalar1=1.0 / D, scalar2=EPS,
             op0=ALU.mult, op1=ALU.add, R=[f"{p}ss{i}"], W=[f"{p}ss{i}"])
        k.op("act", "activation", out=ss[:, 1:2], in_=ss[:, 1:2], func=AF.Sqrt, R=[f"{p}ss{i}"], W=[f"{p}ss{i}"])
        k.op("dve", "reciprocal", out=ss[:, 1:2], in_=ss[:, 1:2], R=[f"{p}ss{i}"], W=[f"{p}ss{i}"])
        k.op("dve", "scalar_tensor_tensor", out=self.tmp[:], in0=xt[:], scalar=ss[:, 1:2], in1=scale_bc,
             op0=ALU.mult, op1=ALU.mult, R=[f"{p}xt{i}", f"{p}ss{i}", scale_key], W=[f"{p}tmp"])
        k.op("pool", "tensor_tensor", out=hb[:], in0=self.tmp[:], in1=shift_bc, op=ALU.add,
             R=[f"{p}tmp", scale_key], W=[f"{p}hb{i}"])
        for b in range(4):
            j = self.nps % 2
            self.nps += 1
            for q in range(8):
                kk = b * 8 + q
                k.op("pe", "transpose", out=self.pst[j][:, q * 128:(q + 1) * 128],
                     in_=hb[:, kk * 128:(kk + 1) * 128], identity=self.ident,
                     R=[f"{p}hb{i}", "ident"], W=[f"{p}pst{j}"], inc=(q == 7))
            eng = "act" if b % 2 == 0 else "dve"
            meth = "copy" if eng == "act" else "tensor_copy"
            k.op(eng, meth, out=hT[:, b * 8:(b + 1) * 8, :],
                 in_=self.pst[j][:].rearrange("p (a b) -> p a b", a=8),
                 R=[f"{p}pst{j}"], W=[hT_key])


def load_bc(k, dst, vec_ap, key):
    k.dma(dst, vec_ap.partition_broadcast(128), W=[key])


def build_B(NT=9):
    nc = new_nc()
    x = din(nc, "x", [NT * 128, D])
    mv = din(nc, "mv", [5, D])
    idn = din(nc, "ident", [128, 128], BF16)
    hT = dout(nc, "hT", [D, NT * 128], BF16)
    with ExitStack() as st:
        k = KB(nc, st)
        ident = k.sb("ident_sb", [128, 128], BF16)
        k.dma(ident[:], idn, W=["ident"])
        mods = emit_mods(k, mv)
        nt = NormT(k, ident[:])
        hts = [k.sb(f"hT{i}", [128, 32, 128], BF16) for i in range(2)]
        for t in range(NT):
            sc, sh = mods[0] if t == 0 else mods[1]
            j = t % 2
            nt.tile(x[t * 128:(t + 1) * 128, :], [], sc[:], sh[:], "mods", hts[j], f"hT{j}")
            k.dma(hT[:, t * 128:(t + 1) * 128].rearrange("(a p) t -> p a t", p=128), hts[j][:],
                  R=[f"hT{j}"], W=["hTout"], is_output=True)
        k.finish()
    return nc


def emit_mods(k, mv, pfx="m"):
    g = k.sb(pfx + "g", [128, D])
    tiles = [k.sb(f"{pfx}{i}", [128, D]) for i in range(4)]
    load_bc(k, g[:], mv[0:1, :], "mods_g")
    for i in range(4):
        load_bc(k, tiles[i][:], mv[1 + i:2 + i, :], f"mods_r{i}")
    for i in (0, 2):
        k.op("dve", "scalar_tensor_tensor", out=tiles[i][:], in0=tiles[i][:], scalar=1.0, in1=g[:],
             op0=ALU.add, op1=ALU.mult, R=["mods_g", f"mods_r{i}"], W=[f"mods_r{i}", "mods"])
    k.op("dve", "tensor_copy", out=g[:, 0:1], in_=tiles[1][:, 0:1], R=["mods_r1", "mods_r3", "mods_g"], W=["mods", "mods_g"])
    return [(tiles[0], tiles[1]), (tiles[2], tiles[3])]


def ref_B(x, mv):
    xf = x.astype(np.float64)
    y = xf / np.sqrt((xf * xf).mean(-1, keepdims=True) + EPS) * mv[0]
    out = np.empty_like(y)
    out[:128] = y[:128] * (1 + mv[1]) + mv[2]
    out[128:] = y[128:] * (1 + mv[3]) + mv[4]
    return out.T


C_Q, C_K, C_V, C_QB, C_KB, C_VB, C_OB, C_GT, C_N = 0, 512, 640, 768, 1024, 1280, 1792, 2304, 2312
ATT_SCALE = 128 ** -0.5


def emit_linear_tm(k, hT, W, out, T, N, out_key, pfx="l"):
    wb = k.sb(pfx + "wb", [128, 32, 512], BF16)
    stg = [k.sb(f"{pfx}stg{i}", [128, 8, 512]) for i in range(2)]
    hb = [k.sb(f"{pfx}hb{i}", [128, 32, 512], BF16) for i in range(2)]
    osb = [k.sb(f"{pfx}o{i}", [128, 512]) for i in range(3)]
    ps = [k.ps(f"{pfx}ps{i}", [128, 512]) for i in range(3)]
    ns = nh = no = 0
    for c0 in range(0, N, 512):
        nw = min(512, N - c0)
        for q in range(4):
            j = ns % 2
            ns += 1
            k.dma(stg[j][:, :, :nw], W[q * 1024:(q + 1) * 1024, c0:c0 + nw].rearrange("(a p) n -> p a n", p=128),
                  W=[f"{pfx}stg{j}"])
            if q % 2 == 0:
                k.op("pool", "tensor_copy", out=wb[:, q * 8:(q + 1) * 8, :nw], in_=stg[j][:, :, :nw],
                     R=[f"{pfx}stg{j}"], W=[pfx + "wb"])
            else:
                k.op("act", "copy", out=wb[:, q * 8:(q + 1) * 8, :nw], in_=stg[j][:, :, :nw],
                     R=[f"{pfx}stg{j}"], W=[pfx + "wb"])
        for t0 in range(0, T, 512):
            tw = min(512, T - t0)
            jh = nh % 2
            nh += 1
            k.dma(hb[jh][:, :, :tw], hT[:, t0:t0 + tw].rearrange("(a p) t -> p a t", p=128), W=[f"{pfx}hb{jh}"])
            for tt in range(0, tw, 128):
                jo = no % 3
                no += 1
                for kk in range(32):
                    k.op("pe", "matmul", out=ps[jo][:, :nw], lhsT=hb[jh][:, kk, tt:tt + 128], rhs=wb[:, kk, :nw],
                         start=(kk == 0), stop=(kk == 31), R=[f"{pfx}hb{jh}", pfx + "wb"], W=[f"{pfx}ps{jo}"],
                         inc=(kk == 31))
                if jo % 2 == 0:
                    k.op("act", "copy", out=osb[jo][:, :nw], in_=ps[jo][:, :nw], R=[f"{pfx}ps{jo}"], W=[f"{pfx}o{jo}"])
                else:
                    k.op("dve", "tensor_copy", out=osb[jo][:, :nw], in_=ps[jo][:, :nw], R=[f"{pfx}ps{jo}"],
                         W=[f"{pfx}o{jo}"])
                k.dma(out[t0 + tt:t0 + tt + 128, c0:c0 + nw], osb[jo][:, :nw], R=[f"{pfx}o{jo}"], W=[out_key])


def build_C0(CT=2, LT=32, stop_after=3, part="all"):
    NTt = CT + LT
    T = NTt * 128
    nc = new_nc()
    hT = din(nc, "hT", [D, T], BF16)
    W = din(nc, "W", [D, C_N])
    cst = din(nc, "cst", [128, 5, 128])
    idn = din(nc, "ident", [128, 128], BF16)
    qkg = din(nc, "qkg", [2, 128])
    gb = din(nc, "gb", [1, 8])
    hng = din(nc, "hng", [2, 256])
    cs_t = din(nc, "rope", [LT * 128, 128])
    yT = dout(nc, "yT", [1024, T], BF16)
    if part == "a":
        Pm = dout(nc, "Pm", [T, C_N])
    elif part == "b":
        Pm = din(nc, "Pm", [T, C_N])
    else:
        Pm = dscr(nc, "Pm", [T, C_N])
    with ExitStack() as st:
        k = KB(nc, st)
        import os
        if part != "b":
            k.begin_phase()
            emit_linear_tm(k, hT, W, Pm, T, C_N, "D:Pm")
            k.end_phase()
        if stop_after < 1.5 or part == "a":
            return nc
        k.begin_phase()
        ident = k.sb("ident_sb", [128, 128], BF16)
        k.dma(ident[:], idn, W=["ident"])
        ones = k.sb("ones", [128, 128], BF16)
        k.op("dve", "memset", ap=ones[:], constant=1.0, W=["ones"])
        g5 = k.sb("g5", [128, 5, 128])
        for j in range(5):
            k.dma(g5[:, j, :], qkg[(0 if j < 4 else 1):(1 if j < 4 else 2), :].partition_broadcast(128), W=["g5"])
        QT = k.sb("QT", [128, 4, T], BF16)
        KT = k.sb("KT", [128, T], BF16)
        Vs = k.sb("Vs", [128, NTt, 128], BF16)
        xin = [k.sb(f"xin{i}", [128, 768]) for i in range(2)]
        cs = [k.sb(f"cs{i}", [128, 128]) for i in range(2)]
        sq = k.sb("sq", [128, 5, 128])
        xn = k.sb("xn", [128, 5, 128])
        ta = k.sb("ta", [128, 5, 64])
        tb = k.sb("tb", [128, 5, 64])
        xo = [k.sb(f"xo{i}", [128, 5, 128], BF16) for i in range(2)]
        st5 = k.sb("st5", [128, 10])
        pst = [k.ps(f"pst{i}", [128, 1024], BF16) for i in range(2)]
        for t in range(NTt):
            i = t % 2
            k.dma(xin[i][:], Pm[t * 128:(t + 1) * 128, 0:768], R=["D:Pm"], W=[f"xin{i}"])
            xv = xin[i][:, 0:640].rearrange("p (h d) -> p h d", h=5)
            if int(os.environ.get("PREP_CUT", "9")) < 1:
                continue
            k.op("pool", "tensor_tensor", out=sq[:], in0=xv, in1=xv, op=ALU.mult, R=[f"xin{i}"], W=["sq"])
            k.op("dve", "tensor_reduce", out=st5[:, 0:5], in_=sq[:], op=ALU.add, axis=AX.X, R=["sq"], W=["st5"])
            k.op("dve", "tensor_scalar", out=st5[:, 5:10], in0=st5[:, 0:5], scalar1=1.0 / 128, scalar2=EPS,
                 op0=ALU.mult, op1=ALU.add, R=["st5"], W=["st5"])
            k.op("act", "activation", out=st5[:, 5:10], in_=st5[:, 5:10], func=AF.Sqrt, R=["st5"], W=["st5"])
            k.op("dve", "reciprocal", out=st5[:, 5:10], in_=st5[:, 5:10], R=["st5"], W=["st5"])
            k.op("dve", "tensor_tensor", out=xn[:], in0=xv, in1=st5[:, 5:10].unsqueeze(2).to_broadcast([128, 5, 128]),
                 op=ALU.mult, R=[f"xin{i}", "st5"], W=["xn"])
            if t >= CT:
                k.dma(cs[i][:], cs_t[(t - CT) * 128:(t - CT + 1) * 128, :], W=[f"cs{i}"])
                k.op("pool", "tensor_tensor", out=xn[:], in0=xn[:], in1=g5[:], op=ALU.mult, R=["xn", "g5"], W=["xn"])
                cosb = cs[i][:, 0:64].unsqueeze(1).to_broadcast([128, 5, 64])
                sinb = cs[i][:, 64:128].unsqueeze(1).to_broadcast([128, 5, 64])
                x1, x2 = xn[:, :, 0:64], xn[:, :, 64:128]
                k.op("dve", "tensor_tensor", out=ta[:], in0=x1, in1=cosb, op=ALU.mult, R=["xn", f"cs{i}"], W=["ta"])
                k.op("dve", "tensor_tensor", out=tb[:], in0=x2, in1=sinb, op=ALU.mult, R=["xn", f"cs{i}"], W=["tb"])
                k.op("dve", "tensor_tensor", out=xo[i][:, :, 0:64], in0=ta[:], in1=tb[:], op=ALU.subtract,
                     R=["ta", "tb"], W=[f"xo{i}"])
                k.op("dve", "tensor_tensor", out=ta[:], in0=x1, in1=sinb, op=ALU.mult, R=["xn", f"cs{i}"], W=["ta"])
                k.op("dve", "tensor_tensor", out=tb[:], in0=x2, in1=cosb, op=ALU.mult, R=["xn", f"cs{i}"], W=["tb"])
                k.op("pool", "tensor_tensor", out=xo[i][:, :, 64:128], in0=ta[:], in1=tb[:], op=ALU.add,
                     R=["ta", "tb"], W=[f"xo{i}"])
            else:
                k.op("pool", "tensor_tensor", out=xo[i][:], in0=xn[:], in1=g5[:], op=ALU.mult, R=["xn", "g5"],
                     W=[f"xo{i}"])
            if int(os.environ.get("PREP_CUT", "9")) < 3:
                continue
            for j in range(5):
                k.op("pe", "transpose", out=pst[i][:, j * 128:(j + 1) * 128], in_=xo[i][:, j, :], identity=ident[:],
                     R=[f"xo{i}", "ident"], W=[f"pst{i}"], inc=(j == 4))
            if int(os.environ.get("PREP_CUT", "9")) < 5:
                continue
            k.op("act", "copy", out=QT[:, :, t * 128:(t + 1) * 128],
                 in_=pst[i][:, 0:512].rearrange("p (h d) -> p h d", h=4), R=[f"pst{i}"], W=["QT"])
            k.op("dve", "tensor_copy", out=KT[:, t * 128:(t + 1) * 128], in_=pst[i][:, 512:640], R=[f"pst{i}"], W=["KT"])
            k.op("act", "copy", out=Vs[:, t, :], in_=xin[i][:, 640:768], R=[f"xin{i}"], W=["Vs"])
        if stop_after < 1.7:
            k.end_phase()
            return nc
        E = [k.sb(f"E{i}", [128, 512], BF16) for i in range(3)]
        pS = [k.ps(f"pS{i}", [128, 512]) for i in range(2)]
        pO = [k.ps(f"pO{i}", [128, 512]) for i in range(2)]
        pD = [k.ps(f"pD{i}", [128, 512]) for i in range(2)]
        rc = k.sb("rc", [128, 512])
        yo = [k.sb(f"yo{i}", [128, 512], BF16) for i in range(2)]
        nE = nS = nb = 0
        for j in range(4):
            blocks = [(0, CT * 128, range(0, CT))]
            for q0 in range(CT * 128, T, 512):
                blocks.append((q0, min(512, T - q0), range(0, NTt)))
            for (q0, qw, kts) in blocks:
                ib = nb % 2
                nb += 1
                kts = list(kts)

                def SA(kt):
                    nonlocal nS, nE
                    iS = nS % 2
                    nS += 1
                    iE = nE % 3
                    nE += 1
                    k.op("pe", "matmul", out=pS[iS][:, :qw], lhsT=KT[:, kt * 128:(kt + 1) * 128],
                         rhs=QT[:, j, q0:q0 + qw], start=True, stop=True, R=["KT", "QT"], W=[f"pS{iS}"])
                    k.op("act", "activation", out=E[iE][:, :qw], in_=pS[iS][:, :qw], func=AF.Exp, scale=ATT_SCALE,
                         R=[f"pS{iS}"], W=[f"E{iE}"])
                    return iE

                pend = SA(kts[0])
                for n_, kt in enumerate(kts):
                    nxt = SA(kts[n_ + 1]) if n_ + 1 < len(kts) else None
                    iE = pend
                    last = (n_ == len(kts) - 1)
                    k.op("pe", "matmul", out=pO[ib][:, :qw], lhsT=Vs[:, kt, :], rhs=E[iE][:, :qw], start=(n_ == 0),
                         stop=last, R=["Vs", f"E{iE}"], W=[f"pO{ib}"], inc=last)
                    k.op("pe", "matmul", out=pD[ib][:, :qw], lhsT=ones[:], rhs=E[iE][:, :qw], start=(n_ == 0),
                         stop=last, R=["ones", f"E{iE}"], W=[f"pD{ib}"], inc=last)
                    pend = nxt
                k.op("dve", "reciprocal", out=rc[:, :qw], in_=pD[ib][:, :qw], R=[f"pD{ib}"], W=["rc"])
                k.op("dve", "tensor_tensor", out=yo[ib][:, :qw], in0=pO[ib][:, :qw], in1=rc[:, :qw], op=ALU.mult,
                     R=[f"pO{ib}", "rc"], W=[f"yo{ib}"])
                k.dma(yT[j * 128:(j + 1) * 128, q0:q0 + qw], yo[ib][:, :qw], R=[f"yo{ib}"], W=["D:yT"], is_output=True)
        k.end_phase()
        if stop_after < 3:
            return nc
        k.begin_phase()
        ident = k.sb("ident_sb", [128, 128], BF16)
        k.dma(ident[:], idn, W=["ident"])
        cst_sb = k.sb("cst_sb", [128, 5, 128])
        k.dma(cst_sb[:], cst, W=["cst"])
        hgb = k.sb("hgb", [128, 2, 256])
        for h in range(2):
            k.dma(hgb[:, h, :], hng[h:h + 1, :].partition_broadcast(128), W=["hgb"])
        gbb = k.sb("gbb", [128, 8])
        k.dma(gbb[:], gb.partition_broadcast(128), W=["gbb"])
        Gt = k.sb("Gt", [128, NTt, 8])
        k.dma(Gt[:], Pm[:, C_GT:C_GT + 8].rearrange("(n p) c -> p n c", p=128), R=["D:Pm"], W=["Gt"])
        k.op("dve", "tensor_tensor", out=Gt[:], in0=Gt[:], in1=gbb[:].unsqueeze(1).to_broadcast([128, NTt, 8]),
             op=ALU.add, R=["Gt", "gbb"], W=["Gt"])
        LF = k.sb("LF", [128, 2, NTt, 2])
        for d_ in range(2):
            zc = Gt[:, :, 2 + 4 * d_:4 + 4 * d_]
            k.op("act", "activation", out=LF[:, d_, :, :], in_=zc, func=AF.Exp, scale=-1.0, R=["Gt"], W=["LF"])
        k.op("act", "activation", out=LF[:], in_=LF[:], func=AF.Ln, bias=1.0, R=["LF"], W=["LF"])
        k.op("dve", "tensor_scalar", out=LF[:], in0=LF[:], scalar1=-1.0, scalar2=None, op0=ALU.mult, R=["LF"], W=["LF"])
        pg = k.ps("pg", [128, 512])
        NG = NTt * 2
        for d_ in range(2):
            rhs = LF[:, d_, :, :].rearrange("p n h -> p (n h)")
            k.op("pe", "matmul", out=pg[:, d_ * NG:(d_ + 1) * NG], lhsT=cst_sb[:, d_, :], rhs=rhs, start=True, stop=True,
                 R=["cst", "LF"], W=["pg"])
            k.op("pe", "matmul", out=pg[:, (2 + d_) * NG:(3 + d_) * NG], lhsT=cst_sb[:, 2, :], rhs=rhs, start=True,
                 stop=True, R=["cst", "LF"], W=["pg"])
        Aa = k.sb("Aa", [128, 2, NTt, 2])
        Cc = k.sb("Cc", [128, 2, NTt, 2])
        EB = k.sb("EB", [128, 2, NTt, 2])
        k.op("act", "activation", out=Aa[:].rearrange("p d n h -> p (d n h)"), in_=pg[:, 0:2 * NG], func=AF.Exp,
             R=["pg"], W=["Aa"])
        k.op("act", "activation", out=EB[:].rearrange("p d n h -> p (d n h)"), in_=pg[:, 2 * NG:4 * NG], func=AF.Exp,
             R=["pg"], W=["EB"])
        for d_ in range(2):
            k.op("dve", "tensor_tensor", out=Cc[:, d_, :, :], in0=Gt[:, :, 4 * d_:4 * d_ + 2],
                 in1=pg[:, d_ * NG:(d_ + 1) * NG].rearrange("p (n h) -> p n h", h=2), op=ALU.subtract,
                 R=["Gt", "pg"], W=["Cc"])
        lnsc = k.sb("lnsc", [128, 1])
        k.op("dve", "memset", ap=lnsc[:], constant=float(-0.5 * np.log(128.0)), W=["lnsc"])
        k.op("act", "activation", out=Cc[:], in_=Cc[:], func=AF.Exp, bias=lnsc[:, 0:1], R=["Cc", "lnsc"], W=["Cc"])
        stg = k.sb("stg", [128, NTt, 256])
        Hs = k.sb("Hs", [128, NTt, 256])
        hsc = k.sb("hsc", [128, 4])
        pst = [k.ps(f"pst{i}", [128, 1024], BF16) for i in range(2)]
        pP = [k.ps(f"pP{i}", [128, 512]) for i in range(2)]
        pN = [k.ps(f"pN{i}", [128, 512]) for i in range(2)]
        pC = k.ps("pC", [128, 512])
        npst = 0

        def transpose_all(src, dst, src_key, dst_key, ncol_tiles=1):
            nonlocal npst
            for a in range(ncol_tiles):
                for c0 in range(0, NTt, 8):
                    cn = min(8, NTt - c0)
                    i = npst % 2
                    npst += 1
                    for c in range(cn):
                        k.op("pe", "transpose", out=pst[i][:, c * 128:(c + 1) * 128],
                             in_=src[:, c0 + c, a * 128:(a + 1) * 128], identity=ident[:],
                             R=[src_key, "ident"], W=[f"pst{i}"], inc=(c == cn - 1))
                    dv = dst[:, c0 * 128:(c0 + cn) * 128] if ncol_tiles == 1 else dst[:, a, c0 * 128:(c0 + cn) * 128]
                    if (c0 // 8) % 2 == 0:
                        k.op("act", "copy", out=dv, in_=pst[i][:, :cn * 128], R=[f"pst{i}"], W=[dst_key])
                    else:
                        k.op("dve", "tensor_copy", out=dv, in_=pst[i][:, :cn * 128], R=[f"pst{i}"], W=[dst_key])

        for hh in range(2):
            k.begin_phase()
            qb = k.sb("qb", [128, NTt, 128], BF16)
            kf = [k.sb(f"kf{d_}", [128, NTt, 128], BF16) for d_ in range(2)]
            qT = k.sb("qT", [128, T], BF16)
            kT = [k.sb(f"kT{d_}", [128, T], BF16) for d_ in range(2)]
            vt = k.sb("vt", [128, NTt, 257], BF16)
            Cst = k.sb("Cst", [128, 257])
            Cb = k.sb("Cb", [128, 257], BF16)
            PTm = [k.sb(f"PTm{i}", [128, 128], BF16) for i in range(2)]
            k.dma(stg[:, :, 0:128], Pm[:, C_QB + 128 * hh:C_QB + 128 * hh + 128].rearrange("(n p) c -> p n c", p=128),
                  R=["D:Pm"], W=["stg"])
            k.op("act", "copy", out=qb[:], in_=stg[:, :, 0:128], R=["stg"], W=["qb"])
            transpose_all(qb, qT, "qb", "qT")
            k.dma(stg[:, :, 0:128], Pm[:, C_KB + 128 * hh:C_KB + 128 * hh + 128].rearrange("(n p) c -> p n c", p=128),
                  R=["D:Pm"], W=["stg"])
            for d_ in range(2):
                k.op("dve", "tensor_tensor", out=kf[d_][:], in0=stg[:, :, 0:128],
                     in1=Cc[:, d_, :, hh:hh + 1].to_broadcast([128, NTt, 128]), op=ALU.mult,
                     R=["stg", "Cc"], W=[f"kf{d_}"])
                transpose_all(kf[d_], kT[d_], f"kf{d_}", f"kT{d_}")
            k.dma(stg[:], Pm[:, C_VB + 256 * hh:C_VB + 256 * hh + 256].rearrange("(n p) c -> p n c", p=128),
                  R=["D:Pm"], W=["stg"])
            k.op("act", "copy", out=vt[:, :, 0:256], in_=stg[:], R=["stg"], W=["vt"])
            k.op("dve", "memset", ap=vt[:, :, 256:257], constant=1.0, W=["vt"])
            nP = 0
            for d_ in range(2):
                order = list(range(NTt)) if d_ == 0 else (list(range(CT - 1, -1, -1)) + list(range(NTt - 1, CT - 1, -1)))
                k.op("dve", "memset", ap=Cst[:], constant=0.0, W=["Cst"])
                k.op("dve", "memset", ap=Cb[:], constant=0.0, W=["Cb"])
                for c in order:
                    i = nP % 2
                    nP += 1
                    cols = slice(c * 128, (c + 1) * 128)
                    k.op("pe", "matmul", out=pP[i][:, 0:128], lhsT=kT[d_][:, cols], rhs=qT[:, cols], start=True, stop=True,
                         R=[f"kT{d_}", "qT"], W=[f"pP{i}"])
                    k.op("dve", "tensor_tensor", out=PTm[i][:], in0=pP[i][:, 0:128], in1=cst_sb[:, d_, :], op=ALU.mult,
                         R=[f"pP{i}", "cst"], W=[f"PTm{i}"])
                    k.op("pe", "matmul", out=pN[i][:, 0:257], lhsT=qT[:, cols], rhs=Cb[:], start=True, stop=False,
                         R=["qT", "Cb"], W=[f"pN{i}"], inc=False)
                    k.op("pe", "matmul", out=pN[i][:, 0:257], lhsT=PTm[i][:], rhs=vt[:, c, :], start=False, stop=True,
                         R=[f"PTm{i}", "vt"], W=[f"pN{i}"])
                    k.op("pe", "matmul", out=pC[:, 0:257], lhsT=kf[d_][:, c, :], rhs=vt[:, c, :], start=True, stop=True,
                         R=[f"kf{d_}", "vt"], W=["pC"])
                    eb = EB[:, d_, c, hh:hh + 1]
                    k.op("dve", "tensor_scalar", out=Cst[:], in0=Cst[:], scalar1=eb, scalar2=None, op0=ALU.mult,
                         R=["Cst", "EB"], W=["Cst"])
                    k.op("dve", "scalar_tensor_tensor", out=Cst[:], in0=pC[:, 0:257], scalar=eb, in1=Cst[:],
                         op0=ALU.mult, op1=ALU.add, R=["pC", "EB", "Cst"], W=["Cst"])
                    k.op("act", "copy", out=Cb[:], in_=Cst[:], R=["Cst"], W=["Cb"])
                    a_ = Aa[:, d_, c, hh:hh + 1]
                    k.op("dve", "tensor_scalar", out=hsc[:, 0:1], in0=pN[i][:, 256:257], scalar1=a_, scalar2=None,
                         op0=ALU.mult, R=[f"pN{i}", "Aa"], W=["hsc"])
                    k.op("dve", "tensor_scalar", out=hsc[:, 1:2], in0=hsc[:, 0:1], scalar1=-1.0, scalar2=None,
                         op0=ALU.mult, R=["hsc"], W=["hsc"])
                    k.op("dve", "scalar_tensor_tensor", out=hsc[:, 1:2], in0=hsc[:, 1:2], scalar=1.0, in1=hsc[:, 0:1],
                         op0=ALU.max, op1=ALU.max, R=["hsc"], W=["hsc"])
                    k.op("dve", "reciprocal", out=hsc[:, 2:3], in_=hsc[:, 1:2], R=["hsc"], W=["hsc"])
                    k.op("dve", "tensor_tensor", out=hsc[:, 3:4], in0=hsc[:, 2:3], in1=a_, op=ALU.mult,
                         R=["hsc", "Aa"], W=["hsc"])
                    if d_ == 0:
                        k.op("dve", "tensor_scalar", out=Hs[:, c, :], in0=pN[i][:, 0:256], scalar1=hsc[:, 3:4],
                             scalar2=None, op0=ALU.mult, R=[f"pN{i}", "hsc"], W=["Hs"])
                    else:
                        k.op("dve", "scalar_tensor_tensor", out=Hs[:, c, :], in0=pN[i][:, 0:256], scalar=hsc[:, 3:4],
                             in1=Hs[:, c, :], op0=ALU.mult, op1=ALU.add, R=[f"pN{i}", "hsc", "Hs"], W=["Hs"])
            k.end_phase()
            k.begin_phase()
            Yb = k.sb("Yb", [128, NTt, 256], BF16)
            yTo = k.sb("yTo", [128, 2, T], BF16)
            rst = k.sb("rst", [128, 2, NTt])
            k.dma(stg[:], Pm[:, C_OB + 256 * hh:C_OB + 256 * hh + 256].rearrange("(n p) c -> p n c", p=128),
                  R=["D:Pm"], W=["stg"])
            k.op("act", "activation", out=stg[:], in_=stg[:], func=AF.Sigmoid, R=["stg"], W=["stg"])
            k.op("pool", "tensor_tensor", out=Yb[:], in0=Hs[:], in1=Hs[:], op=ALU.mult, R=["Hs"], W=["Yb"])
            k.op("dve", "tensor_reduce", out=rst[:, 0, :], in_=Yb[:], op=ALU.add, axis=AX.X, R=["Yb"], W=["rst"])
            k.op("dve", "tensor_scalar", out=rst[:, 1, :], in0=rst[:, 0, :], scalar1=1.0 / 256, scalar2=EPS,
                 op0=ALU.mult, op1=ALU.add, R=["rst"], W=["rst"])
            k.op("act", "activation", out=rst[:, 1, :], in_=rst[:, 1, :], func=AF.Sqrt, R=["rst"], W=["rst"])
            k.op("dve", "reciprocal", out=rst[:, 1, :], in_=rst[:, 1, :], R=["rst"], W=["rst"])
            k.op("dve", "tensor_tensor", out=Hs[:], in0=Hs[:], in1=rst[:, 1, :].unsqueeze(2).to_broadcast([128, NTt, 256]),
                 op=ALU.mult, R=["Hs", "rst"], W=["Hs"])
            k.op("dve", "tensor_tensor", out=Hs[:], in0=Hs[:], in1=hgb[:, hh, :].unsqueeze(1).to_broadcast([128, NTt, 256]),
                 op=ALU.mult, R=["Hs", "hgb"], W=["Hs"])
            k.op("dve", "tensor_tensor", out=Yb[:], in0=Hs[:], in1=stg[:], op=ALU.mult, R=["Hs", "stg"], W=["Yb"])
            transpose_all(Yb, yTo, "Yb", "yTo", ncol_tiles=2)
            for a in range(2):
                k.dma(yT[512 + hh * 256 + a * 128:512 + hh * 256 + (a + 1) * 128, :], yTo[:, a, :], R=["yTo"],
                      W=["D:yT"], is_output=True)
            k.end_phase()
        k.end_phase()
    return nc


NH1 = 8


def na_bias_table(rpb_h, rows):
    out = np.full((128, 8, 4, 64), -30000.0, np.float32)
    q = np.arange(64)
    cs = np.clip(q - 8, 0, 48)
    for di in range(8):
        for a in range(4):
            for rr2 in range(2):
                rr = 2 * a + rr2
                roff = 7 - di + rr
                for kc in range(64):
                    valid = (kc >= cs) & (kc < cs + 16)
                    coff = np.clip(kc - q + 15, 0, 30)
                    out[rr2 * 64 + kc, di, a, :] = np.where(valid, rpb_h[roff, coff], -30000.0)
    return out


def build_C1(CT=2, LT=32, nheads=NH1):
    NTt = CT + LT
    T = NTt * 128
    L0 = CT * 128
    rows = LT * 2
    NW = 3 * 128 * nheads
    nc = new_nc()
    hT = din(nc, "hT", [D, T], BF16)
    W = din(nc, "W", [D, NW])
    idn = din(nc, "ident", [128, 128], BF16)
    Bt = din(nc, "Bt", [nheads, 128, 8 * 4 * 64])
    yT = dout(nc, "yT", [nheads * 128, LT * 128], BF16)
    Pm = dscr(nc, "Pm", [T, NW])
    with ExitStack() as st:
        k = KB(nc, st)
        k.begin_phase()
        emit_linear_tm(k, hT, W, Pm, T, NW, "D:Pm")
        k.end_phase()
        k.begin_phase()
        ident = k.sb("ident_sb", [128, 128], BF16)
        k.dma(ident[:], idn, W=["ident"])
        ones = k.sb("ones", [128, 128], BF16)
        k.op("dve", "memset", ap=ones[:], constant=1.0, W=["ones"])
        stg = k.sb("stg", [128, NTt, 128])
        xb = k.sb("xb", [128, NTt, 128], BF16)
        QT = k.sb("QT", [128, T], BF16)
        KT = k.sb("KT", [128, T], BF16)
        Va = k.sb("Va", [128, NTt, 128], BF16)
        Vb = k.sb("Vb", [128, NTt, 128], BF16)
        Mt = k.sb("Mt", [128, 8, 256])
        E = [k.sb(f"E{i}", [128, 64 * (4 + CT)], BF16) for i in range(3)]
        rc = k.sb("rc", [128, 512])
        yo = [k.sb(f"yo{i}", [128, 512], BF16) for i in range(2)]
        pst = [k.ps(f"pst{i}", [128, 1024], BF16) for i in range(2)]
        pS = [k.ps(f"pS{i}", [128, 512]) for i in range(2)]
        pO = [k.ps(f"pO{i}", [128, 512]) for i in range(2)]
        pD = [k.ps(f"pD{i}", [128, 512]) for i in range(2)]
        npst = [0]

        def transpose_all(src, dst, src_key, dst_key):
            for c0 in range(0, NTt, 8):
                cn = min(8, NTt - c0)
                i = npst[0] % 2
                npst[0] += 1
                for c in range(cn):
                    k.op("pe", "transpose", out=pst[i][:, c * 128:(c + 1) * 128], in_=src[:, c0 + c, :],
                         identity=ident[:], R=[src_key, "ident"], W=[f"pst{i}"], inc=(c == cn - 1))
                dv = dst[:, c0 * 128:(c0 + cn) * 128]
                if (c0 // 8) % 2 == 0:
                    k.op("act", "copy", out=dv, in_=pst[i][:, :cn * 128], R=[f"pst{i}"], W=[dst_key])
                else:
                    k.op("dve", "tensor_copy", out=dv, in_=pst[i][:, :cn * 128], R=[f"pst{i}"], W=[dst_key])

        nE = nS = nG = 0
        NA_ = 4 + CT
        for h in range(nheads):
            for (col, dst, dkey) in ((h * 128, QT, "QT"), ((nheads + h) * 128, KT, "KT")):
                k.dma(stg[:], Pm[:, col:col + 128].rearrange("(n p) c -> p n c", p=128), R=["D:Pm"], W=["stg"])
                k.op("act", "copy", out=xb[:], in_=stg[:], R=["stg"], W=["xb"])
                transpose_all(xb, dst, "xb", dkey)
            vcol = (2 * nheads + h) * 128
            k.dma(stg[:], Pm[:, vcol:vcol + 128].rearrange("(n p) c -> p n c", p=128), R=["D:Pm"], W=["stg"])
            k.op("act", "copy", out=Va[:], in_=stg[:], R=["stg"], W=["Va"])
            k.dma(stg[:, 0:NTt - 1, :], Pm[64:64 + (NTt - 1) * 128, vcol:vcol + 128].rearrange("(n p) c -> p n c", p=128),
                  R=["D:Pm"], W=["stg"])
            k.op("dve", "tensor_copy", out=Vb[:, 0:NTt - 1, :], in_=stg[:, 0:NTt - 1, :], R=["stg"], W=["Vb"])
            k.dma(Mt[:].rearrange("p d c -> p (d c)"), Bt[h], W=["Mt"])
            k.op("act", "activation", out=Mt[:], in_=Mt[:], func=AF.Exp, R=["Mt"], W=["Mt"])
            def SA(r):
                nonlocal nS, nE
                r0 = min(max(r - 4, 0), rows - 8)
                di = r - r0
                iS = nS % 2
                nS += 1
                iE = nE % 3
                nE += 1
                qs = slice(L0 + 64 * r, L0 + 64 * r + 64)
                for a in range(NA_):
                    ks = (L0 + 64 * r0 + 128 * a) if a < 4 else 128 * (a - 4)
                    k.op("pe", "matmul", out=pS[iS][:, a * 64:(a + 1) * 64], lhsT=KT[:, ks:ks + 128], rhs=QT[:, qs],
                         start=True, stop=True, R=["KT", "QT"], W=[f"pS{iS}"], inc=(a == NA_ - 1))
                k.op("act", "activation", out=E[iE][:], in_=pS[iS][:, 0:64 * NA_], func=AF.Exp, scale=ATT_SCALE,
                     R=[f"pS{iS}"], W=[f"E{iE}"])
                k.op("dve", "tensor_tensor", out=E[iE][:, 0:256], in0=E[iE][:, 0:256], in1=Mt[:, di, :], op=ALU.mult,
                     R=[f"E{iE}", "Mt"], W=[f"E{iE}"])
                return iE

            pend = SA(0)
            for r in range(rows):
                nxt = SA(r + 1) if r + 1 < rows else None
                iE = pend
                pend = nxt
                r0 = min(max(r - 4, 0), rows - 8)
                g8, rg = r // 8, r % 8
                ig = nG % 2
                for a in range(NA_):
                    if a < 4:
                        t64 = CT * 2 + r0 + 2 * a
                        vt = Va[:, t64 // 2, :] if t64 % 2 == 0 else Vb[:, (t64 - 1) // 2, :]
                        vkey = "Va" if t64 % 2 == 0 else "Vb"
                    else:
                        vt, vkey = Va[:, a - 4, :], "Va"
                    last = (a == NA_ - 1)
                    k.op("pe", "matmul", out=pO[ig][:, rg * 64:(rg + 1) * 64], lhsT=vt, rhs=E[iE][:, a * 64:(a + 1) * 64],
                         start=(a == 0), stop=last, R=[vkey, f"E{iE}"], W=[f"pO{ig}"], inc=last)
                    k.op("pe", "matmul", out=pD[ig][:, rg * 64:(rg + 1) * 64], lhsT=ones[:], rhs=E[iE][:, a * 64:(a + 1) * 64],
                         start=(a == 0), stop=last, R=["ones", f"E{iE}"], W=[f"pD{ig}"], inc=last)
                if rg == 7:
                    nG += 1
                    k.op("dve", "reciprocal", out=rc[:], in_=pD[ig][:], R=[f"pD{ig}"], W=["rc"])
                    k.op("dve", "tensor_tensor", out=yo[ig][:], in0=pO[ig][:], in1=rc[:], op=ALU.mult,
                         R=[f"pO{ig}", "rc"], W=[f"yo{ig}"])
                    k.dma(yT[h * 128:(h + 1) * 128, g8 * 512:(g8 + 1) * 512], yo[ig][:], R=[f"yo{ig}"], W=["D:yT"],
                          is_output=True)
        k.end_phase()
    return nc


NEG_INF = -1.0e30


def build_D(NT=9, ctx_tiles=1, final=False, stop_after=9, neg=64, cut=9, part="all"):
    R = NT * 128
    if part == "a":
        stop_after = 5
    nc = new_nc()
    x = din(nc, "x", [R, D])
    yT = din(nc, "yT", [D, R], BF16)
    wout = din(nc, "wout", [D, D])
    mv = din(nc, "mv", [10, D])
    wq = din(nc, "wq", [D, 2048])
    skT = din(nc, "skT", [128, 2, 128])
    if stop_after >= 6:
        UTb = din(nc, "UTb", [neg, 128, 32, 256], BF16)
        Vb = din(nc, "Vb", [neg * 256, D], BF16)
    idn = din(nc, "ident", [128, 128], BF16)
    xo = dout(nc, "xo", [R, D]) if part != "a" else None
    if part == "a":
        stop_after = 5
        x1 = dout(nc, "x1", [R, D])
        h2T = dout(nc, "h2T", [D, R], BF16)
        G = dout(nc, "G", [R, 16384], BF16)
    else:
        x1 = dscr(nc, "x1", [R, D])
        h2T = dscr(nc, "h2T", [D, R], BF16)
        G = dscr(nc, "G", [R, 16384], BF16)
    S = dscr(nc, "S", [R, 2048])
    x2 = dscr(nc, "x2", [R, D]) if final else xo
    typ = lambda t: 0 if t < ctx_tiles else 1
    with ExitStack() as st:
        k = KB(nc, st)

        def load_wblock(wb, stg, Wd, c0, nw, ns):
            for q in range(4):
                j = ns[0] % 2
                ns[0] += 1
                k.dma(stg[j][:, :, :nw], Wd[q * 1024:(q + 1) * 1024, c0:c0 + nw].rearrange("(a p) n -> p a n", p=128),
                      W=[f"stg{j}"])
                if q % 2 == 0:
                    k.op("pool", "tensor_copy", out=wb[:, q * 8:(q + 1) * 8, :nw], in_=stg[j][:, :, :nw],
                         R=[f"stg{j}"], W=["wb"])
                else:
                    k.op("act", "copy", out=wb[:, q * 8:(q + 1) * 8, :nw], in_=stg[j][:, :, :nw],
                         R=[f"stg{j}"], W=["wb"])

        k.begin_phase()
        yTs = k.sb("yTs", [128, 32, R], BF16)
        k.dma(yTs[:], yT.rearrange("(a p) t -> p a t", p=128), W=["yTs"])
        wb = k.sb("wb", [128, 32, 512], BF16)
        stg = [k.sb(f"stg{i}", [128, 8, 512]) for i in range(2)]
        g1s = k.sb("g1s", [128, 2, 512])
        xs = [k.sb(f"xs{i}", [128, 512]) for i in range(3)]
        tmp = [k.sb(f"tmp{i}", [128, 512]) for i in range(3)]
        ps = [k.ps(f"ps{i}", [128, 512]) for i in range(3)]
        ns = [0]
        n = 0
        for nb in range(8):
            c0 = nb * 512
            load_wblock(wb, stg, wout, c0, 512, ns)
            for ty in range(2):
                k.dma(g1s[:, ty, :], mv[ty:ty + 1, c0:c0 + 512].partition_broadcast(128), W=["g1s"])
            for t in range(NT):
                i = n % 3
                n += 1
                for kk in range(32):
                    k.op("pe", "matmul", out=ps[i][:], lhsT=yTs[:, kk, t * 128:(t + 1) * 128], rhs=wb[:, kk, :],
                         start=(kk == 0), stop=(kk == 31), R=["yTs", "wb"], W=[f"ps{i}"], inc=(kk == 31))
                k.dma(xs[i][:], x[t * 128:(t + 1) * 128, c0:c0 + 512], W=[f"xs{i}"])
                k.op("dve", "tensor_tensor", out=tmp[i][:], in0=ps[i][:], in1=g1s[:, typ(t), :], op=ALU.mult,
                     R=[f"ps{i}", "g1s"], W=[f"tmp{i}"])
                k.op("pool", "tensor_tensor", out=tmp[i][:], in0=tmp[i][:], in1=xs[i][:], op=ALU.add,
                     R=[f"tmp{i}", f"xs{i}"], W=[f"tmp{i}"])
                k.dma(x1[t * 128:(t + 1) * 128, c0:c0 + 512], tmp[i][:], R=[f"tmp{i}"], W=["D:x1"])
        k.end_phase()
        if stop_after < 2:
            return nc
        k.begin_phase()
        ident = k.sb("ident_sb", [128, 128], BF16)
        k.dma(ident[:], idn, W=["ident"])
        mods = emit_mods(k, mv[2:7, :])
        ntm = NormT(k, ident[:])
        hts = [k.sb(f"hT{i}", [128, 32, 128], BF16) for i in range(2)]
        for t in range(NT):
            sc, sh = mods[typ(t)]
            j = t % 2
            ntm.tile(x1[t * 128:(t + 1) * 128, :], ["D:x1"], sc[:], sh[:], "mods", hts[j], f"hT{j}")
            k.dma(h2T[:, t * 128:(t + 1) * 128].rearrange("(a p) t -> p a t", p=128), hts[j][:],
                  R=[f"hT{j}"], W=["D:h2T"])
        k.end_phase()
        if stop_after < 3:
            return nc
        k.begin_phase()
        h2s = k.sb("h2s", [128, 32, R], BF16)
        k.dma(h2s[:], h2T.rearrange("(a p) t -> p a t", p=128), R=["D:h2T"], W=["h2s"])
        sk = k.sb("sk", [128, 2, 128])
        k.dma(sk[:], skT, W=["sk"])
        wb = k.sb("wb", [128, 32, 512], BF16)
        stg = [k.sb(f"stg{i}", [128, 8, 512]) for i in range(2)]
        qn = k.sb("qn", [128, 4, R])
        so = [k.sb(f"so{i}", [128, 512]) for i in range(2)]
        ps = [k.ps(f"ps{i}", [128, 512]) for i in range(3)]
        p2 = [k.ps(f"p2{i}", [128, 512]) for i in range(2)]
        n = n2 = 0
        for nb in range(4):
            load_wblock(wb, stg, wq, nb * 512, 512, ns)
            for j in range(4):
                for t0 in range(0, R, 512):
                    tw = min(512, R - t0)
                    i = n % 3
                    n += 1
                    for kk in range(32):
                        k.op("pe", "matmul", out=ps[i][:, :tw], lhsT=wb[:, kk, j * 128:(j + 1) * 128],
                             rhs=h2s[:, kk, t0:t0 + tw], start=(kk == 0), stop=(kk == 31), R=["wb", "h2s"],
                             W=[f"ps{i}"], inc=(kk == 31))
                    if n % 2 == 0:
                        k.op("act", "copy", out=qn[:, j, t0:t0 + tw], in_=ps[i][:, :tw], R=[f"ps{i}"], W=["qn"])
                    else:
                        k.op("dve", "tensor_copy", out=qn[:, j, t0:t0 + tw], in_=ps[i][:, :tw], R=[f"ps{i}"], W=["qn"])
            for t in range(NT):
                i = n2 % 2
                n2 += 1
                for j in range(4):
                    k.op("pe", "matmul", out=p2[i][:, j * 128:(j + 1) * 128], lhsT=qn[:, j, t * 128:(t + 1) * 128],
                         rhs=sk[:, j % 2, :], start=True, stop=True, R=["qn", "sk"], W=[f"p2{i}"], inc=(j == 3))
                k.op("dve", "tensor_copy", out=so[i][:], in_=p2[i][:], R=[f"p2{i}"], W=[f"so{i}"])
                k.dma(S[t * 128:(t + 1) * 128, nb * 512:(nb + 1) * 512], so[i][:], R=[f"so{i}"], W=["D:S"])
        k.end_phase()
        if stop_after < 4:
            return nc
        k.begin_phase()
        St = [k.sb(f"St{i}", [128, 16, 128]) for i in range(2)]
        V16 = k.sb("V16", [128, 16, 16])
        scr = k.sb("scr", [128, 256])
        cand = k.sb("cand", [128, 8, 256])
        T16 = k.sb("T16", [128, 8, 16])
        E16 = k.sb("E16", [128, 8, 16])
        zz = k.sb("zz", [128, 16])
        ident = k.sb("ident_sb", [128, 128], BF16)
        k.dma(ident[:], idn, W=["ident"])
        e1 = k.sb("e1", [128, 8, 128])
        e2 = k.sb("e2", [128, 8, 128])
        th = k.sb("th", [128, 8])
        Eg = [k.sb(f"Eg{i}", [128, 16, 128]) for i in range(2)]
        Fb = [k.sb(f"Fb{i}", [128, 2048], BF16) for i in range(2)]
        Gb = [k.sb(f"Gb{i}", [128, 4096], BF16) for i in range(2)]
        pG = [k.ps(f"pG{i}", [128, 2048]) for i in range(2)]
        nd = ngb = 0
        for t in range(NT):
            s_ = St[t % 2]
            sk_ = f"St{t % 2}"
            k.dma(s_[:], S[t * 128:(t + 1) * 128, :].rearrange("p (b c) -> p b c", b=16), R=["D:S"], W=[sk_])
            for b in range(16):
                k.op("dve", "max", out=V16[:, b, 0:8], in_=s_[:, b, :], R=[sk_], W=["V16"])
                k.op("dve", "match_replace", out=scr[:, 0:128], in_to_replace=V16[:, b, 0:8], in_values=s_[:, b, :],
                     imm_value=NEG_INF, R=[sk_, "V16"], W=["scr"])
                k.op("dve", "max", out=V16[:, b, 8:16], in_=scr[:, 0:128], R=["scr"], W=["V16"])
            Vv = V16[:].rearrange("p (h two) r -> p h two r", two=2)
            k.op("dve", "tensor_tensor", out=cand[:].rearrange("p h (a b) -> p h a b", a=16),
                 in0=Vv[:, :, 0, :].unsqueeze(3).to_broadcast([128, 8, 16, 16]),
                 in1=Vv[:, :, 1, :].unsqueeze(2).to_broadcast([128, 8, 16, 16]), op=ALU.add, R=["V16"], W=["cand"])
            for h in range(8):
                k.op("dve", "max", out=T16[:, h, 0:8], in_=cand[:, h, :], R=["cand"], W=["T16"])
                k.op("dve", "match_replace", out=scr[:], in_to_replace=T16[:, h, 0:8], in_values=cand[:, h, :],
                     imm_value=NEG_INF, R=["cand", "T16"], W=["scr"])
                k.op("dve", "max", out=T16[:, h, 8:16], in_=scr[:], R=["scr"], W=["T16"])
            k.op("act", "activation", out=E16[:], in_=T16[:], func=AF.Exp, R=["T16"], W=["E16"])
            k.op("dve", "tensor_reduce", out=zz[:, 0:8], in_=E16[:], op=ALU.add, axis=AX.X, R=["E16"], W=["zz"])
            k.op("act", "activation", out=zz[:, 8:16], in_=zz[:, 0:8], func=AF.Ln, R=["zz"], W=["zz"])
            k.op("dve", "tensor_scalar", out=zz[:, 8:16], in0=zz[:, 8:16], scalar1=-1.0, scalar2=None, op0=ALU.mult,
                 R=["zz"], W=["zz"])
            Sv = s_[:].rearrange("p (h two) c -> p h two c", two=2)
            for h in range(8):
                k.op("act", "activation", out=e1[:, h, :], in_=Sv[:, h, 0, :], func=AF.Exp, bias=zz[:, 8 + h:9 + h],
                     R=[sk_, "zz"], W=["e1"])
            k.op("act", "activation", out=e2[:], in_=Sv[:, :, 1, :], func=AF.Exp, R=[sk_], W=["e2"])
            k.op("dve", "scalar_tensor_tensor", out=th[:], in0=T16[:, :, 15], scalar=-1.0e-4, in1=zz[:, 8:16],
                 op0=ALU.add, op1=ALU.add, R=["T16", "zz"], W=["th"])
            k.op("act", "activation", out=th[:], in_=th[:], func=AF.Exp, R=["th"], W=["th"])
            for oi in range(8):
                ig = ngb % 2
                ngb += 1
                for h in range(8):
                    i = nd % 2
                    nd += 1
                    k.op("dve" if nd % 6 == 0 else "pool", "tensor_tensor", out=Eg[i][:],
                         in0=e1[:, h, oi * 16:(oi + 1) * 16].unsqueeze(2).to_broadcast([128, 16, 128]),
                         in1=e2[:, h, :].unsqueeze(1).to_broadcast([128, 16, 128]), op=ALU.mult,
                         R=["e1", "e2"], W=[f"Eg{i}"])
                    egf = Eg[i][:].rearrange("p a b -> p (a b)")
                    k.op("dve", "scalar_tensor_tensor", out=Fb[i][:], in0=egf, scalar=th[:, h:h + 1], in1=egf,
                         op0=ALU.is_ge, op1=ALU.mult, R=[f"Eg{i}", "th"], W=[f"Fb{i}"])
                    for blk in range(4):
                        k.op("pe", "matmul", out=pG[ig][:, blk * 512:(blk + 1) * 512], lhsT=ident[:],
                             rhs=Fb[i][:, blk * 512:(blk + 1) * 512], start=(h == 0), stop=(h == 7),
                             R=[f"Fb{i}", "ident"], W=[f"pG{ig}"], inc=(blk == 3))
                gbi = (t * 4 + oi // 2) % 2
                k.op("act", "copy", out=Gb[gbi][:, (oi % 2) * 2048:(oi % 2 + 1) * 2048], in_=pG[ig][:],
                     R=[f"pG{ig}"], W=[f"Gb{gbi}"])
                if oi % 2 == 1:
                    qi = oi // 2
                    k.dma(G[t * 128:(t + 1) * 128, qi * 4096:(qi + 1) * 4096], Gb[gbi][:], R=[f"Gb{gbi}"], W=["D:G"])
        k.end_phase()
        if stop_after < 6:
            return nc
        k.begin_phase()
        ident = k.sb("ident_sb", [128, 128], BF16)
        k.dma(ident[:], idn, W=["ident"])
        h2b = k.sb("h2b", [128, 32, 384], BF16)
        acc = k.sb("acc", [128, 3, D])
        UTs = [k.sb(f"UTs{i}", [128, 32, 256], BF16) for i in range(2)]
        Vs = [k.sb(f"Vs{i}", [128, 2, D], BF16) for i in range(2)]
        Gs = [k.sb(f"Gs{i}", [128, 3, 256], BF16) for i in range(2)]
        Wg = [k.sb(f"Wg{i}", [128, 256]) for i in range(2)]
        Wb = [k.sb(f"Wb{i}", [128, 256], BF16) for i in range(2)]
        WT = [k.sb(f"WT{i}", [128, 2, 128], BF16) for i in range(2)]
        xs = [k.sb(f"xs{i}", [128, 512]) for i in range(2)]
        g2s = [k.sb(f"g2s{i}", [128, 512]) for i in range(2)]
        tm = [k.sb(f"tm{i}", [128, 512]) for i in range(2)]
        pA = [k.ps(f"pA{i}", [128, 512]) for i in range(2)]
        pT = k.ps("pT", [128, 1024], BF16)
        po = [k.ps(f"po{i}", [128, 1024]) for i in range(2)]
        nA = npo = ne = 0
        for tb0 in range(0, NT, 3):
            tiles = list(range(tb0, min(tb0 + 3, NT)))
            nt_ = len(tiles)
            r0, r1 = tb0 * 128, (tb0 + nt_) * 128
            k.dma(h2b[:, :, :nt_ * 128], h2T[:, r0:r1].rearrange("(a p) t -> p a t", p=128), R=["D:h2T"], W=["h2b"])
            for eg in range(neg):
                j = eg % 2
                k.dma(UTs[j][:], UTb[eg], W=[f"UTs{j}"])
                k.dma(Vs[j][:], Vb[eg * 256:(eg + 1) * 256, :].rearrange("(c p) d -> p c d", p=128), W=[f"Vs{j}"])
                k.dma(Gs[j][:, :nt_, :], G[r0:r1, eg * 256:(eg + 1) * 256].rearrange("(n p) c -> p n c", p=128),
                      R=["D:G"], W=[f"Gs{j}"])
                for ti in range(nt_):
                    i = nA % 2
                    nA += 1
                    for kk in range(32):
                        k.op("pe", "matmul", out=pA[i][:, 0:256], lhsT=h2b[:, kk, ti * 128:(ti + 1) * 128],
                             rhs=UTs[j][:, kk, :], start=(kk == 0), stop=(kk == 31), R=["h2b", f"UTs{j}"],
                             W=[f"pA{i}"], inc=(kk == 31))
                    k.op("act", "activation", out=Wg[i][:], in_=pA[i][:, 0:256], func=AF.Gelu_apprx_tanh,
                         R=[f"pA{i}"], W=[f"Wg{i}"])
                    if cut < 2:
                        continue
                    k.op("pool", "tensor_tensor", out=Wb[i][:], in0=Wg[i][:], in1=Gs[j][:, ti, :], op=ALU.mult,
                         R=[f"Wg{i}", f"Gs{j}"], W=[f"Wb{i}"])
                    if cut < 3:
                        continue
                    for c in range(2):
                        k.op("pe", "transpose", out=pT[:, c * 128:(c + 1) * 128], in_=Wb[i][:, c * 128:(c + 1) * 128],
                             identity=ident[:], R=[f"Wb{i}", "ident"], W=["pT"], inc=(c == 1))
                    k.op("act", "copy", out=WT[i][:], in_=pT[:, 0:256].rearrange("p (c t) -> p c t", c=2),
                         R=["pT"], W=[f"WT{i}"])
                    if cut < 4:
                        continue
                    for ch in range(4):
                        ip = npo % 2
                        npo += 1
                        for db in range(2):
                            for c in range(2):
                                d0 = ch * 1024 + db * 512
                                k.op("pe", "matmul", out=po[ip][:, db * 512:(db + 1) * 512], lhsT=WT[i][:, c, :],
                                     rhs=Vs[j][:, c, d0:d0 + 512], start=(c == 0), stop=(c == 1),
                                     R=[f"WT{i}", f"Vs{j}"], W=[f"po{ip}"], inc=(db == 1 and c == 1))
                        a_ = acc[:, ti, ch * 1024:(ch + 1) * 1024]
                        if cut < 5:
                            continue
                        if eg == 0:
                            k.op("dve", "tensor_copy", out=a_, in_=po[ip][:], R=[f"po{ip}"], W=["acc"])
                        else:
                            k.op("dve", "tensor_tensor", out=a_, in0=po[ip][:], in1=a_, op=ALU.add,
                                 R=[f"po{ip}", "acc"], W=["acc"])
            for ti, t in enumerate(tiles):
                for db in range(8):
                    i = ne % 2
                    ne += 1
                    c0 = db * 512
                    k.dma(xs[i][:], x1[t * 128:(t + 1) * 128, c0:c0 + 512], R=["D:x1"], W=[f"xs{i}"])
                    k.dma(g2s[i][:], mv[7 + typ(t):8 + typ(t), c0:c0 + 512].partition_broadcast(128), W=[f"g2s{i}"])
                    k.op("dve", "tensor_tensor", out=tm[i][:], in0=acc[:, ti, c0:c0 + 512], in1=g2s[i][:], op=ALU.mult,
                         R=["acc", f"g2s{i}"], W=[f"tm{i}"])
                    k.op("pool", "tensor_tensor", out=tm[i][:], in0=tm[i][:], in1=xs[i][:], op=ALU.add,
                         R=[f"tm{i}", f"xs{i}"], W=[f"tm{i}"])
                    k.dma(x2[t * 128:(t + 1) * 128, c0:c0 + 512], tm[i][:], R=[f"tm{i}"], W=["D:x2"],
                          is_output=(not final))
        k.end_phase()
        if final:
            k.begin_phase()
            fg = k.sb("fg", [128, D])
            k.dma(fg[:], mv[9:10, :].partition_broadcast(128), W=["fg"])
            xt = [k.sb(f"xt{i}", [128, D]) for i in range(2)]
            jk = k.sb("jk", [128, D])
            ss = [k.sb(f"ss{i}", [128, 2]) for i in range(2)]
            for t in range(NT):
                i = t % 2
                k.dma(xt[i][:], x2[t * 128:(t + 1) * 128, :], R=["D:x2"], W=[f"xt{i}"])
                k.op("act", "activation", out=jk[:], in_=xt[i][:], func=AF.Square, accum_out=ss[i][:, 0:1],
                     R=[f"xt{i}"], W=["jk", f"ss{i}"])
                k.op("dve", "tensor_scalar", out=ss[i][:, 1:2], in0=ss[i][:, 0:1], scalar1=1.0 / D, scalar2=EPS,
                     op0=ALU.mult, op1=ALU.add, R=[f"ss{i}"], W=[f"ss{i}"])
                k.op("act", "activation", out=ss[i][:, 1:2], in_=ss[i][:, 1:2], func=AF.Sqrt, R=[f"ss{i}"], W=[f"ss{i}"])
                k.op("dve", "reciprocal", out=ss[i][:, 1:2], in_=ss[i][:, 1:2], R=[f"ss{i}"], W=[f"ss{i}"])
                k.op("dve", "scalar_tensor_tensor", out=xt[i][:], in0=xt[i][:], scalar=ss[i][:, 1:2], in1=fg[:],
                     op0=ALU.mult, op1=ALU.mult, R=[f"xt{i}", f"ss{i}", "fg"], W=[f"xt{i}"])
                k.dma(xo[t * 128:(t + 1) * 128, :], xt[i][:], R=[f"xt{i}"], W=["D:xo"], is_output=True)
            k.end_phase()
    return nc


def build_Db(NT=9, ctx_tiles=1, final=False, neg=16, first=True, last=True):
    R = NT * 128
    nc = new_nc()
    h2T = din(nc, "h2T", [D, R], BF16)
    G = din(nc, "G", [R, neg * 256], BF16)
    UTb = din(nc, "UTb", [neg, 128, 32, 256], BF16)
    Vb = din(nc, "Vb", [neg * 256, D], BF16)
    idn = din(nc, "ident", [128, 128], BF16)
    acc_in = None if first else din(nc, "acc_in", [R, D])
    if last:
        x1 = din(nc, "x1", [R, D])
        mv = din(nc, "mv", [10, D])
        xo = dout(nc, "xo", [R, D])
        x2 = dscr(nc, "x2", [R, D]) if final else xo
    else:
        acc_out = dout(nc, "acc_out", [R, D])
    typ = lambda t: 0 if t < ctx_tiles else 1
    with ExitStack() as st:
        k = KB(nc, st)
        k.begin_phase()
        ident = k.sb("ident_sb", [128, 128], BF16)
        k.dma(ident[:], idn, W=["ident"])
        h2b = k.sb("h2b", [128, 32, 384], BF16)
        acc = k.sb("acc", [128, 3, D])
        UTs = [k.sb(f"UTs{i}", [128, 32, 256], BF16) for i in range(2)]
        Vs = [k.sb(f"Vs{i}", [128, 2, D], BF16) for i in range(2)]
        Gs = [k.sb(f"Gs{i}", [128, 3, 256], BF16) for i in range(2)]
        Wg = [k.sb(f"Wg{i}", [128, 256]) for i in range(2)]
        Wb = [k.sb(f"Wb{i}", [128, 256], BF16) for i in range(2)]
        WT = [k.sb(f"WT{i}", [128, 2, 128], BF16) for i in range(2)]
        xs = [k.sb(f"xs{i}", [128, 512]) for i in range(2)]
        g2s = [k.sb(f"g2s{i}", [128, 512]) for i in range(2)]
        tm = [k.sb(f"tm{i}", [128, 512]) for i in range(2)]
        pA = [k.ps(f"pA{i}", [128, 512]) for i in range(2)]
        pT = k.ps("pT", [128, 1024], BF16)
        po = [k.ps(f"po{i}", [128, 1024]) for i in range(2)]
        nA = npo = ne = 0
        for tb0 in range(0, NT, 3):
            tiles = list(range(tb0, min(tb0 + 3, NT)))
            nt_ = len(tiles)
            r0, r1 = tb0 * 128, (tb0 + nt_) * 128
            k.dma(h2b[:, :, :nt_ * 128], h2T[:, r0:r1].rearrange("(a p) t -> p a t", p=128), W=["h2b"])
            if not first:
                k.dma(acc[:, :nt_, :], acc_in[r0:r1, :].rearrange("(n p) d -> p n d", p=128), W=["acc"])
            units = [(eg, ti) for eg in range(neg) for ti in range(nt_)]

            def S1(eg, ti):
                nonlocal nA
                j = eg % 2
                if ti == 0:
                    k.dma(UTs[j][:], UTb[eg], W=[f"UTs{j}"])
                    k.dma(Vs[j][:], Vb[eg * 256:(eg + 1) * 256, :].rearrange("(c p) d -> p c d", p=128), W=[f"Vs{j}"])
                    k.dma(Gs[j][:, :nt_, :], G[r0:r1, eg * 256:(eg + 1) * 256].rearrange("(n p) c -> p n c", p=128),
                          W=[f"Gs{j}"])
                i = nA % 2
                nA += 1
                for kk in range(32):
                    k.op("pe", "matmul", out=pA[i][:, 0:256], lhsT=h2b[:, kk, ti * 128:(ti + 1) * 128],
                         rhs=UTs[j][:, kk, :], start=(kk == 0), stop=(kk == 31), R=["h2b", f"UTs{j}"],
                         W=[f"pA{i}"], inc=(kk == 31))
                k.op("act", "activation", out=Wg[i][:], in_=pA[i][:, 0:256], func=AF.Gelu_apprx_tanh,
                     R=[f"pA{i}"], W=[f"Wg{i}"])
                k.op("pool", "tensor_tensor", out=Wb[i][:], in0=Wg[i][:], in1=Gs[j][:, ti, :], op=ALU.mult,
                     R=[f"Wg{i}", f"Gs{j}"], W=[f"Wb{i}"])
                return i

            def S2(eg, ti, i):
                nonlocal npo
                j = eg % 2
                for c in range(2):
                    k.op("pe", "transpose", out=pT[:, c * 128:(c + 1) * 128], in_=Wb[i][:, c * 128:(c + 1) * 128],
                         identity=ident[:], R=[f"Wb{i}", "ident"], W=["pT"], inc=(c == 1))
                k.op("act", "copy", out=WT[i][:], in_=pT[:, 0:256].rearrange("p (c t) -> p c t", c=2),
                     R=["pT"], W=[f"WT{i}"])
                for ch in range(4):
                    ip = npo % 2
                    npo += 1
                    for db in range(2):
                        for c in range(2):
                            d0 = ch * 1024 + db * 512
                            k.op("pe", "matmul", out=po[ip][:, db * 512:(db + 1) * 512], lhsT=WT[i][:, c, :],
                                 rhs=Vs[j][:, c, d0:d0 + 512], start=(c == 0), stop=(c == 1),
                                 R=[f"WT{i}", f"Vs{j}"], W=[f"po{ip}"], inc=(db == 1 and c == 1))
                    a_ = acc[:, ti, ch * 1024:(ch + 1) * 1024]
                    if eg == 0 and first:
                        k.op("dve", "tensor_copy", out=a_, in_=po[ip][:], R=[f"po{ip}"], W=["acc"])
                    else:
                        k.op("dve", "tensor_tensor", out=a_, in0=po[ip][:], in1=a_, op=ALU.add,
                             R=[f"po{ip}", "acc"], W=["acc"])

            pend = S1(*units[0])
            for ui, (eg, ti) in enumerate(units):
                nxt = S1(*units[ui + 1]) if ui + 1 < len(units) else None
                S2(eg, ti, pend)
                pend = nxt
            if not last:
                k.dma(acc_out[r0:r1, :].rearrange("(n p) d -> p n d", p=128), acc[:, :nt_, :], R=["acc"],
                      W=["D:acc_out"], is_output=True)
                continue
            for ti, t in enumerate(tiles):
                for db in range(8):
                    i = ne % 2
                    ne += 1
                    c0 = db * 512
                    k.dma(xs[i][:], x1[t * 128:(t + 1) * 128, c0:c0 + 512], W=[f"xs{i}"])
                    k.dma(g2s[i][:], mv[7 + typ(t):8 + typ(t), c0:c0 + 512].partition_broadcast(128), W=[f"g2s{i}"])
                    k.op("dve", "tensor_tensor", out=tm[i][:], in0=acc[:, ti, c0:c0 + 512], in1=g2s[i][:], op=ALU.mult,
                         R=["acc", f"g2s{i}"], W=[f"tm{i}"])
                    k.op("pool", "tensor_tensor", out=tm[i][:], in0=tm[i][:], in1=xs[i][:], op=ALU.add,
                         R=[f"tm{i}", f"xs{i}"], W=[f"tm{i}"])
                    k.dma(x2[t * 128:(t + 1) * 128, c0:c0 + 512], tm[i][:], R=[f"tm{i}"], W=["D:x2"],
                          is_output=(not final))
        k.end_phase()
        if last and final:
            k.begin_phase()
            fg = k.sb("fg", [128, D])
            k.dma(fg[:], mv[9:10, :].partition_broadcast(128), W=["fg"])
            xt = [k.sb(f"xt{i}", [128, D]) for i in range(2)]
            jk = k.sb("jk", [128, D])
            ss = [k.sb(f"ss{i}", [128, 2]) for i in range(2)]
            for t in range(NT):
                i = t % 2
                k.dma(xt[i][:], x2[t * 128:(t + 1) * 128, :], R=["D:x2"], W=[f"xt{i}"])
                k.op("act", "activation", out=jk[:], in_=xt[i][:], func=AF.Square, accum_out=ss[i][:, 0:1],
                     R=[f"xt{i}"], W=["jk", f"ss{i}"])
                k.op("dve", "tensor_scalar", out=ss[i][:, 1:2], in0=ss[i][:, 0:1], scalar1=1.0 / D, scalar2=EPS,
                     op0=ALU.mult, op1=ALU.add, R=[f"ss{i}"], W=[f"ss{i}"])
                k.op("act", "activation", out=ss[i][:, 1:2], in_=ss[i][:, 1:2], func=AF.Sqrt, R=[f"ss{i}"], W=[f"ss{i}"])
                k.op("dve", "reciprocal", out=ss[i][:, 1:2], in_=ss[i][:, 1:2], R=[f"ss{i}"], W=[f"ss{i}"])
                k.op("dve", "scalar_tensor_tensor", out=xt[i][:], in0=xt[i][:], scalar=ss[i][:, 1:2], in1=fg[:],
                     op0=ALU.mult, op1=ALU.mult, R=[f"xt{i}", f"ss{i}", "fg"], W=[f"xt{i}"])
                k.dma(xo[t * 128:(t + 1) * 128, :], xt[i][:], R=[f"xt{i}"], W=["D:xo"], is_output=True)
            k.end_phase()
    return nc


def build_E(NEG=8):
    nc = new_nc()
    U = din(nc, "U", [NEG * 256, D])
    V = din(nc, "V", [NEG * 256, D])
    idf = din(nc, "identf", [128, 128])
    UTb = dout(nc, "UTb", [NEG, 128, 32, 256], BF16)
    Vb = dout(nc, "Vb", [NEG * 256, D], BF16)
    with ExitStack() as st:
        k = KB(nc, st)
        k.begin_phase()
        ident = k.sb("identf_sb", [128, 128])
        k.dma(ident[:], idf, W=["ident"])
        ut = [k.sb(f"ut{i}", [128, D]) for i in range(3)]
        vb = [k.sb(f"vb{i}", [128, D], BF16) for i in range(2)]
        uo = [k.sb(f"uo{i}", [128, 32, 256], BF16) for i in range(2)]
        pt = [k.ps(f"pt{i}", [128, 512]) for i in range(4)]
        nu = nv = npt = 0
        for g in range(NEG):
            io = g % 2
            for c in range(2):
                iu = nu % 3
                nu += 1
                r0 = g * 256 + c * 128
                k.dma(ut[iu][:], U[r0:r0 + 128, :], W=[f"ut{iu}"])
                for k4 in range(8):
                    ip = npt % 4
                    npt += 1
                    for q in range(4):
                        kk = k4 * 4 + q
                        k.op("pe", "transpose", out=pt[ip][:, q * 128:(q + 1) * 128], in_=ut[iu][:, kk * 128:(kk + 1) * 128],
                             identity=ident[:], R=[f"ut{iu}", "ident"], W=[f"pt{ip}"], inc=(q == 3))
                    src = pt[ip][:].rearrange("p (a b) -> p a b", a=4)
                    dst = uo[io][:, k4 * 4:(k4 + 1) * 4, c * 128:(c + 1) * 128]
                    if k4 % 2 == 0:
                        k.op("act", "copy", out=dst, in_=src, R=[f"pt{ip}"], W=[f"uo{io}"])
                    else:
                        k.op("dve", "tensor_copy", out=dst, in_=src, R=[f"pt{ip}"], W=[f"uo{io}"])
                iu2 = nu % 3
                nu += 1
                iv = nv % 2
                nv += 1
                k.dma(ut[iu2][:], V[r0:r0 + 128, :], W=[f"ut{iu2}"])
                k.op("pool", "tensor_copy", out=vb[iv][:], in_=ut[iu2][:], R=[f"ut{iu2}"], W=[f"vb{iv}"])
                k.dma(Vb[r0:r0 + 128, :], vb[iv][:], R=[f"vb{iv}"], W=["D:Vb"], is_output=True)
            k.dma(UTb[g], uo[io][:], R=[f"uo{io}"], W=["D:UTb"], is_output=True)
        k.end_phase()
    return nc


def build_mod(NL=2, NC=3072):
    nc = bass.Bass("TRN2", target_bir_lowering=False)
    cT = nc.dram_tensor("cT", [128, 32, 3], F32, kind="ExternalInput").ap()
    w = nc.dram_tensor("w", [NL, 4096, NC], F32, kind="ExternalInput").ap()
    b = nc.dram_tensor("b", [NL, 1, NC], F32, kind="ExternalInput").ap()
    m = nc.dram_tensor("m", [NL, 3, NC], F32, kind="ExternalOutput").ap()
    with ExitStack() as st:
        k = KB(nc, st)
        c_sb = k.sb("c_sb", [128, 32, 3])
        s_sb = k.sb("s_sb", [128, 32, 3])
        wb = [k.sb(f"wb{i}", [128, 32, 512]) for i in range(2)]
        bias = k.sb("bias", [3, NL, NC])
        o_sb = [k.sb(f"o{i}", [3, 512]) for i in range(2)]
        ps = [k.ps(f"ps{i}", [128, 512]) for i in range(2)]
        k.dma(c_sb[:], cT, W=["c"])
        for l in range(NL):
            k.dma(bias[:, l, :], b[l].partition_broadcast(3), W=["bias"])
        k.op("act", "activation", out=s_sb[:], in_=c_sb[:], func=AF.Silu, R=["c"], W=["s"])
        it = 0
        for l in range(NL):
            for cb in range(NC // 512):
                j = it % 2
                k.dma(wb[j][:], w[l, :, cb * 512:(cb + 1) * 512].rearrange("(k p) n -> p k n", p=128),
                      W=[f"wb{j}"])
                for kk in range(32):
                    k.op("pe", "matmul", out=ps[j][0:3, :], lhsT=s_sb[:, kk, :], rhs=wb[j][:, kk, :],
                         start=(kk == 0), stop=(kk == 31),
                         R=["s", f"wb{j}"], W=[f"ps{j}"], inc=(kk == 31))
                k.op("dve", "tensor_tensor", out=o_sb[j][:], in0=ps[j][0:3, :],
                     in1=bias[:, l, cb * 512:(cb + 1) * 512], op=ALU.add,
                     R=[f"ps{j}", "bias"], W=[f"o{j}"])
                k.dma(m[l, :, cb * 512:(cb + 1) * 512], o_sb[j][:], R=[f"o{j}"], W=["m"], is_output=True)
                it += 1
        k.finish()
        print("instrs", k.ninstr)
    return nc


_PROGS = {}


def _prog(key, fn):
    if key not in _PROGS:
        _PROGS[key] = fn()
    return _PROGS[key]


def _run(nc, in_maps):
    return run_bass_kernel_spmd(nc, in_maps, core_ids=list(range(8))).results


def _c(a):
    return np.ascontiguousarray(a)


def kernel(x, c, ctx, c_ctx, mod_w, mod_b, norm_g, ev_w_in, ev_gate_b, ev_qk_g, ev_hnorm_g, ev_w_out,
           od_w_in, od_rpb, od_w_out, peer_wq, peer_subkeys, peer_u, peer_v, final_g):
    f32 = np.float32
    x, c, ctx, c_ctx = (np.asarray(a, f32) for a in (x, c, ctx, c_ctx))
    mod_w, mod_b, norm_g = (np.asarray(a, f32) for a in (mod_w, mod_b, norm_g))
    ev_w_in, ev_gate_b, ev_qk_g, ev_hnorm_g, ev_w_out = (np.asarray(a, f32) for a in
                                                         (ev_w_in, ev_gate_b, ev_qk_g, ev_hnorm_g, ev_w_out))
    od_w_in, od_rpb, od_w_out = (np.asarray(a, f32) for a in (od_w_in, od_rpb, od_w_out))
    peer_wq, peer_subkeys, peer_u, peer_v, final_g = (np.asarray(a, f32) for a in
                                                      (peer_wq, peer_subkeys, peer_u, peer_v, final_g))
    ident = np.eye(128, dtype=f32).astype(BF)
    identf = np.eye(128, dtype=f32)
    cst = np.zeros((128, 5, 128), f32)
    s_, t_ = np.meshgrid(np.arange(128), np.arange(128), indexing="ij")
    cst[:, 0, :] = (s_ <= t_)
    cst[:, 1, :] = (s_ >= t_)
    cst[:, 2, :] = 1.0
    tt = np.arange(4096)
    inv = (10000.0 ** (-np.arange(32, dtype=f32) / 32)).astype(f32)
    ang = np.concatenate([(tt // 64).astype(f32)[:, None] * inv, (tt % 64).astype(f32)[:, None] * inv], -1)
    rope = np.concatenate([np.cos(ang), np.sin(ang)], -1).astype(f32)

    cvec = np.stack([c[0], c[1], c_ctx])
    cT = _c(cvec.reshape(3, 32, 128).transpose(2, 1, 0))
    ncA = _prog("A", lambda: build_mod(2, 3072))
    rA = _run(ncA, [{"cT": cT, "w": _c(mod_w[:, :, i * 3072:(i + 1) * 3072]),
                     "b": _c(mod_b[:, None, i * 3072:(i + 1) * 3072])} for i in range(8)])
    mod = np.concatenate([r["m"] for r in rA], axis=-1).reshape(2, 3, 6, 4096)

    ncE = _prog("E", lambda: build_E(8))
    UTb, Vbf = [], []
    for l in range(2):
        rE = _run(ncE, [{"U": _c(peer_u[l, i * 2048:(i + 1) * 2048]), "V": _c(peer_v[l, i * 2048:(i + 1) * 2048]),
                         "identf": identf} for i in range(8)])
        UTb.append(np.concatenate([r["UTb"] for r in rE], axis=0))
        Vbf.append(np.concatenate([r["Vb"] for r in rE], axis=0))

    xl = x
    xc = ctx
    zpad = np.zeros((64, 4096), f32)
    out = np.empty((2, 4096, 4096), f32)
    for layer in range(2):
        last = layer == 1
        sh1, sc1, g1, sh2, sc2, g2 = (mod[layer, :, i] for i in range(6))
        ncB = _prog("B", lambda: build_B(9))
        rows = {}
        maps = []
        for b in range(2):
            for q in range(4):
                r_ = np.concatenate([xc[b, 64 * q:64 * q + 64], zpad, xl[b, 1024 * q:1024 * (q + 1)]], axis=0)
                rows[(b, q)] = r_
                mv5 = np.stack([norm_g[layer, 0], sc1[2], sh1[2], sc1[b], sh1[b]])
                maps.append({"x": r_, "mv": mv5, "ident": ident})
        rB = _run(ncB, maps)
        hTb = []
        for b in range(2):
            parts = [rB[b * 4 + q]["hT"][:, 0:64] for q in range(4)] + [rB[b * 4 + q]["hT"][:, 128:] for q in range(4)]
            hTb.append(_c(np.concatenate(parts, axis=1)))
        if layer == 0:
            ncC = _prog("C0", lambda: build_C0(2, 32))
            w = ev_w_in[0]
            maps = []
            for b in range(2):
                for g in range(4):
                    gcols = [9216 + o + 2 * g + hh for o in (0, 8, 16, 24) for hh in (0, 1)]
                    Wc = np.concatenate([w[:, 512 * g:512 * g + 512], w[:, 2048 + 128 * g:2048 + 128 * g + 128],
                                         w[:, 2560 + 128 * g:2560 + 128 * g + 128],
                                         w[:, 3072 + 256 * g:3072 + 256 * g + 256],
                                         w[:, 4096 + 256 * g:4096 + 256 * g + 256],
                                         w[:, 5120 + 512 * g:5120 + 512 * g + 512],
                                         w[:, 7168 + 512 * g:7168 + 512 * g + 512], w[:, gcols]], axis=1)
                    gbv = ev_gate_b[0][[o + 2 * g + hh for o in (0, 8, 16, 24) for hh in (0, 1)]][None]
                    maps.append({"hT": hTb[b], "W": _c(Wc), "cst": cst, "ident": ident, "qkg": _c(ev_qk_g[0]),
                                 "gb": _c(gbv), "hng": _c(ev_hnorm_g[0].reshape(8, 256)[2 * g:2 * g + 2]),
                                 "rope": rope})
            rC = _run(ncC, maps)
            yTb = []
            for b in range(2):
                y = np.empty((4096, 4352), BF)
                for g in range(4):
                    p = rC[b * 4 + g]["yT"]
                    y[512 * g:512 * g + 512] = p[0:512]
                    y[2048 + 512 * g:2048 + 512 * g + 512] = p[512:1024]
                yTb.append(y)
        else:
            ncC = _prog("C1", lambda: build_C1(2, 32, 8))
            w = od_w_in[0]
            maps = []
            for b in range(2):
                for g in range(4):
                    Wc = np.concatenate([w[:, 1024 * g:1024 * (g + 1)], w[:, 4096 + 1024 * g:4096 + 1024 * (g + 1)],
                                         w[:, 8192 + 1024 * g:8192 + 1024 * (g + 1)]], axis=1)
                    Bt = np.stack([na_bias_table(od_rpb[0][8 * g + h], 64).reshape(128, -1) for h in range(8)])
                    maps.append({"hT": hTb[b], "W": _c(Wc), "ident": ident, "Bt": _c(Bt)})
            rC = _run(ncC, maps)
            yTb = []
            for b in range(2):
                y = np.empty((4096, 4352), BF)
                y[:, :256] = 0
                for g in range(4):
                    y[1024 * g:1024 * (g + 1), 256:] = rC[b * 4 + g]["yT"]
                yTb.append(y)
        NT = 8 if last else 9
        CTXT = 0 if last else 1
        w_out = (od_w_out if last else ev_w_out)[0]
        skT = _c(peer_subkeys[layer].transpose(2, 0, 1))
        ncDa = _prog(("Da", NT), lambda: build_D(NT, CTXT, part="a"))
        maps = []
        mvs = {}
        for b in range(2):
            for q in range(4):
                lat = yTb[b][:, 256 + 1024 * q:256 + 1024 * (q + 1)]
                if last:
                    xr = rows[(b, q)][128:]
                    yc = _c(lat)
                else:
                    xr = rows[(b, q)]
                    yc = _c(np.concatenate([yTb[b][:, 64 * q:64 * q + 64], np.zeros((4096, 64), BF), lat], axis=1))
                mv10 = np.stack([g1[2], g1[b], norm_g[layer, 1], sc2[2], sh2[2], sc2[b], sh2[b], g2[2], g2[b], final_g])
                mvs[(b, q)] = mv10
                maps.append({"x": _c(xr), "yT": yc, "wout": w_out, "mv": mv10, "wq": peer_wq[layer], "skT": skT,
                             "ident": ident})
        rDa = _run(ncDa, maps)
        acc = [None] * 8
        for ch in range(4):
            first, lastc = ch == 0, ch == 3
            ncDb = _prog(("Db", NT, first, lastc), lambda: build_Db(NT, CTXT, last, 16, first, lastc))
            maps = []
            for ci in range(8):
                m = {"h2T": rDa[ci]["h2T"], "G": _c(rDa[ci]["G"][:, ch * 4096:(ch + 1) * 4096]),
                     "UTb": _c(UTb[layer][ch * 16:(ch + 1) * 16]), "Vb": _c(Vbf[layer][ch * 4096:(ch + 1) * 4096]),
                     "ident": ident}
                if not first:
                    m["acc_in"] = acc[ci]
                if lastc:
                    m["x1"] = rDa[ci]["x1"]
                    m["mv"] = mvs[(ci // 4, ci % 4)]
                maps.append(m)
            rDb = _run(ncDb, maps)
            if not lastc:
                acc = [r["acc_out"] for r in rDb]
        if last:
            for b in range(2):
                for q in range(4):
                    out[b, 1024 * q:1024 * (q + 1)] = rDb[b * 4 + q]["xo"]
        else:
            xl = np.empty_like(x)
            xc = np.empty_like(ctx)
            for b in range(2):
                for q in range(4):
                    r_ = rDb[b * 4 + q]["xo"]
                    xc[b, 64 * q:64 * q + 64] = r_[0:64]
                    xl[b, 1024 * q:1024 * (q + 1)] = r_[128:]
    return out
```

```python
import numpy as np
import concourse.bass as bass
import concourse.mybir as mybir

F32 = mybir.dt.float32
BF16 = mybir.dt.bfloat16
AF = mybir.ActivationFunctionType
ALU = mybir.AluOpType
AX = mybir.AxisListType

SEM_LIMIT = 30000
import os
N_DMA_SEMS = int(os.environ.get("NDS", "48"))


class KB:
    def __init__(self, nc, stack):
        self.nc = nc
        self.stack = stack
        self.sem_stack = stack
        self.phase_stack = None
        self.eng = {"pe": nc.tensor, "dve": nc.vector, "act": nc.scalar,
                    "pool": nc.gpsimd, "sp": nc.sync}
        self.cnt = {}
        self.semkey = {}
        self.sems = {}
        self.nsem = 0
        for e in ("pe", "dve", "act", "pool"):
            self._new_epoch(e, 0)
        self.epoch = {e: 0 for e in ("pe", "dve", "act", "pool")}
        self.known = {e: {} for e in self.eng}
        self.last_w = {}
        self.readers = {}
        self.dma_sems = []
        for i in range(N_DMA_SEMS):
            s = self.sem_stack.enter_context(nc.semaphore(f"dq{i}"))
            key = ("dma", i)
            self.sems[key] = s
            self.dma_sems.append(key)
        self.dma_uses = {k: 0 for k in self.dma_sems}
        self.dma_rr = 0
        self.ninstr = 0
        self.out_tokens = []
        self.prog = {e: [] for e in self.eng}
        self.psum_keys = set()

    def _new_epoch(self, e, ep):
        key = (e, ep)
        s = self.sem_stack.enter_context(self.nc.semaphore(f"c_{e}_{ep}"))
        self.sems[key] = s
        self.semkey[e] = key
        self.cnt[e] = 0

    def _deps(self, R, W):
        deps = set()
        for b in R:
            t = self.last_w.get(b)
            if t is not None:
                deps.add(t)
        for b in W:
            t = self.last_w.get(b)
            if t is not None:
                deps.add(t)
            for t in self.readers.get(b, ()):
                deps.add(t)
        return deps

    def _record(self, tok, R, W):
        for b in R:
            self.readers.setdefault(b, []).append(tok)
        for b in W:
            self.last_w[b] = tok
            self.readers[b] = []

    def _emit_waits(self, x, deps):
        kn = self.known[x]
        best = {}
        for (key, val) in deps:
            if kn.get(key, 0) >= val:
                continue
            if key == self.semkey.get(x) and val > self.cnt[x]:
                continue
            if best.get(key, 0) < val:
                best[key] = val
        for key, val in best.items():
            self.prog[x].append(("w", self.sems[key], val))
            kn[key] = val
            self.ninstr += 1

    def op(self, x, meth, R=(), W=(), inc=True, **kw):
        import os
        if x == "pool" and os.environ.get("NOPOOL"):
            x = "dve"
        fn = (lambda e, meth=meth, kw=kw: getattr(e, meth)(**kw))
        pk = [b for b in R if b in self.psum_keys]
        if pk:
            R = [b for b in R if b not in self.psum_keys]
            W = list(W) + pk
        deps = self._deps(R, W)
        self._emit_waits(x, deps)
        if self.cnt[x] + 1 > SEM_LIMIT:
            self.epoch[x] += 1
            self._new_epoch(x, self.epoch[x])
        self.ninstr += 1
        key = self.semkey[x]
        if inc:
            self.cnt[x] += 1
            self.prog[x].append(("o", fn, self.sems[key], 1))
            tok = (key, self.cnt[x])
        else:
            self.prog[x].append(("o", fn, None, 0))
            tok = (key, self.cnt[x] + 1)
        self._record(tok, R, W)
        return tok

    def dma(self, out, in_, R=(), W=(), q="sp", is_output=False, **kw):
        key = self.dma_sems[self.dma_rr % N_DMA_SEMS]
        self.dma_rr += 1
        deps = self._deps(R, W)
        prev = self.dma_uses[key]
        if prev:
            deps.add((key, 16 * prev))
        self._emit_waits(q, deps)
        self.prog[q].append(("o", (lambda e, out=out, in_=in_, kw=kw: e.dma_start(out=out, in_=in_, **kw)),
                             self.sems[key], 16))
        self.ninstr += 1
        self.dma_uses[key] = prev + 1
        tok = (key, 16 * (prev + 1))
        self._record(tok, R, W)
        if is_output:
            self.out_tokens.append(tok)
        return tok

    def finish(self, q="sp"):
        self._emit_waits(q, set(self.out_tokens))
        deps = set()
        for e in ("pe", "dve", "act", "pool"):
            if self.cnt[e] > 0:
                deps.add((self.semkey[e], self.cnt[e]))
        for key, n in self.dma_uses.items():
            if n:
                deps.add((key, 16 * n))
        self._emit_waits(q, deps)
        self.emit()

    def begin_phase(self):
        from contextlib import ExitStack
        ps = ExitStack()
        ps.__enter__()
        if not hasattr(self, "_stk"):
            self._stk = []
        self._stk.append(self.stack)
        self.stack = ps

    def end_phase(self, q="sp"):
        deps = set()
        for key, n in self.dma_uses.items():
            if n:
                deps.add((key, 16 * n))
        for e in ("pe", "dve", "act", "pool"):
            if self.cnt[e] > 0:
                deps.add((self.semkey[e], self.cnt[e]))
        self._emit_waits(q, deps)
        self.emit()
        self.prog = {e: [] for e in self.eng}
        ps = self.stack
        self.stack = self._stk.pop()
        ps.__exit__(None, None, None)
        self.last_w = {b: t for b, t in self.last_w.items() if isinstance(b, str) and b.startswith("D:")}
        self.readers = {b: t for b, t in self.readers.items() if isinstance(b, str) and b.startswith("D:")}

    def emit(self):
        def runner(lst):
            def f(eng):
                for it in lst:
                    if it[0] == "w":
                        eng.wait_ge(it[1], it[2])
                    else:
                        ins = it[1](eng)
                        if it[2] is not None:
                            ins.then_inc(it[2], it[3])
            return f
        with self.nc.Block() as block:
            block.sync(runner(self.prog["sp"]))
            block.tensor(runner(self.prog["pe"]))
            block.vector(runner(self.prog["dve"]))
            block.scalar(runner(self.prog["act"]))
            block.gpsimd(runner(self.prog["pool"]))

    def sb(self, name, shape, dtype=F32):
        self.nsem += 1
        return self.stack.enter_context(self.nc.sbuf_tensor(f"{name}_{self.nsem}", list(shape), dtype))

    def ps(self, name, shape, dtype=F32):
        self.nsem += 1
        self.psum_keys.add(name)
        return self.stack.enter_context(self.nc.psum_tensor(f"{name}_{self.nsem}", list(shape), dtype))


import numpy as np
from contextlib import ExitStack
import concourse.bass as bass
import concourse.mybir as mybir
from concourse.bass_utils import run_bass_kernel_spmd
import ml_dtypes

BF = ml_dtypes.bfloat16
EPS = 1e-6
D = 4096


def new_nc():
    return bass.Bass("TRN2", target_bir_lowering=False)


def din(nc, name, shape, dt=F32):
    return nc.dram_tensor(name, list(shape), dt, kind="ExternalInput").ap()


def dout(nc, name, shape, dt=F32):
    return nc.dram_tensor(name, list(shape), dt, kind="ExternalOutput").ap()


def dscr(nc, name, shape, dt=F32):
    return nc.dram_tensor(name, list(shape), dt, kind="Internal").ap()


class NormT:
    def __init__(self, k, ident, nbuf=2, pfx="n"):
        self.k = k
        self.ident = ident
        self.pfx = pfx
        self.xt = [k.sb(f"{pfx}xt{i}", [128, D]) for i in range(nbuf)]
        self.tmp = k.sb(f"{pfx}tmp", [128, D])
        self.hb = [k.sb(f"{pfx}hb{i}", [128, D], BF16) for i in range(nbuf)]
        self.ss = [k.sb(f"{pfx}ss{i}", [128, 2]) for i in range(nbuf)]
        self.pst = [k.ps(f"{pfx}pst{i}", [128, 1024], BF16) for i in range(2)]
        self.n = 0
        self.nps = 0

    def tile(self, x_rows, xkey_R, scale_bc, shift_bc, scale_key, hT, hT_key):
        k = self.k
        i = self.n % len(self.xt)
        self.n += 1
        p = self.pfx
        xt, hb, ss = self.xt[i], self.hb[i], self.ss[i]
        k.dma(xt[:], x_rows, R=xkey_R, W=[f"{p}xt{i}"])
        k.op("act", "activation", out=self.tmp[:], in_=xt[:], func=AF.Square, accum_out=ss[:, 0:1],
             R=[f"{p}xt{i}"], W=[f"{p}tmp", f"{p}ss{i}"])
        k.op("dve", "tensor_scalar", out=ss[:, 1:2], in0=ss[:, 0:1], scalar1=1.0 / D, scalar2=EPS,
             op0=ALU.mult, op1=ALU.add, R=[f"{p}ss{i}"], W=[f"{p}ss{i}"])
        k.op("act", "activation", out=ss[:, 1:2], in_=ss[:, 1:2], func=AF.Sqrt, R=[f"{p}ss{i}"], W=[f"{p}ss{i}"])
        k.op("dve", "reciprocal", out=ss[:, 1:2], in_=ss[:, 1:2], R=[f"{p}ss{i}"], W=[f"{p}ss{i}"])
        k.op("dve", "scalar_tensor_tensor", out=self.tmp[:], in0=xt[:], scalar=ss[:, 1:2], in1=scale_bc,
             op0=ALU.mult, op1=ALU.mult, R=[f"{p}xt{i}", f"{p}ss{i}", scale_key], W=[f"{p}tmp"])
        k.op("pool", "tensor_tensor", out=hb[:], in0=self.tmp[:], in1=shift_bc, op=ALU.add,
             R=[f"{p}tmp", scale_key], W=[f"{p}hb{i}"])
        for b in range(4):
            j = self.nps % 2
            self.nps += 1
            for q in range(8):
                kk = b * 8 + q
                k.op("pe", "transpose", out=self.pst[j][:, q * 128:(q + 1) * 128],
                     in_=hb[:, kk * 128:(kk + 1) * 128], identity=self.ident,
                     R=[f"{p}hb{i}", "ident"], W=[f"{p}pst{j}"], inc=(q == 7))
            eng = "act" if b % 2 == 0 else "dve"
            meth = "copy" if eng == "act" else "tensor_copy"
            k.op(eng, meth, out=hT[:, b * 8:(b + 1) * 8, :],
                 in_=self.pst[j][:].rearrange("p (a b) -> p a b", a=8),
                 R=[f"{p}pst{j}"], W=[hT_key])


def load_bc(k, dst, vec_ap, key):
    k.dma(dst, vec_ap.partition_broadcast(128), W=[key])


def build_B(NT=9):
    nc = new_nc()
    x = din(nc, "x", [NT * 128, D])
    mv = din(nc, "mv", [5, D])
    idn = din(nc, "ident", [128, 128], BF16)
    hT = dout(nc, "hT", [D, NT * 128], BF16)
    with ExitStack() as st:
        k = KB(nc, st)
        ident = k.sb("ident_sb", [128, 128], BF16)
        k.dma(ident[:], idn, W=["ident"])
        mods = emit_mods(k, mv)
        nt = NormT(k, ident[:])
        hts = [k.sb(f"hT{i}", [128, 32, 128], BF16) for i in range(2)]
        for t in range(NT):
            sc, sh = mods[0] if t == 0 else mods[1]
            j = t % 2
            nt.tile(x[t * 128:(t + 1) * 128, :], [], sc[:], sh[:], "mods", hts[j], f"hT{j}")
            k.dma(hT[:, t * 128:(t + 1) * 128].rearrange("(a p) t -> p a t", p=128), hts[j][:],
                  R=[f"hT{j}"], W=["hTout"], is_output=True)
        k.finish()
    return nc


def emit_mods(k, mv, pfx="m"):
    g = k.sb(pfx + "g", [128, D])
    tiles = [k.sb(f"{pfx}{i}", [128, D]) for i in range(4)]
    load_bc(k, g[:], mv[0:1, :], "mods_g")
    for i in range(4):
        load_bc(k, tiles[i][:], mv[1 + i:2 + i, :], f"mods_r{i}")
    for i in (0, 2):
        k.op("dve", "scalar_tensor_tensor", out=tiles[i][:], in0=tiles[i][:], scalar=1.0, in1=g[:],
             op0=ALU.add, op1=ALU.mult, R=["mods_g", f"mods_r{i}"], W=[f"mods_r{i}", "mods"])
    k.op("dve", "tensor_copy", out=g[:, 0:1], in_=tiles[1][:, 0:1], R=["mods_r1", "mods_r3", "mods_g"], W=["mods", "mods_g"])
    return [(tiles[0], tiles[1]), (tiles[2], tiles[3])]


def ref_B(x, mv):
    xf = x.astype(np.float64)
    y = xf / np.sqrt((xf * xf).mean(-1, keepdims=True) + EPS) * mv[0]
    out = np.empty_like(y)
    out[:128] = y[:128] * (1 + mv[1]) + mv[2]
    out[128:] = y[128:] * (1 + mv[3]) + mv[4]
    return out.T


C_Q, C_K, C_V, C_QB, C_KB, C_VB, C_OB, C_GT, C_N = 0, 512, 640, 768, 1024, 1280, 1792, 2304, 2312
ATT_SCALE = 128 ** -0.5


def emit_linear_tm(k, hT, W, out, T, N, out_key, pfx="l"):
    wbs = [k.sb(f"{pfx}wb{i}", [128, 32, 512], BF16) for i in range(2)]
    stg = [k.sb(f"{pfx}stg{i}", [128, 4, 512]) for i in range(3)]
    hb = [k.sb(f"{pfx}hb{i}", [128, 32, 512], BF16) for i in range(2)]
    osb = [k.sb(f"{pfx}o{i}", [128, 512]) for i in range(3)]
    ps = [k.ps(f"{pfx}ps{i}", [128, 512]) for i in range(3)]
    ns = nh = no = 0
    blocks = [(c0, min(512, N - c0)) for c0 in range(0, N, 512)]

    def load_block(bi):
        nonlocal ns
        c0, nw = blocks[bi]
        wb = wbs[bi % 2]
        for q in range(8):
            j = ns % 3
            ns += 1
            k.dma(stg[j][:, :, :nw], W[q * 512:(q + 1) * 512, c0:c0 + nw].rearrange("(a p) n -> p a n", p=128),
                  W=[f"{pfx}stg{j}"])
            if q % 2 == 0:
                k.op("dve", "tensor_copy", out=wb[:, q * 4:(q + 1) * 4, :nw], in_=stg[j][:, :, :nw],
                     R=[f"{pfx}stg{j}"], W=[f"{pfx}wb{bi % 2}"])
            else:
                k.op("act", "copy", out=wb[:, q * 4:(q + 1) * 4, :nw], in_=stg[j][:, :, :nw],
                     R=[f"{pfx}stg{j}"], W=[f"{pfx}wb{bi % 2}"])

    load_block(0)
    for bi, (c0, nw) in enumerate(blocks):
        wb = wbs[bi % 2]
        wkey = f"{pfx}wb{bi % 2}"
        first_tile = True
        for t0 in range(0, T, 512):
            tw = min(512, T - t0)
            jh = nh % 2
            nh += 1
            k.dma(hb[jh][:, :, :tw], hT[:, t0:t0 + tw].rearrange("(a p) t -> p a t", p=128), W=[f"{pfx}hb{jh}"])
            for tt in range(0, tw, 128):
                jo = no % 3
                no += 1
                for kk in range(32):
                    k.op("pe", "matmul", out=ps[jo][:, :nw], lhsT=hb[jh][:, kk, tt:tt + 128], rhs=wb[:, kk, :nw],
                         start=(kk == 0), stop=(kk == 31), R=[f"{pfx}hb{jh}", wkey], W=[f"{pfx}ps{jo}"],
                         inc=(kk == 31))
                if first_tile and bi + 1 < len(blocks):
                    load_block(bi + 1)
                    first_tile = False
                if jo % 2 == 0:
                    k.op("act", "copy", out=osb[jo][:, :nw], in_=ps[jo][:, :nw], R=[f"{pfx}ps{jo}"], W=[f"{pfx}o{jo}"])
                else:
                    k.op("dve", "tensor_copy", out=osb[jo][:, :nw], in_=ps[jo][:, :nw], R=[f"{pfx}ps{jo}"],
                         W=[f"{pfx}o{jo}"])
                k.dma(out[t0 + tt:t0 + tt + 128, c0:c0 + nw], osb[jo][:, :nw], R=[f"{pfx}o{jo}"], W=[out_key])


def build_C0(CT=2, LT=32, stop_after=3, part="all"):
    NTt = CT + LT
    T = NTt * 128
    nc = new_nc()
    hT = din(nc, "hT", [D, T], BF16)
    W = din(nc, "W", [D, C_N])
    cst = din(nc, "cst", [128, 5, 128])
    idn = din(nc, "ident", [128, 128], BF16)
    qkg = din(nc, "qkg", [2, 128])
    gb = din(nc, "gb", [1, 8])
    hng = din(nc, "hng", [2, 256])
    cs_t = din(nc, "rope", [LT * 128, 128])
    yT = dout(nc, "yT", [1024, T], BF16)
    if part == "a":
        Pm = dout(nc, "Pm", [T, C_N])
    elif part == "b":
        Pm = din(nc, "Pm", [T, C_N])
    else:
        Pm = dscr(nc, "Pm", [T, C_N])
    with ExitStack() as st:
        k = KB(nc, st)
        import os
        if part != "b":
            k.begin_phase()
            emit_linear_tm(k, hT, W, Pm, T, C_N, "D:Pm")
            k.end_phase()
        if stop_after < 1.5 or part == "a":
            return nc
        k.begin_phase()
        ident = k.sb("ident_sb", [128, 128], BF16)
        k.dma(ident[:], idn, W=["ident"])
        ones = k.sb("ones", [128, 128], BF16)
        k.op("dve", "memset", ap=ones[:], constant=1.0, W=["ones"])
        g5 = k.sb("g5", [128, 5, 128])
        for j in range(5):
            k.dma(g5[:, j, :], qkg[(0 if j < 4 else 1):(1 if j < 4 else 2), :].partition_broadcast(128), W=["g5"])
        QT = k.sb("QT", [128, 4, T], BF16)
        KT = k.sb("KT", [128, T], BF16)
        Vs = k.sb("Vs", [128, NTt, 128], BF16)
        xin = [k.sb(f"xin{i}", [128, 768]) for i in range(2)]
        cs = [k.sb(f"cs{i}", [128, 128]) for i in range(2)]
        sq = k.sb("sq", [128, 5, 128])
        xn = k.sb("xn", [128, 5, 128])
        ta = k.sb("ta", [128, 5, 64])
        tb = k.sb("tb", [128, 5, 64])
        xo = [k.sb(f"xo{i}", [128, 5, 128], BF16) for i in range(2)]
        st5 = k.sb("st5", [128, 10])
        pst = [k.ps(f"pst{i}", [128, 1024], BF16) for i in range(2)]
        for t in range(NTt):
            i = t % 2
            k.dma(xin[i][:], Pm[t * 128:(t + 1) * 128, 0:768], R=["D:Pm"], W=[f"xin{i}"])
            xv = xin[i][:, 0:640].rearrange("p (h d) -> p h d", h=5)
            if int(os.environ.get("PREP_CUT", "9")) < 1:
                continue
            k.op("pool", "tensor_tensor", out=sq[:], in0=xv, in1=xv, op=ALU.mult, R=[f"xin{i}"], W=["sq"])
            k.op("dve", "tensor_reduce", out=st5[:, 0:5], in_=sq[:], op=ALU.add, axis=AX.X, R=["sq"], W=["st5"])
            k.op("dve", "tensor_scalar", out=st5[:, 5:10], in0=st5[:, 0:5], scalar1=1.0 / 128, scalar2=EPS,
                 op0=ALU.mult, op1=ALU.add, R=["st5"], W=["st5"])
            k.op("act", "activation", out=st5[:, 5:10], in_=st5[:, 5:10], func=AF.Sqrt, R=["st5"], W=["st5"])
            k.op("dve", "reciprocal", out=st5[:, 5:10], in_=st5[:, 5:10], R=["st5"], W=["st5"])
            k.op("dve", "tensor_tensor", out=xn[:], in0=xv, in1=st5[:, 5:10].unsqueeze(2).to_broadcast([128, 5, 128]),
                 op=ALU.mult, R=[f"xin{i}", "st5"], W=["xn"])
            if t >= CT:
                k.dma(cs[i][:], cs_t[(t - CT) * 128:(t - CT + 1) * 128, :], W=[f"cs{i}"])
                k.op("pool", "tensor_tensor", out=xn[:], in0=xn[:], in1=g5[:], op=ALU.mult, R=["xn", "g5"], W=["xn"])
                cosb = cs[i][:, 0:64].unsqueeze(1).to_broadcast([128, 5, 64])
                sinb = cs[i][:, 64:128].unsqueeze(1).to_broadcast([128, 5, 64])
                x1, x2 = xn[:, :, 0:64], xn[:, :, 64:128]
                k.op("dve", "tensor_tensor", out=ta[:], in0=x1, in1=cosb, op=ALU.mult, R=["xn", f"cs{i}"], W=["ta"])
                k.op("dve", "tensor_tensor", out=tb[:], in0=x2, in1=sinb, op=ALU.mult, R=["xn", f"cs{i}"], W=["tb"])
                k.op("dve", "tensor_tensor", out=xo[i][:, :, 0:64], in0=ta[:], in1=tb[:], op=ALU.subtract,
                     R=["ta", "tb"], W=[f"xo{i}"])
                k.op("dve", "tensor_tensor", out=ta[:], in0=x1, in1=sinb, op=ALU.mult, R=["xn", f"cs{i}"], W=["ta"])
                k.op("dve", "tensor_tensor", out=tb[:], in0=x2, in1=cosb, op=ALU.mult, R=["xn", f"cs{i}"], W=["tb"])
                k.op("pool", "tensor_tensor", out=xo[i][:, :, 64:128], in0=ta[:], in1=tb[:], op=ALU.add,
                     R=["ta", "tb"], W=[f"xo{i}"])
            else:
                k.op("pool", "tensor_tensor", out=xo[i][:], in0=xn[:], in1=g5[:], op=ALU.mult, R=["xn", "g5"],
                     W=[f"xo{i}"])
            if int(os.environ.get("PREP_CUT", "9")) < 3:
                continue
            for j in range(5):
                k.op("pe", "transpose", out=pst[i][:, j * 128:(j + 1) * 128], in_=xo[i][:, j, :], identity=ident[:],
                     R=[f"xo{i}", "ident"], W=[f"pst{i}"], inc=(j == 4))
            if int(os.environ.get("PREP_CUT", "9")) < 5:
                continue
            k.op("act", "copy", out=QT[:, :, t * 128:(t + 1) * 128],
                 in_=pst[i][:, 0:512].rearrange("p (h d) -> p h d", h=4), R=[f"pst{i}"], W=["QT"])
            k.op("dve", "tensor_copy", out=KT[:, t * 128:(t + 1) * 128], in_=pst[i][:, 512:640], R=[f"pst{i}"], W=["KT"])
            k.op("act", "copy", out=Vs[:, t, :], in_=xin[i][:, 640:768], R=[f"xin{i}"], W=["Vs"])
        if stop_after < 1.7:
            k.end_phase()
            return nc
        E = [k.sb(f"E{i}", [128, 512], BF16) for i in range(3)]
        pS = [k.ps(f"pS{i}", [128, 512]) for i in range(2)]
        pO = [k.ps(f"pO{i}", [128, 512]) for i in range(2)]
        pD = [k.ps(f"pD{i}", [128, 512]) for i in range(2)]
        rc = k.sb("rc", [128, 512])
        yo = [k.sb(f"yo{i}", [128, 512], BF16) for i in range(2)]
        nE = nS = nb = 0
        for j in range(4):
            blocks = [(0, CT * 128, range(0, CT))]
            for q0 in range(CT * 128, T, 512):
                blocks.append((q0, min(512, T - q0), range(0, NTt)))
            for (q0, qw, kts) in blocks:
                ib = nb % 2
                nb += 1
                kts = list(kts)

                def SA(kt):
                    nonlocal nS, nE
                    iS = nS % 2
                    nS += 1
                    iE = nE % 3
                    nE += 1
                    k.op("pe", "matmul", out=pS[iS][:, :qw], lhsT=KT[:, kt * 128:(kt + 1) * 128],
                         rhs=QT[:, j, q0:q0 + qw], start=True, stop=True, R=["KT", "QT"], W=[f"pS{iS}"])
                    k.op("act", "activation", out=E[iE][:, :qw], in_=pS[iS][:, :qw], func=AF.Exp, scale=ATT_SCALE,
                         R=[f"pS{iS}"], W=[f"E{iE}"])
                    return iE

                pend = SA(kts[0])
                for n_, kt in enumerate(kts):
                    nxt = SA(kts[n_ + 1]) if n_ + 1 < len(kts) else None
                    iE = pend
                    last = (n_ == len(kts) - 1)
                    k.op("pe", "matmul", out=pO[ib][:, :qw], lhsT=Vs[:, kt, :], rhs=E[iE][:, :qw], start=(n_ == 0),
                         stop=last, R=["Vs", f"E{iE}"], W=[f"pO{ib}"], inc=last)
                    k.op("pe", "matmul", out=pD[ib][:, :qw], lhsT=ones[:], rhs=E[iE][:, :qw], start=(n_ == 0),
                         stop=last, R=["ones", f"E{iE}"], W=[f"pD{ib}"], inc=last)
                    pend = nxt
                k.op("dve", "reciprocal", out=rc[:, :qw], in_=pD[ib][:, :qw], R=[f"pD{ib}"], W=["rc"])
                k.op("dve", "tensor_tensor", out=yo[ib][:, :qw], in0=pO[ib][:, :qw], in1=rc[:, :qw], op=ALU.mult,
                     R=[f"pO{ib}", "rc"], W=[f"yo{ib}"])
                k.dma(yT[j * 128:(j + 1) * 128, q0:q0 + qw], yo[ib][:, :qw], R=[f"yo{ib}"], W=["D:yT"], is_output=True)
        k.end_phase()
        if stop_after < 3:
            return nc
        k.begin_phase()
        ident = k.sb("ident_sb", [128, 128], BF16)
        k.dma(ident[:], idn, W=["ident"])
        cst_sb = k.sb("cst_sb", [128, 5, 128])
        k.dma(cst_sb[:], cst, W=["cst"])
        hgb = k.sb("hgb", [128, 2, 256])
        for h in range(2):
            k.dma(hgb[:, h, :], hng[h:h + 1, :].partition_broadcast(128), W=["hgb"])
        gbb = k.sb("gbb", [128, 8])
        k.dma(gbb[:], gb.partition_broadcast(128), W=["gbb"])
        Gt = k.sb("Gt", [128, NTt, 8])
        k.dma(Gt[:], Pm[:, C_GT:C_GT + 8].rearrange("(n p) c -> p n c", p=128), R=["D:Pm"], W=["Gt"])
        k.op("dve", "tensor_tensor", out=Gt[:], in0=Gt[:], in1=gbb[:].unsqueeze(1).to_broadcast([128, NTt, 8]),
             op=ALU.add, R=["Gt", "gbb"], W=["Gt"])
        LF = k.sb("LF", [128, 2, NTt, 2])
        for d_ in range(2):
            zc = Gt[:, :, 2 + 4 * d_:4 + 4 * d_]
            k.op("act", "activation", out=LF[:, d_, :, :], in_=zc, func=AF.Exp, scale=-1.0, R=["Gt"], W=["LF"])
        k.op("act", "activation", out=LF[:], in_=LF[:], func=AF.Ln, bias=1.0, R=["LF"], W=["LF"])
        k.op("dve", "tensor_scalar", out=LF[:], in0=LF[:], scalar1=-1.0, scalar2=None, op0=ALU.mult, R=["LF"], W=["LF"])
        pg = k.ps("pg", [128, 512])
        NG = NTt * 2
        for d_ in range(2):
            rhs = LF[:, d_, :, :].rearrange("p n h -> p (n h)")
            k.op("pe", "matmul", out=pg[:, d_ * NG:(d_ + 1) * NG], lhsT=cst_sb[:, d_, :], rhs=rhs, start=True, stop=True,
                 R=["cst", "LF"], W=["pg"])
            k.op("pe", "matmul", out=pg[:, (2 + d_) * NG:(3 + d_) * NG], lhsT=cst_sb[:, 2, :], rhs=rhs, start=True,
                 stop=True, R=["cst", "LF"], W=["pg"])
        Aa = k.sb("Aa", [128, 2, NTt, 2])
        Cc = k.sb("Cc", [128, 2, NTt, 2])
        EB = k.sb("EB", [128, 2, NTt, 2])
        k.op("act", "activation", out=Aa[:].rearrange("p d n h -> p (d n h)"), in_=pg[:, 0:2 * NG], func=AF.Exp,
             R=["pg"], W=["Aa"])
        k.op("act", "activation", out=EB[:].rearrange("p d n h -> p (d n h)"), in_=pg[:, 2 * NG:4 * NG], func=AF.Exp,
             R=["pg"], W=["EB"])
        for d_ in range(2):
            k.op("dve", "tensor_tensor", out=Cc[:, d_, :, :], in0=Gt[:, :, 4 * d_:4 * d_ + 2],
                 in1=pg[:, d_ * NG:(d_ + 1) * NG].rearrange("p (n h) -> p n h", h=2), op=ALU.subtract,
                 R=["Gt", "pg"], W=["Cc"])
        lnsc = k.sb("lnsc", [128, 1])
        k.op("dve", "memset", ap=lnsc[:], constant=float(-0.5 * np.log(128.0)), W=["lnsc"])
        k.op("act", "activation", out=Cc[:], in_=Cc[:], func=AF.Exp, bias=lnsc[:, 0:1], R=["Cc", "lnsc"], W=["Cc"])
        stg = k.sb("stg", [128, NTt, 256])
        Hs = k.sb("Hs", [128, NTt, 256])
        hsc = [k.sb(f"hsc{i}", [128, 4]) for i in range(2)]
        pst = [k.ps(f"pst{i}", [128, 1024], BF16) for i in range(1)]
        pP = [k.ps(f"pP{i}", [128, 512]) for i in range(2)]
        pN = [k.ps(f"pN{i}", [128, 512]) for i in range(2)]
        pC = [k.ps(f"pC{i}", [128, 512]) for i in range(2)]
        npst = 0

        def transpose_all(src, dst, src_key, dst_key, ncol_tiles=1):
            nonlocal npst
            for a in range(ncol_tiles):
                for c0 in range(0, NTt, 8):
                    cn = min(8, NTt - c0)
                    i = npst % len(pst)
                    npst += 1
                    for c in range(cn):
                        k.op("pe", "transpose", out=pst[i][:, c * 128:(c + 1) * 128],
                             in_=src[:, c0 + c, a * 128:(a + 1) * 128], identity=ident[:],
                             R=[src_key, "ident"], W=[f"pst{i}"], inc=(c == cn - 1))
                    dv = dst[:, c0 * 128:(c0 + cn) * 128] if ncol_tiles == 1 else dst[:, a, c0 * 128:(c0 + cn) * 128]
                    if (c0 // 8) % 2 == 0:
                        k.op("act", "copy", out=dv, in_=pst[i][:, :cn * 128], R=[f"pst{i}"], W=[dst_key])
                    else:
                        k.op("dve", "tensor_copy", out=dv, in_=pst[i][:, :cn * 128], R=[f"pst{i}"], W=[dst_key])

        for hh in range(2):
            k.begin_phase()
            qb = k.sb("qb", [128, NTt, 128], BF16)
            kf = [k.sb(f"kf{d_}", [128, NTt, 128], BF16) for d_ in range(2)]
            qT = k.sb("qT", [128, T], BF16)
            kT = [k.sb(f"kT{d_}", [128, T], BF16) for d_ in range(2)]
            vt = k.sb("vt", [128, NTt, 257], BF16)
            Cst = [k.sb(f"Cst{i}", [128, 257]) for i in range(2)]
            Cb = [k.sb(f"Cb{i}", [128, 257], BF16) for i in range(2)]
            PTm = [k.sb(f"PTm{i}", [128, 128], BF16) for i in range(2)]
            k.dma(stg[:, :, 0:128], Pm[:, C_QB + 128 * hh:C_QB + 128 * hh + 128].rearrange("(n p) c -> p n c", p=128),
                  R=["D:Pm"], W=["stg"])
            k.op("act", "copy", out=qb[:], in_=stg[:, :, 0:128], R=["stg"], W=["qb"])
            transpose_all(qb, qT, "qb", "qT")
            k.dma(stg[:, :, 0:128], Pm[:, C_KB + 128 * hh:C_KB + 128 * hh + 128].rearrange("(n p) c -> p n c", p=128),
                  R=["D:Pm"], W=["stg"])
            for d_ in range(2):
                k.op("dve", "tensor_tensor", out=kf[d_][:], in0=stg[:, :, 0:128],
                     in1=Cc[:, d_, :, hh:hh + 1].to_broadcast([128, NTt, 128]), op=ALU.mult,
                     R=["stg", "Cc"], W=[f"kf{d_}"])
                transpose_all(kf[d_], kT[d_], f"kf{d_}", f"kT{d_}")
            k.dma(stg[:], Pm[:, C_VB + 256 * hh:C_VB + 256 * hh + 256].rearrange("(n p) c -> p n c", p=128),
                  R=["D:Pm"], W=["stg"])
            k.op("act", "copy", out=vt[:, :, 0:256], in_=stg[:], R=["stg"], W=["vt"])
            k.op("dve", "memset", ap=vt[:, :, 256:257], constant=1.0, W=["vt"])
            orders = [list(range(NTt)), list(range(CT - 1, -1, -1)) + list(range(NTt - 1, CT - 1, -1))]
            k.op("dve", "memset", ap=Hs[:], constant=0.0, W=[f"Hs{c}" for c in range(NTt)])
            for d_ in range(2):
                k.op("dve", "memset", ap=Cst[d_][:], constant=0.0, W=[f"Cst{d_}"])
                k.op("dve", "memset", ap=Cb[d_][:], constant=0.0, W=[f"Cb{d_}"])
            for step in range(NTt):
                for d_ in range(2):
                    c = orders[d_][step]
                    i = d_
                    cols = slice(c * 128, (c + 1) * 128)
                    k.op("pe", "matmul", out=pP[i][:, 0:128], lhsT=kT[d_][:, cols], rhs=qT[:, cols], start=True, stop=True,
                         R=[f"kT{d_}", "qT"], W=[f"pP{i}"])
                    k.op("dve", "tensor_tensor", out=PTm[i][:], in0=pP[i][:, 0:128], in1=cst_sb[:, d_, :], op=ALU.mult,
                         R=[f"pP{i}", "cst"], W=[f"PTm{i}"])
                    k.op("pe", "matmul", out=pN[i][:, 0:257], lhsT=qT[:, cols], rhs=Cb[d_][:], start=True, stop=False,
                         R=["qT", f"Cb{d_}"], W=[f"pN{i}"], inc=False)
                    k.op("pe", "matmul", out=pN[i][:, 0:257], lhsT=PTm[i][:], rhs=vt[:, c, :], start=False, stop=True,
                         R=[f"PTm{i}", "vt"], W=[f"pN{i}"])
                    k.op("pe", "matmul", out=pC[d_][:, 0:257], lhsT=kf[d_][:, c, :], rhs=vt[:, c, :], start=True, stop=True,
                         R=[f"kf{d_}", "vt"], W=[f"pC{d_}"])
                    eb = EB[:, d_, c, hh:hh + 1]
                    k.op("dve", "tensor_scalar", out=Cst[d_][:], in0=Cst[d_][:], scalar1=eb, scalar2=None, op0=ALU.mult,
                         R=[f"Cst{d_}", "EB"], W=[f"Cst{d_}"])
                    k.op("dve", "scalar_tensor_tensor", out=Cst[d_][:], in0=pC[d_][:, 0:257], scalar=eb, in1=Cst[d_][:],
                         op0=ALU.mult, op1=ALU.add, R=[f"pC{d_}", "EB", f"Cst{d_}"], W=[f"Cst{d_}"])
                    k.op("act", "copy", out=Cb[d_][:], in_=Cst[d_][:], R=[f"Cst{d_}"], W=[f"Cb{d_}"])
                    a_ = Aa[:, d_, c, hh:hh + 1]
                    hs_ = hsc[d_]
                    hk = f"hsc{d_}"
                    k.op("dve", "tensor_scalar", out=hs_[:, 0:1], in0=pN[i][:, 256:257], scalar1=a_, scalar2=None,
                         op0=ALU.mult, R=[f"pN{i}", "Aa"], W=[hk])
                    k.op("dve", "tensor_scalar", out=hs_[:, 1:2], in0=hs_[:, 0:1], scalar1=-1.0, scalar2=None,
                         op0=ALU.mult, R=[hk], W=[hk])
                    k.op("dve", "scalar_tensor_tensor", out=hs_[:, 1:2], in0=hs_[:, 1:2], scalar=1.0, in1=hs_[:, 0:1],
                         op0=ALU.max, op1=ALU.max, R=[hk], W=[hk])
                    k.op("dve", "reciprocal", out=hs_[:, 2:3], in_=hs_[:, 1:2], R=[hk], W=[hk])
                    k.op("dve", "tensor_tensor", out=hs_[:, 3:4], in0=hs_[:, 2:3], in1=a_, op=ALU.mult,
                         R=[hk, "Aa"], W=[hk])
                    k.op("dve", "scalar_tensor_tensor", out=Hs[:, c, :], in0=pN[i][:, 0:256], scalar=hs_[:, 3:4],
                         in1=Hs[:, c, :], op0=ALU.mult, op1=ALU.add, R=[f"pN{i}", hk, f"Hs{c}"], W=[f"Hs{c}"])
            k.end_phase()
            k.begin_phase()
            Yb = k.sb("Yb", [128, NTt, 256], BF16)
            yTo = k.sb("yTo", [128, 2, T], BF16)
            rst = k.sb("rst", [128, 2, NTt])
            HK = [f"Hs{c_}" for c_ in range(NTt)]
            k.dma(stg[:], Pm[:, C_OB + 256 * hh:C_OB + 256 * hh + 256].rearrange("(n p) c -> p n c", p=128),
                  R=["D:Pm"], W=["stg"])
            k.op("act", "activation", out=stg[:], in_=stg[:], func=AF.Sigmoid, R=["stg"], W=["stg"])
            k.op("pool", "tensor_tensor", out=Yb[:], in0=Hs[:], in1=Hs[:], op=ALU.mult, R=HK, W=["Yb"])
            k.op("dve", "tensor_reduce", out=rst[:, 0, :], in_=Yb[:], op=ALU.add, axis=AX.X, R=["Yb"], W=["rst"])
            k.op("dve", "tensor_scalar", out=rst[:, 1, :], in0=rst[:, 0, :], scalar1=1.0 / 256, scalar2=EPS,
                 op0=ALU.mult, op1=ALU.add, R=["rst"], W=["rst"])
            k.op("act", "activation", out=rst[:, 1, :], in_=rst[:, 1, :], func=AF.Sqrt, R=["rst"], W=["rst"])
            k.op("dve", "reciprocal", out=rst[:, 1, :], in_=rst[:, 1, :], R=["rst"], W=["rst"])
            k.op("dve", "tensor_tensor", out=Hs[:], in0=Hs[:], in1=rst[:, 1, :].unsqueeze(2).to_broadcast([128, NTt, 256]),
                 op=ALU.mult, R=HK + ["rst"], W=HK)
            k.op("dve", "tensor_tensor", out=Hs[:], in0=Hs[:], in1=hgb[:, hh, :].unsqueeze(1).to_broadcast([128, NTt, 256]),
                 op=ALU.mult, R=HK + ["hgb"], W=HK)
            k.op("dve", "tensor_tensor", out=Yb[:], in0=Hs[:], in1=stg[:], op=ALU.mult, R=HK + ["stg"], W=["Yb"])
            transpose_all(Yb, yTo, "Yb", "yTo", ncol_tiles=2)
            for a in range(2):
                k.dma(yT[512 + hh * 256 + a * 128:512 + hh * 256 + (a + 1) * 128, :], yTo[:, a, :], R=["yTo"],
                      W=["D:yT"], is_output=True)
            k.end_phase()
        k.end_phase()
    return nc


NH1 = 8


def na_bias_table(rpb_h, rows):
    out = np.full((128, 8, 4, 64), -30000.0, np.float32)
    q = np.arange(64)
    cs = np.clip(q - 8, 0, 48)
    for di in range(8):
        for a in range(4):
            for rr2 in range(2):
                rr = 2 * a + rr2
                roff = 7 - di + rr
                for kc in range(64):
                    valid = (kc >= cs) & (kc < cs + 16)
                    coff = np.clip(kc - q + 15, 0, 30)
                    out[rr2 * 64 + kc, di, a, :] = np.where(valid, rpb_h[roff, coff], -30000.0)
    return out


def build_C1(CT=2, LT=32, nheads=NH1):
    NTt = CT + LT
    T = NTt * 128
    L0 = CT * 128
    rows = LT * 2
    NW = 3 * 128 * nheads
    nc = new_nc()
    hT = din(nc, "hT", [D, T], BF16)
    W = din(nc, "W", [D, NW])
    idn = din(nc, "ident", [128, 128], BF16)
    Bt = din(nc, "Bt", [nheads, 128, 8 * 4 * 64])
    yT = dout(nc, "yT", [nheads * 128, LT * 128], BF16)
    Pm = dscr(nc, "Pm", [T, NW])
    with ExitStack() as st:
        k = KB(nc, st)
        k.begin_phase()
        emit_linear_tm(k, hT, W, Pm, T, NW, "D:Pm")
        k.end_phase()
        k.begin_phase()
        ident = k.sb("ident_sb", [128, 128], BF16)
        k.dma(ident[:], idn, W=["ident"])
        ones = k.sb("ones", [128, 128], BF16)
        k.op("dve", "memset", ap=ones[:], constant=1.0, W=["ones"])
        stg = k.sb("stg", [128, NTt, 128])
        xb = k.sb("xb", [128, NTt, 128], BF16)
        QT = k.sb("QT", [128, T], BF16)
        KT = k.sb("KT", [128, T], BF16)
        Va = k.sb("Va", [128, NTt, 128], BF16)
        Vb = k.sb("Vb", [128, NTt, 128], BF16)
        Mt = k.sb("Mt", [128, 8, 256])
        E = [k.sb(f"E{i}", [128, 64 * (4 + CT)], BF16) for i in range(3)]
        rc = k.sb("rc", [128, 512])
        yo = [k.sb(f"yo{i}", [128, 512], BF16) for i in range(2)]
        pst = [k.ps(f"pst{i}", [128, 1024], BF16) for i in range(2)]
        pS = [k.ps(f"pS{i}", [128, 512]) for i in range(2)]
        pO = [k.ps(f"pO{i}", [128, 512]) for i in range(2)]
        pD = [k.ps(f"pD{i}", [128, 512]) for i in range(2)]
        npst = [0]

        def transpose_all(src, dst, src_key, dst_key):
            for c0 in range(0, NTt, 8):
                cn = min(8, NTt - c0)
                i = npst[0] % 2
                npst[0] += 1
                for c in range(cn):
                    k.op("pe", "transpose", out=pst[i][:, c * 128:(c + 1) * 128], in_=src[:, c0 + c, :],
                         identity=ident[:], R=[src_key, "ident"], W=[f"pst{i}"], inc=(c == cn - 1))
                dv = dst[:, c0 * 128:(c0 + cn) * 128]
                if (c0 // 8) % 2 == 0:
                    k.op("act", "copy", out=dv, in_=pst[i][:, :cn * 128], R=[f"pst{i}"], W=[dst_key])
                else:
                    k.op("dve", "tensor_copy", out=dv, in_=pst[i][:, :cn * 128], R=[f"pst{i}"], W=[dst_key])

        nE = nS = nG = 0
        NA_ = 4 + CT
        for h in range(nheads):
            for (col, dst, dkey) in ((h * 128, QT, "QT"), ((nheads + h) * 128, KT, "KT")):
                k.dma(stg[:], Pm[:, col:col + 128].rearrange("(n p) c -> p n c", p=128), R=["D:Pm"], W=["stg"])
                k.op("act", "copy", out=xb[:], in_=stg[:], R=["stg"], W=["xb"])
                transpose_all(xb, dst, "xb", dkey)
            vcol = (2 * nheads + h) * 128
            k.dma(stg[:], Pm[:, vcol:vcol + 128].rearrange("(n p) c -> p n c", p=128), R=["D:Pm"], W=["stg"])
            k.op("act", "copy", out=Va[:], in_=stg[:], R=["stg"], W=["Va"])
            k.dma(stg[:, 0:NTt - 1, :], Pm[64:64 + (NTt - 1) * 128, vcol:vcol + 128].rearrange("(n p) c -> p n c", p=128),
                  R=["D:Pm"], W=["stg"])
            k.op("dve", "tensor_copy", out=Vb[:, 0:NTt - 1, :], in_=stg[:, 0:NTt - 1, :], R=["stg"], W=["Vb"])
            k.dma(Mt[:].rearrange("p d c -> p (d c)"), Bt[h], W=["Mt"])
            k.op("act", "activation", out=Mt[:], in_=Mt[:], func=AF.Exp, R=["Mt"], W=["Mt"])
            def SA(r):
                nonlocal nS, nE
                r0 = min(max(r - 4, 0), rows - 8)
                di = r - r0
                iS = nS % 2
                nS += 1
                iE = nE % 3
                nE += 1
                qs = slice(L0 + 64 * r, L0 + 64 * r + 64)
                for a in range(NA_):
                    ks = (L0 + 64 * r0 + 128 * a) if a < 4 else 128 * (a - 4)
                    k.op("pe", "matmul", out=pS[iS][:, a * 64:(a + 1) * 64], lhsT=KT[:, ks:ks + 128], rhs=QT[:, qs],
                         start=True, stop=True, R=["KT", "QT"], W=[f"pS{iS}"], inc=(a == NA_ - 1))
                k.op("act", "activation", out=E[iE][:], in_=pS[iS][:, 0:64 * NA_], func=AF.Exp, scale=ATT_SCALE,
                     R=[f"pS{iS}"], W=[f"E{iE}"])
                k.op("dve", "tensor_tensor", out=E[iE][:, 0:256], in0=E[iE][:, 0:256], in1=Mt[:, di, :], op=ALU.mult,
                     R=[f"E{iE}", "Mt"], W=[f"E{iE}"])
                return iE

            pend = SA(0)
            for r in range(rows):
                nxt = SA(r + 1) if r + 1 < rows else None
                iE = pend
                pend = nxt
                r0 = min(max(r - 4, 0), rows - 8)
                g8, rg = r // 8, r % 8
                ig = nG % 2
                for a in range(NA_):
                    if a < 4:
                        t64 = CT * 2 + r0 + 2 * a
                        vt = Va[:, t64 // 2, :] if t64 % 2 == 0 else Vb[:, (t64 - 1) // 2, :]
                        vkey = "Va" if t64 % 2 == 0 else "Vb"
                    else:
                        vt, vkey = Va[:, a - 4, :], "Va"
                    last = (a == NA_ - 1)
                    k.op("pe", "matmul", out=pO[ig][:, rg * 64:(rg + 1) * 64], lhsT=vt, rhs=E[iE][:, a * 64:(a + 1) * 64],
                         start=(a == 0), stop=last, R=[vkey, f"E{iE}"], W=[f"pO{ig}"], inc=last)
                    k.op("pe", "matmul", out=pD[ig][:, rg * 64:(rg + 1) * 64], lhsT=ones[:], rhs=E[iE][:, a * 64:(a + 1) * 64],
                         start=(a == 0), stop=last, R=["ones", f"E{iE}"], W=[f"pD{ig}"], inc=last)
                if rg == 7:
                    nG += 1
                    k.op("dve", "reciprocal", out=rc[:], in_=pD[ig][:], R=[f"pD{ig}"], W=["rc"])
                    k.op("dve", "tensor_tensor", out=yo[ig][:], in0=pO[ig][:], in1=rc[:], op=ALU.mult,
                         R=[f"pO{ig}", "rc"], W=[f"yo{ig}"])
                    k.dma(yT[h * 128:(h + 1) * 128, g8 * 512:(g8 + 1) * 512], yo[ig][:], R=[f"yo{ig}"], W=["D:yT"],
                          is_output=True)
        k.end_phase()
    return nc


NEG_INF = -1.0e30


def build_D(NT=9, ctx_tiles=1, final=False, stop_after=9, neg=64, cut=9, part="all"):
    R = NT * 128
    if part == "a":
        stop_after = 5
    nc = new_nc()
    x = din(nc, "x", [R, D])
    yT = din(nc, "yT", [D, R], BF16)
    wout = din(nc, "wout", [D, D])
    mv = din(nc, "mv", [10, D])
    wq = din(nc, "wq", [D, 2048])
    skT = din(nc, "skT", [128, 2, 128])
    if stop_after >= 6:
        UTb = din(nc, "UTb", [neg, 128, 32, 256], BF16)
        Vb = din(nc, "Vb", [neg * 256, D], BF16)
    idn = din(nc, "ident", [128, 128], BF16)
    xo = dout(nc, "xo", [R, D]) if part != "a" else None
    if part == "a":
        stop_after = 5
        x1 = dout(nc, "x1", [R, D])
        h2T = dout(nc, "h2T", [D, R], BF16)
        G = dout(nc, "G", [R, 16384], BF16)
    else:
        x1 = dscr(nc, "x1", [R, D])
        h2T = dscr(nc, "h2T", [D, R], BF16)
        G = dscr(nc, "G", [R, 16384], BF16)
    S = dscr(nc, "S", [R, 2048])
    x2 = dscr(nc, "x2", [R, D]) if final else xo
    typ = lambda t: 0 if t < ctx_tiles else 1
    with ExitStack() as st:
        k = KB(nc, st)

        def load_wblock(wb, wkey, stg, Wd, c0, nw, ns):
            for q in range(8):
                j = ns[0] % 3
                ns[0] += 1
                k.dma(stg[j][:, :, :nw], Wd[q * 512:(q + 1) * 512, c0:c0 + nw].rearrange("(a p) n -> p a n", p=128),
                      W=[f"stg{j}"])
                if q % 2 == 0:
                    k.op("dve", "tensor_copy", out=wb[:, q * 4:(q + 1) * 4, :nw], in_=stg[j][:, :, :nw],
                         R=[f"stg{j}"], W=[wkey])
                else:
                    k.op("act", "copy", out=wb[:, q * 4:(q + 1) * 4, :nw], in_=stg[j][:, :, :nw],
                         R=[f"stg{j}"], W=[wkey])

        k.begin_phase()
        yTs = k.sb("yTs", [128, 32, R], BF16)
        k.dma(yTs[:], yT.rearrange("(a p) t -> p a t", p=128), W=["yTs"])
        wbs = [k.sb(f"wb{i}", [128, 32, 512], BF16) for i in range(2)]
        stg = [k.sb(f"stg{i}", [128, 4, 512]) for i in range(3)]
        g1s = k.sb("g1s", [128, 2, 512])
        xs = [k.sb(f"xs{i}", [128, 512]) for i in range(3)]
        tmp = [k.sb(f"tmp{i}", [128, 512]) for i in range(3)]
        ps = [k.ps(f"ps{i}", [128, 512]) for i in range(3)]
        ns = [0]
        n = 0
        load_wblock(wbs[0], "wb0", stg, wout, 0, 512, ns)
        for nb in range(8):
            c0 = nb * 512
            wb, wkey = wbs[nb % 2], f"wb{nb % 2}"
            for ty in range(2):
                k.dma(g1s[:, ty, :], mv[ty:ty + 1, c0:c0 + 512].partition_broadcast(128), W=["g1s"])
            for t in range(NT):
                i = n % 3
                n += 1
                for kk in range(32):
                    k.op("pe", "matmul", out=ps[i][:], lhsT=yTs[:, kk, t * 128:(t + 1) * 128], rhs=wb[:, kk, :],
                         start=(kk == 0), stop=(kk == 31), R=["yTs", wkey], W=[f"ps{i}"], inc=(kk == 31))
                if t == 0 and nb + 1 < 8:
                    load_wblock(wbs[(nb + 1) % 2], f"wb{(nb + 1) % 2}", stg, wout, c0 + 512, 512, ns)
                k.dma(xs[i][:], x[t * 128:(t + 1) * 128, c0:c0 + 512], W=[f"xs{i}"])
                k.op("dve", "tensor_tensor", out=tmp[i][:], in0=ps[i][:], in1=g1s[:, typ(t), :], op=ALU.mult,
                     R=[f"ps{i}", "g1s"], W=[f"tmp{i}"])
                k.op("pool", "tensor_tensor", out=tmp[i][:], in0=tmp[i][:], in1=xs[i][:], op=ALU.add,
                     R=[f"tmp{i}", f"xs{i}"], W=[f"tmp{i}"])
                k.dma(x1[t * 128:(t + 1) * 128, c0:c0 + 512], tmp[i][:], R=[f"tmp{i}"], W=["D:x1"])
        k.end_phase()
        if stop_after < 2:
            return nc
        k.begin_phase()
        ident = k.sb("ident_sb", [128, 128], BF16)
        k.dma(ident[:], idn, W=["ident"])
        mods = emit_mods(k, mv[2:7, :])
        ntm = NormT(k, ident[:])
        hts = [k.sb(f"hT{i}", [128, 32, 128], BF16) for i in range(2)]
        for t in range(NT):
            sc, sh = mods[typ(t)]
            j = t % 2
            ntm.tile(x1[t * 128:(t + 1) * 128, :], ["D:x1"], sc[:], sh[:], "mods", hts[j], f"hT{j}")
            k.dma(h2T[:, t * 128:(t + 1) * 128].rearrange("(a p) t -> p a t", p=128), hts[j][:],
                  R=[f"hT{j}"], W=["D:h2T"])
        k.end_phase()
        if stop_after < 3:
            return nc
        k.begin_phase()
        h2s = k.sb("h2s", [128, 32, R], BF16)
        k.dma(h2s[:], h2T.rearrange("(a p) t -> p a t", p=128), R=["D:h2T"], W=["h2s"])
        sk = k.sb("sk", [128, 2, 128])
        k.dma(sk[:], skT, W=["sk"])
        wbs = [k.sb(f"wb{i}", [128, 32, 512], BF16) for i in range(2)]
        stg = [k.sb(f"stg{i}", [128, 4, 512]) for i in range(3)]
        qn = k.sb("qn", [128, 4, R])
        so = [k.sb(f"so{i}", [128, 512]) for i in range(2)]
        ps = [k.ps(f"ps{i}", [128, 512]) for i in range(3)]
        p2 = [k.ps(f"p2{i}", [128, 512]) for i in range(2)]
        n = n2 = 0
        load_wblock(wbs[0], "wb0", stg, wq, 0, 512, ns)
        for nb in range(4):
            wb, wkey = wbs[nb % 2], f"wb{nb % 2}"
            for j in range(4):
                for t0 in range(0, R, 512):
                    tw = min(512, R - t0)
                    i = n % 3
                    n += 1
                    for kk in range(32):
                        k.op("pe", "matmul", out=ps[i][:, :tw], lhsT=wb[:, kk, j * 128:(j + 1) * 128],
                             rhs=h2s[:, kk, t0:t0 + tw], start=(kk == 0), stop=(kk == 31), R=[wkey, "h2s"],
                             W=[f"ps{i}"], inc=(kk == 31))
                    if j == 0 and t0 == 0 and nb + 1 < 4:
                        load_wblock(wbs[(nb + 1) % 2], f"wb{(nb + 1) % 2}", stg, wq, (nb + 1) * 512, 512, ns)
                    if n % 2 == 0:
                        k.op("act", "copy", out=qn[:, j, t0:t0 + tw], in_=ps[i][:, :tw], R=[f"ps{i}"], W=["qn"])
                    else:
                        k.op("dve", "tensor_copy", out=qn[:, j, t0:t0 + tw], in_=ps[i][:, :tw], R=[f"ps{i}"], W=["qn"])
            for t in range(NT):
                i = n2 % 2
                n2 += 1
                for j in range(4):
                    k.op("pe", "matmul", out=p2[i][:, j * 128:(j + 1) * 128], lhsT=qn[:, j, t * 128:(t + 1) * 128],
                         rhs=sk[:, j % 2, :], start=True, stop=True, R=["qn", "sk"], W=[f"p2{i}"], inc=(j == 3))
                k.op("dve", "tensor_copy", out=so[i][:], in_=p2[i][:], R=[f"p2{i}"], W=[f"so{i}"])
                k.dma(S[t * 128:(t + 1) * 128, nb * 512:(nb + 1) * 512], so[i][:], R=[f"so{i}"], W=["D:S"])
        k.end_phase()
        if stop_after < 4:
            return nc
        k.begin_phase()
        St = [k.sb(f"St{i}", [128, 16, 128]) for i in range(2)]
        V16 = k.sb("V16", [128, 16, 16])
        scr = k.sb("scr", [128, 256])
        cand = k.sb("cand", [128, 8, 256])
        T16 = k.sb("T16", [128, 8, 16])
        E16 = k.sb("E16", [128, 8, 16])
        zz = k.sb("zz", [128, 16])
        ident = k.sb("ident_sb", [128, 128], BF16)
        k.dma(ident[:], idn, W=["ident"])
        e1 = k.sb("e1", [128, 8, 128])
        e2 = k.sb("e2", [128, 8, 128])
        th = k.sb("th", [128, 8])
        Eg = [k.sb(f"Eg{i}", [128, 16, 128]) for i in range(2)]
        Fb = [k.sb(f"Fb{i}", [128, 2048], BF16) for i in range(2)]
        Gb = [k.sb(f"Gb{i}", [128, 4096], BF16) for i in range(2)]
        pG = [k.ps(f"pG{i}", [128, 2048]) for i in range(2)]
        nd = ngb = 0
        for t in range(NT):
            s_ = St[t % 2]
            sk_ = f"St{t % 2}"
            k.dma(s_[:], S[t * 128:(t + 1) * 128, :].rearrange("p (b c) -> p b c", b=16), R=["D:S"], W=[sk_])
            for b in range(16):
                k.op("dve", "max", out=V16[:, b, 0:8], in_=s_[:, b, :], R=[sk_], W=["V16"])
                k.op("dve", "match_replace", out=scr[:, 0:128], in_to_replace=V16[:, b, 0:8], in_values=s_[:, b, :],
                     imm_value=NEG_INF, R=[sk_, "V16"], W=["scr"])
                k.op("dve", "max", out=V16[:, b, 8:16], in_=scr[:, 0:128], R=["scr"], W=["V16"])
            Vv = V16[:].rearrange("p (h two) r -> p h two r", two=2)
            k.op("dve", "tensor_tensor", out=cand[:].rearrange("p h (a b) -> p h a b", a=16),
                 in0=Vv[:, :, 0, :].unsqueeze(3).to_broadcast([128, 8, 16, 16]),
                 in1=Vv[:, :, 1, :].unsqueeze(2).to_broadcast([128, 8, 16, 16]), op=ALU.add, R=["V16"], W=["cand"])
            for h in range(8):
                k.op("dve", "max", out=T16[:, h, 0:8], in_=cand[:, h, :], R=["cand"], W=["T16"])
                k.op("dve", "match_replace", out=scr[:], in_to_replace=T16[:, h, 0:8], in_values=cand[:, h, :],
                     imm_value=NEG_INF, R=["cand", "T16"], W=["scr"])
                k.op("dve", "max", out=T16[:, h, 8:16], in_=scr[:], R=["scr"], W=["T16"])
            k.op("act", "activation", out=E16[:], in_=T16[:], func=AF.Exp, R=["T16"], W=["E16"])
            k.op("dve", "tensor_reduce", out=zz[:, 0:8], in_=E16[:], op=ALU.add, axis=AX.X, R=["E16"], W=["zz"])
            k.op("act", "activation", out=zz[:, 8:16], in_=zz[:, 0:8], func=AF.Ln, R=["zz"], W=["zz"])
            k.op("dve", "tensor_scalar", out=zz[:, 8:16], in0=zz[:, 8:16], scalar1=-1.0, scalar2=None, op0=ALU.mult,
                 R=["zz"], W=["zz"])
            Sv = s_[:].rearrange("p (h two) c -> p h two c", two=2)
            for h in range(8):
                k.op("act", "activation", out=e1[:, h, :], in_=Sv[:, h, 0, :], func=AF.Exp, bias=zz[:, 8 + h:9 + h],
                     R=[sk_, "zz"], W=["e1"])
            k.op("act", "activation", out=e2[:], in_=Sv[:, :, 1, :], func=AF.Exp, R=[sk_], W=["e2"])
            k.op("dve", "scalar_tensor_tensor", out=th[:], in0=T16[:, :, 15], scalar=-1.0e-4, in1=zz[:, 8:16],
                 op0=ALU.add, op1=ALU.add, R=["T16", "zz"], W=["th"])
            k.op("act", "activation", out=th[:], in_=th[:], func=AF.Exp, R=["th"], W=["th"])
            for oi in range(8):
                ig = ngb % 2
                ngb += 1
                for h in range(8):
                    i = nd % 2
                    nd += 1
                    k.op("dve" if nd % 6 == 0 else "pool", "tensor_tensor", out=Eg[i][:],
                         in0=e1[:, h, oi * 16:(oi + 1) * 16].unsqueeze(2).to_broadcast([128, 16, 128]),
                         in1=e2[:, h, :].unsqueeze(1).to_broadcast([128, 16, 128]), op=ALU.mult,
                         R=["e1", "e2"], W=[f"Eg{i}"])
                    egf = Eg[i][:].rearrange("p a b -> p (a b)")
                    k.op("dve", "scalar_tensor_tensor", out=Fb[i][:], in0=egf, scalar=th[:, h:h + 1], in1=egf,
                         op0=ALU.is_ge, op1=ALU.mult, R=[f"Eg{i}", "th"], W=[f"Fb{i}"])
                    for blk in range(4):
                        k.op("pe", "matmul", out=pG[ig][:, blk * 512:(blk + 1) * 512], lhsT=ident[:],
                             rhs=Fb[i][:, blk * 512:(blk + 1) * 512], start=(h == 0), stop=(h == 7),
                             R=[f"Fb{i}", "ident"], W=[f"pG{ig}"], inc=(blk == 3))
                gbi = (t * 4 + oi // 2) % 2
                k.op("act", "copy", out=Gb[gbi][:, (oi % 2) * 2048:(oi % 2 + 1) * 2048], in_=pG[ig][:],
                     R=[f"pG{ig}"], W=[f"Gb{gbi}"])
                if oi % 2 == 1:
                    qi = oi // 2
                    k.dma(G[t * 128:(t + 1) * 128, qi * 4096:(qi + 1) * 4096], Gb[gbi][:], R=[f"Gb{gbi}"], W=["D:G"])
        k.end_phase()
        if stop_after < 6:
            return nc
        k.begin_phase()
        ident = k.sb("ident_sb", [128, 128], BF16)
        k.dma(ident[:], idn, W=["ident"])
        h2b = k.sb("h2b", [128, 32, 384], BF16)
        acc = k.sb("acc", [128, 3, D])
        UTs = [k.sb(f"UTs{i}", [128, 32, 256], BF16) for i in range(2)]
        Vs = [k.sb(f"Vs{i}", [128, 2, D], BF16) for i in range(2)]
        Gs = [k.sb(f"Gs{i}", [128, 3, 256], BF16) for i in range(2)]
        Wg = [k.sb(f"Wg{i}", [128, 256]) for i in range(2)]
        Wb = [k.sb(f"Wb{i}", [128, 256], BF16) for i in range(2)]
        WT = [k.sb(f"WT{i}", [128, 2, 128], BF16) for i in range(2)]
        xs = [k.sb(f"xs{i}", [128, 512]) for i in range(2)]
        g2s = [k.sb(f"g2s{i}", [128, 512]) for i in range(2)]
        tm = [k.sb(f"tm{i}", [128, 512]) for i in range(2)]
        pA = [k.ps(f"pA{i}", [128, 512]) for i in range(2)]
        pT = k.ps("pT", [128, 1024], BF16)
        po = [k.ps(f"po{i}", [128, 1024]) for i in range(2)]
        nA = npo = ne = 0
        for tb0 in range(0, NT, 3):
            tiles = list(range(tb0, min(tb0 + 3, NT)))
            nt_ = len(tiles)
            r0, r1 = tb0 * 128, (tb0 + nt_) * 128
            k.dma(h2b[:, :, :nt_ * 128], h2T[:, r0:r1].rearrange("(a p) t -> p a t", p=128), R=["D:h2T"], W=["h2b"])
            for eg in range(neg):
                j = eg % 2
                k.dma(UTs[j][:], UTb[eg], W=[f"UTs{j}"])
                k.dma(Vs[j][:], Vb[eg * 256:(eg + 1) * 256, :].rearrange("(c p) d -> p c d", p=128), W=[f"Vs{j}"])
                k.dma(Gs[j][:, :nt_, :], G[r0:r1, eg * 256:(eg + 1) * 256].rearrange("(n p) c -> p n c", p=128),
                      R=["D:G"], W=[f"Gs{j}"])
                for ti in range(nt_):
                    i = nA % 2
                    nA += 1
                    for kk in range(32):
                        k.op("pe", "matmul", out=pA[i][:, 0:256], lhsT=h2b[:, kk, ti * 128:(ti + 1) * 128],
                             rhs=UTs[j][:, kk, :], start=(kk == 0), stop=(kk == 31), R=["h2b", f"UTs{j}"],
                             W=[f"pA{i}"], inc=(kk == 31))
                    k.op("act", "activation", out=Wg[i][:], in_=pA[i][:, 0:256], func=AF.Gelu_apprx_tanh,
                         R=[f"pA{i}"], W=[f"Wg{i}"])
                    if cut < 2:
                        continue
                    k.op("pool", "tensor_tensor", out=Wb[i][:], in0=Wg[i][:], in1=Gs[j][:, ti, :], op=ALU.mult,
                         R=[f"Wg{i}", f"Gs{j}"], W=[f"Wb{i}"])
                    if cut < 3:
                        continue
                    for c in range(2):
                        k.op("pe", "transpose", out=pT[:, c * 128:(c + 1) * 128], in_=Wb[i][:, c * 128:(c + 1) * 128],
                             identity=ident[:], R=[f"Wb{i}", "ident"], W=["pT"], inc=(c == 1))
                    k.op("act", "copy", out=WT[i][:], in_=pT[:, 0:256].rearrange("p (c t) -> p c t", c=2),
                         R=["pT"], W=[f"WT{i}"])
                    if cut < 4:
                        continue
                    for ch in range(4):
                        ip = npo % 2
                        npo += 1
                        for db in range(2):
                            for c in range(2):
                                d0 = ch * 1024 + db * 512
                                k.op("pe", "matmul", out=po[ip][:, db * 512:(db + 1) * 512], lhsT=WT[i][:, c, :],
                                     rhs=Vs[j][:, c, d0:d0 + 512], start=(c == 0), stop=(c == 1),
                                     R=[f"WT{i}", f"Vs{j}"], W=[f"po{ip}"], inc=(db == 1 and c == 1))
                        a_ = acc[:, ti, ch * 1024:(ch + 1) * 1024]
                        if cut < 5:
                            continue
                        if eg == 0:
                            k.op("dve", "tensor_copy", out=a_, in_=po[ip][:], R=[f"po{ip}"], W=["acc"])
                        else:
                            k.op("dve", "tensor_tensor", out=a_, in0=po[ip][:], in1=a_, op=ALU.add,
                                 R=[f"po{ip}", "acc"], W=["acc"])
            for ti, t in enumerate(tiles):
                for db in range(8):
                    i = ne % 2
                    ne += 1
                    c0 = db * 512
                    k.dma(xs[i][:], x1[t * 128:(t + 1) * 128, c0:c0 + 512], R=["D:x1"], W=[f"xs{i}"])
                    k.dma(g2s[i][:], mv[7 + typ(t):8 + typ(t), c0:c0 + 512].partition_broadcast(128), W=[f"g2s{i}"])
                    k.op("dve", "tensor_tensor", out=tm[i][:], in0=acc[:, ti, c0:c0 + 512], in1=g2s[i][:], op=ALU.mult,
                         R=["acc", f"g2s{i}"], W=[f"tm{i}"])
                    k.op("pool", "tensor_tensor", out=tm[i][:], in0=tm[i][:], in1=xs[i][:], op=ALU.add,
                         R=[f"tm{i}", f"xs{i}"], W=[f"tm{i}"])
                    k.dma(x2[t * 128:(t + 1) * 128, c0:c0 + 512], tm[i][:], R=[f"tm{i}"], W=["D:x2"],
                          is_output=(not final))
        k.end_phase()
        if final:
            k.begin_phase()
            fg = k.sb("fg", [128, D])
            k.dma(fg[:], mv[9:10, :].partition_broadcast(128), W=["fg"])
            xt = [k.sb(f"xt{i}", [128, D]) for i in range(2)]
            jk = k.sb("jk", [128, D])
            ss = [k.sb(f"ss{i}", [128, 2]) for i in range(2)]
            for t in range(NT):
                i = t % 2
                k.dma(xt[i][:], x2[t * 128:(t + 1) * 128, :], R=["D:x2"], W=[f"xt{i}"])
                k.op("act", "activation", out=jk[:], in_=xt[i][:], func=AF.Square, accum_out=ss[i][:, 0:1],
                     R=[f"xt{i}"], W=["jk", f"ss{i}"])
                k.op("dve", "tensor_scalar", out=ss[i][:, 1:2], in0=ss[i][:, 0:1], scalar1=1.0 / D, scalar2=EPS,
                     op0=ALU.mult, op1=ALU.add, R=[f"ss{i}"], W=[f"ss{i}"])
                k.op("act", "activation", out=ss[i][:, 1:2], in_=ss[i][:, 1:2], func=AF.Sqrt, R=[f"ss{i}"], W=[f"ss{i}"])
                k.op("dve", "reciprocal", out=ss[i][:, 1:2], in_=ss[i][:, 1:2], R=[f"ss{i}"], W=[f"ss{i}"])
                k.op("dve", "scalar_tensor_tensor", out=xt[i][:], in0=xt[i][:], scalar=ss[i][:, 1:2], in1=fg[:],
                     op0=ALU.mult, op1=ALU.mult, R=[f"xt{i}", f"ss{i}", "fg"], W=[f"xt{i}"])
                k.dma(xo[t * 128:(t + 1) * 128, :], xt[i][:], R=[f"xt{i}"], W=["D:xo"], is_output=True)
            k.end_phase()
    return nc


def build_Db(NT=9, ctx_tiles=1, final=False, neg=16, first=True, last=True):
    R = NT * 128
    nc = new_nc()
    h2T = din(nc, "h2T", [D, R], BF16)
    G = din(nc, "G", [R, neg * 256], BF16)
    UTb = din(nc, "UTb", [neg, 128, 32, 256], BF16)
    Vb = din(nc, "Vb", [neg * 256, D], BF16)
    idn = din(nc, "ident", [128, 128], BF16)
    acc_in = None if first else din(nc, "acc_in", [R, D])
    if last:
        x1 = din(nc, "x1", [R, D])
        mv = din(nc, "mv", [10, D])
        xo = dout(nc, "xo", [R, D])
        x2 = dscr(nc, "x2", [R, D]) if final else xo
    else:
        acc_out = dout(nc, "acc_out", [R, D])
    typ = lambda t: 0 if t < ctx_tiles else 1
    with ExitStack() as st:
        k = KB(nc, st)
        k.begin_phase()
        ident = k.sb("ident_sb", [128, 128], BF16)
        k.dma(ident[:], idn, W=["ident"])
        h2b = k.sb("h2b", [128, 32, 384], BF16)
        acc = k.sb("acc", [128, 3, D])
        UTs = [k.sb(f"UTs{i}", [128, 32, 256], BF16) for i in range(2)]
        Vs = [k.sb(f"Vs{i}", [128, 2, D], BF16) for i in range(2)]
        Gs = [k.sb(f"Gs{i}", [128, 3, 256], BF16) for i in range(2)]
        Wg = [k.sb(f"Wg{i}", [128, 256]) for i in range(2)]
        Wb = [k.sb(f"Wb{i}", [128, 256], BF16) for i in range(2)]
        WT = [k.sb(f"WT{i}", [128, 2, 128], BF16) for i in range(2)]
        xs = [k.sb(f"xs{i}", [128, 512]) for i in range(2)]
        g2s = [k.sb(f"g2s{i}", [128, 512]) for i in range(2)]
        tm = [k.sb(f"tm{i}", [128, 512]) for i in range(2)]
        pA = [k.ps(f"pA{i}", [128, 512]) for i in range(2)]
        pT = k.ps("pT", [128, 1024], BF16)
        po = [k.ps(f"po{i}", [128, 1024]) for i in range(2)]
        nA = npo = ne = 0
        for tb0 in range(0, NT, 3):
            tiles = list(range(tb0, min(tb0 + 3, NT)))
            nt_ = len(tiles)
            r0, r1 = tb0 * 128, (tb0 + nt_) * 128
            k.dma(h2b[:, :, :nt_ * 128], h2T[:, r0:r1].rearrange("(a p) t -> p a t", p=128), W=["h2b"])
            if not first:
                k.dma(acc[:, :nt_, :], acc_in[r0:r1, :].rearrange("(n p) d -> p n d", p=128), W=["acc"])
            units = [(eg, ti) for eg in range(neg) for ti in range(nt_)]

            def S1(eg, ti):
                nonlocal nA
                j = eg % 2
                if ti == 0:
                    k.dma(UTs[j][:], UTb[eg], W=[f"UTs{j}"])
                    k.dma(Vs[j][:], Vb[eg * 256:(eg + 1) * 256, :].rearrange("(c p) d -> p c d", p=128), W=[f"Vs{j}"])
                    k.dma(Gs[j][:, :nt_, :], G[r0:r1, eg * 256:(eg + 1) * 256].rearrange("(n p) c -> p n c", p=128),
                          W=[f"Gs{j}"])
                i = nA % 2
                nA += 1
                for kk in range(32):
                    k.op("pe", "matmul", out=pA[i][:, 0:256], lhsT=h2b[:, kk, ti * 128:(ti + 1) * 128],
                         rhs=UTs[j][:, kk, :], start=(kk == 0), stop=(kk == 31), R=["h2b", f"UTs{j}"],
                         W=[f"pA{i}"], inc=(kk == 31))
                k.op("act", "activation", out=Wg[i][:], in_=pA[i][:, 0:256], func=AF.Gelu_apprx_tanh,
                     R=[f"pA{i}"], W=[f"Wg{i}"])
                k.op("pool", "tensor_tensor", out=Wb[i][:], in0=Wg[i][:], in1=Gs[j][:, ti, :], op=ALU.mult,
                     R=[f"Wg{i}", f"Gs{j}"], W=[f"Wb{i}"])
                return i

            def S2(eg, ti, i):
                nonlocal npo
                j = eg % 2
                for c in range(2):
                    k.op("pe", "transpose", out=pT[:, c * 128:(c + 1) * 128], in_=Wb[i][:, c * 128:(c + 1) * 128],
                         identity=ident[:], R=[f"Wb{i}", "ident"], W=["pT"], inc=(c == 1))
                k.op("act", "copy", out=WT[i][:], in_=pT[:, 0:256].rearrange("p (c t) -> p c t", c=2),
                     R=["pT"], W=[f"WT{i}"])
                for ch in range(4):
                    ip = npo % 2
                    npo += 1
                    for db in range(2):
                        for c in range(2):
                            d0 = ch * 1024 + db * 512
                            k.op("pe", "matmul", out=po[ip][:, db * 512:(db + 1) * 512], lhsT=WT[i][:, c, :],
                                 rhs=Vs[j][:, c, d0:d0 + 512], start=(c == 0), stop=(c == 1),
                                 R=[f"WT{i}", f"Vs{j}"], W=[f"po{ip}"], inc=(db == 1 and c == 1))
                    a_ = acc[:, ti, ch * 1024:(ch + 1) * 1024]
                    if eg == 0 and first:
                        k.op("dve", "tensor_copy", out=a_, in_=po[ip][:], R=[f"po{ip}"], W=["acc"])
                    else:
                        k.op("dve", "tensor_tensor", out=a_, in0=po[ip][:], in1=a_, op=ALU.add,
                             R=[f"po{ip}", "acc"], W=["acc"])

            pend = S1(*units[0])
            for ui, (eg, ti) in enumerate(units):
                nxt = S1(*units[ui + 1]) if ui + 1 < len(units) else None
                S2(eg, ti, pend)
                pend = nxt
            if not last:
                k.dma(acc_out[r0:r1, :].rearrange("(n p) d -> p n d", p=128), acc[:, :nt_, :], R=["acc"],
                      W=["D:acc_out"], is_output=True)
                continue
            for ti, t in enumerate(tiles):
                for db in range(8):
                    i = ne % 2
                    ne += 1
                    c0 = db * 512
                    k.dma(xs[i][:], x1[t * 128:(t + 1) * 128, c0:c0 + 512], W=[f"xs{i}"])
                    k.dma(g2s[i][:], mv[7 + typ(t):8 + typ(t), c0:c0 + 512].partition_broadcast(128), W=[f"g2s{i}"])
                    k.op("dve", "tensor_tensor", out=tm[i][:], in0=acc[:, ti, c0:c0 + 512], in1=g2s[i][:], op=ALU.mult,
                         R=["acc", f"g2s{i}"], W=[f"tm{i}"])
                    k.op("pool", "tensor_tensor", out=tm[i][:], in0=tm[i][:], in1=xs[i][:], op=ALU.add,
                         R=[f"tm{i}", f"xs{i}"], W=[f"tm{i}"])
                    k.dma(x2[t * 128:(t + 1) * 128, c0:c0 + 512], tm[i][:], R=[f"tm{i}"], W=["D:x2"],
                          is_output=(not final))
        k.end_phase()
        if last and final:
            k.begin_phase()
            fg = k.sb("fg", [128, D])
            k.dma(fg[:], mv[9:10, :].partition_broadcast(128), W=["fg"])
            xt = [k.sb(f"xt{i}", [128, D]) for i in range(2)]
            jk = k.sb("jk", [128, D])
            ss = [k.sb(f"ss{i}", [128, 2]) for i in range(2)]
            for t in range(NT):
                i = t % 2
                k.dma(xt[i][:], x2[t * 128:(t + 1) * 128, :], R=["D:x2"], W=[f"xt{i}"])
                k.op("act", "activation", out=jk[:], in_=xt[i][:], func=AF.Square, accum_out=ss[i][:, 0:1],
                     R=[f"xt{i}"], W=["jk", f"ss{i}"])
                k.op("dve", "tensor_scalar", out=ss[i][:, 1:2], in0=ss[i][:, 0:1], scalar1=1.0 / D, scalar2=EPS,
                     op0=ALU.mult, op1=ALU.add, R=[f"ss{i}"], W=[f"ss{i}"])
                k.op("act", "activation", out=ss[i][:, 1:2], in_=ss[i][:, 1:2], func=AF.Sqrt, R=[f"ss{i}"], W=[f"ss{i}"])
                k.op("dve", "reciprocal", out=ss[i][:, 1:2], in_=ss[i][:, 1:2], R=[f"ss{i}"], W=[f"ss{i}"])
                k.op("dve", "scalar_tensor_tensor", out=xt[i][:], in0=xt[i][:], scalar=ss[i][:, 1:2], in1=fg[:],
                     op0=ALU.mult, op1=ALU.mult, R=[f"xt{i}", f"ss{i}", "fg"], W=[f"xt{i}"])
                k.dma(xo[t * 128:(t + 1) * 128, :], xt[i][:], R=[f"xt{i}"], W=["D:xo"], is_output=True)
            k.end_phase()
    return nc


def build_E(NEG=8):
    nc = new_nc()
    U = din(nc, "U", [NEG * 256, D])
    V = din(nc, "V", [NEG * 256, D])
    idf = din(nc, "identf", [128, 128])
    UTb = dout(nc, "UTb", [NEG, 128, 32, 256], BF16)
    Vb = dout(nc, "Vb", [NEG * 256, D], BF16)
    with ExitStack() as st:
        k = KB(nc, st)
        k.begin_phase()
        ident = k.sb("identf_sb", [128, 128])
        k.dma(ident[:], idf, W=["ident"])
        ut = [k.sb(f"ut{i}", [128, D]) for i in range(3)]
        vb = [k.sb(f"vb{i}", [128, D], BF16) for i in range(2)]
        uo = [k.sb(f"uo{i}", [128, 32, 256], BF16) for i in range(2)]
        pt = [k.ps(f"pt{i}", [128, 512]) for i in range(4)]
        nu = nv = npt = 0
        for g in range(NEG):
            io = g % 2
            for c in range(2):
                iu = nu % 3
                nu += 1
                r0 = g * 256 + c * 128
                k.dma(ut[iu][:], U[r0:r0 + 128, :], W=[f"ut{iu}"])
                for k4 in range(8):
                    ip = npt % 4
                    npt += 1
                    for q in range(4):
                        kk = k4 * 4 + q
                        k.op("pe", "transpose", out=pt[ip][:, q * 128:(q + 1) * 128], in_=ut[iu][:, kk * 128:(kk + 1) * 128],
                             identity=ident[:], R=[f"ut{iu}", "ident"], W=[f"pt{ip}"], inc=(q == 3))
                    src = pt[ip][:].rearrange("p (a b) -> p a b", a=4)
                    dst = uo[io][:, k4 * 4:(k4 + 1) * 4, c * 128:(c + 1) * 128]
                    if k4 % 2 == 0:
                        k.op("act", "copy", out=dst, in_=src, R=[f"pt{ip}"], W=[f"uo{io}"])
                    else:
                        k.op("dve", "tensor_copy", out=dst, in_=src, R=[f"pt{ip}"], W=[f"uo{io}"])
                iu2 = nu % 3
                nu += 1
                iv = nv % 2
                nv += 1
                k.dma(ut[iu2][:], V[r0:r0 + 128, :], W=[f"ut{iu2}"])
                k.op("pool", "tensor_copy", out=vb[iv][:], in_=ut[iu2][:], R=[f"ut{iu2}"], W=[f"vb{iv}"])
                k.dma(Vb[r0:r0 + 128, :], vb[iv][:], R=[f"vb{iv}"], W=["D:Vb"], is_output=True)
            k.dma(UTb[g], uo[io][:], R=[f"uo{io}"], W=["D:UTb"], is_output=True)
        k.end_phase()
    return nc


def build_mod(NL=2, NC=3072):
    nc = bass.Bass("TRN2", target_bir_lowering=False)
    cT = nc.dram_tensor("cT", [128, 32, 3], F32, kind="ExternalInput").ap()
    w = nc.dram_tensor("w", [NL, 4096, NC], F32, kind="ExternalInput").ap()
    b = nc.dram_tensor("b", [NL, 1, NC], F32, kind="ExternalInput").ap()
    m = nc.dram_tensor("m", [NL, 3, NC], F32, kind="ExternalOutput").ap()
    with ExitStack() as st:
        k = KB(nc, st)
        c_sb = k.sb("c_sb", [128, 32, 3])
        s_sb = k.sb("s_sb", [128, 32, 3])
        wb = [k.sb(f"wb{i}", [128, 32, 512]) for i in range(2)]
        bias = k.sb("bias", [3, NL, NC])
        o_sb = [k.sb(f"o{i}", [3, 512]) for i in range(2)]
        ps = [k.ps(f"ps{i}", [128, 512]) for i in range(2)]
        k.dma(c_sb[:], cT, W=["c"])
        for l in range(NL):
            k.dma(bias[:, l, :], b[l].partition_broadcast(3), W=["bias"])
        k.op("act", "activation", out=s_sb[:], in_=c_sb[:], func=AF.Silu, R=["c"], W=["s"])
        it = 0
        for l in range(NL):
            for cb in range(NC // 512):
                j = it % 2
                k.dma(wb[j][:], w[l, :, cb * 512:(cb + 1) * 512].rearrange("(k p) n -> p k n", p=128),
                      W=[f"wb{j}"])
                for kk in range(32):
                    k.op("pe", "matmul", out=ps[j][0:3, :], lhsT=s_sb[:, kk, :], rhs=wb[j][:, kk, :],
                         start=(kk == 0), stop=(kk == 31),
                         R=["s", f"wb{j}"], W=[f"ps{j}"], inc=(kk == 31))
                k.op("dve", "tensor_tensor", out=o_sb[j][:], in0=ps[j][0:3, :],
                     in1=bias[:, l, cb * 512:(cb + 1) * 512], op=ALU.add,
                     R=[f"ps{j}", "bias"], W=[f"o{j}"])
                k.dma(m[l, :, cb * 512:(cb + 1) * 512], o_sb[j][:], R=[f"o{j}"], W=["m"], is_output=True)
                it += 1
        k.finish()
        print("instrs", k.ninstr)
    return nc


_PROGS = {}


def _prog(key, fn):
    if key not in _PROGS:
        _PROGS[key] = fn()
    return _PROGS[key]


def _run(nc, in_maps):
    return run_bass_kernel_spmd(nc, in_maps, core_ids=list(range(8))).results


def _c(a):
    return np.ascontiguousarray(a)


def kernel(x, c, ctx, c_ctx, mod_w, mod_b, norm_g, ev_w_in, ev_gate_b, ev_qk_g, ev_hnorm_g, ev_w_out,
           od_w_in, od_rpb, od_w_out, peer_wq, peer_subkeys, peer_u, peer_v, final_g):
    f32 = np.float32
    x, c, ctx, c_ctx = (np.asarray(a, f32) for a in (x, c, ctx, c_ctx))
    mod_w, mod_b, norm_g = (np.asarray(a, f32) for a in (mod_w, mod_b, norm_g))
    ev_w_in, ev_gate_b, ev_qk_g, ev_hnorm_g, ev_w_out = (np.asarray(a, f32) for a in
                                                         (ev_w_in, ev_gate_b, ev_qk_g, ev_hnorm_g, ev_w_out))
    od_w_in, od_rpb, od_w_out = (np.asarray(a, f32) for a in (od_w_in, od_rpb, od_w_out))
    peer_wq, peer_subkeys, peer_u, peer_v, final_g = (np.asarray(a, f32) for a in
                                                      (peer_wq, peer_subkeys, peer_u, peer_v, final_g))
    ident = np.eye(128, dtype=f32).astype(BF)
    identf = np.eye(128, dtype=f32)
    cst = np.zeros((128, 5, 128), f32)
    s_, t_ = np.meshgrid(np.arange(128), np.arange(128), indexing="ij")
    cst[:, 0, :] = (s_ <= t_)
    cst[:, 1, :] = (s_ >= t_)
    cst[:, 2, :] = 1.0
    tt = np.arange(4096)
    inv = (10000.0 ** (-np.arange(32, dtype=f32) / 32)).astype(f32)
    ang = np.concatenate([(tt // 64).astype(f32)[:, None] * inv, (tt % 64).astype(f32)[:, None] * inv], -1)
    rope = np.concatenate([np.cos(ang), np.sin(ang)], -1).astype(f32)

    cvec = np.stack([c[0], c[1], c_ctx])
    cT = _c(cvec.reshape(3, 32, 128).transpose(2, 1, 0))
    ncA = _prog("A", lambda: build_mod(2, 3072))
    rA = _run(ncA, [{"cT": cT, "w": _c(mod_w[:, :, i * 3072:(i + 1) * 3072]),
                     "b": _c(mod_b[:, None, i * 3072:(i + 1) * 3072])} for i in range(8)])
    mod = np.concatenate([r["m"] for r in rA], axis=-1).reshape(2, 3, 6, 4096)

    ncE = _prog("E", lambda: build_E(8))
    UTb, Vbf = [], []
    for l in range(2):
        rE = _run(ncE, [{"U": _c(peer_u[l, i * 2048:(i + 1) * 2048]), "V": _c(peer_v[l, i * 2048:(i + 1) * 2048]),
                         "identf": identf} for i in range(8)])
        UTb.append(np.concatenate([r["UTb"] for r in rE], axis=0))
        Vbf.append(np.concatenate([r["Vb"] for r in rE], axis=0))

    xl = x
    xc = ctx
    zpad = np.zeros((64, 4096), f32)
    out = np.empty((2, 4096, 4096), f32)
    for layer in range(2):
        last = layer == 1
        sh1, sc1, g1, sh2, sc2, g2 = (mod[layer, :, i] for i in range(6))
        ncB = _prog("B", lambda: build_B(9))
        rows = {}
        maps = []
        for b in range(2):
            for q in range(4):
                r_ = np.concatenate([xc[b, 64 * q:64 * q + 64], zpad, xl[b, 1024 * q:1024 * (q + 1)]], axis=0)
                rows[(b, q)] = r_
                mv5 = np.stack([norm_g[layer, 0], sc1[2], sh1[2], sc1[b], sh1[b]])
                maps.append({"x": r_, "mv": mv5, "ident": ident})
        rB = _run(ncB, maps)
        hTb = []
        for b in range(2):
            parts = [rB[b * 4 + q]["hT"][:, 0:64] for q in range(4)] + [rB[b * 4 + q]["hT"][:, 128:] for q in range(4)]
            hTb.append(_c(np.concatenate(parts, axis=1)))
        if layer == 0:
            ncC = _prog("C0", lambda: build_C0(2, 32))
            w = ev_w_in[0]
            maps = []
            for b in range(2):
                for g in range(4):
                    gcols = [9216 + o + 2 * g + hh for o in (0, 8, 16, 24) for hh in (0, 1)]
                    Wc = np.concatenate([w[:, 512 * g:512 * g + 512], w[:, 2048 + 128 * g:2048 + 128 * g + 128],
                                         w[:, 2560 + 128 * g:2560 + 128 * g + 128],
                                         w[:, 3072 + 256 * g:3072 + 256 * g + 256],
                                         w[:, 4096 + 256 * g:4096 + 256 * g + 256],
                                         w[:, 5120 + 512 * g:5120 + 512 * g + 512],
                                         w[:, 7168 + 512 * g:7168 + 512 * g + 512], w[:, gcols]], axis=1)
                    gbv = ev_gate_b[0][[o + 2 * g + hh for o in (0, 8, 16, 24) for hh in (0, 1)]][None]
                    maps.append({"hT": hTb[b], "W": _c(Wc), "cst": cst, "ident": ident, "qkg": _c(ev_qk_g[0]),
                                 "gb": _c(gbv), "hng": _c(ev_hnorm_g[0].reshape(8, 256)[2 * g:2 * g + 2]),
                                 "rope": rope})
            rC = _run(ncC, maps)
            yTb = []
            for b in range(2):
                y = np.empty((4096, 4352), BF)
                for g in range(4):
                    p = rC[b * 4 + g]["yT"]
                    y[512 * g:512 * g + 512] = p[0:512]
                    y[2048 + 512 * g:2048 + 512 * g + 512] = p[512:1024]
                yTb.append(y)
        else:
            ncC = _prog("C1", lambda: build_C1(2, 32, 8))
            w = od_w_in[0]
            maps = []
            for b in range(2):
                for g in range(4):
                    Wc = np.concatenate([w[:, 1024 * g:1024 * (g + 1)], w[:, 4096 + 1024 * g:4096 + 1024 * (g + 1)],
                                         w[:, 8192 + 1024 * g:8192 + 1024 * (g + 1)]], axis=1)
                    Bt = np.stack([na_bias_table(od_rpb[0][8 * g + h], 64).reshape(128, -1) for h in range(8)])
                    maps.append({"hT": hTb[b], "W": _c(Wc), "ident": ident, "Bt": _c(Bt)})
            rC = _run(ncC, maps)
            yTb = []
            for b in range(2):
                y = np.empty((4096, 4352), BF)
                y[:, :256] = 0
                for g in range(4):
                    y[1024 * g:1024 * (g + 1), 256:] = rC[b * 4 + g]["yT"]
                yTb.append(y)
        NT = 8 if last else 9
        CTXT = 0 if last else 1
        w_out = (od_w_out if last else ev_w_out)[0]
        skT = _c(peer_subkeys[layer].transpose(2, 0, 1))
        ncDa = _prog(("Da", NT), lambda: build_D(NT, CTXT, part="a"))
        maps = []
        mvs = {}
        for b in range(2):
            for q in range(4):
                lat = yTb[b][:, 256 + 1024 * q:256 + 1024 * (q + 1)]
                if last:
                    xr = rows[(b, q)][128:]
                    yc = _c(lat)
                else:
                    xr = rows[(b, q)]
                    yc = _c(np.concatenate([yTb[b][:, 64 * q:64 * q + 64], np.zeros((4096, 64), BF), lat], axis=1))
                mv10 = np.stack([g1[2], g1[b], norm_g[layer, 1], sc2[2], sh2[2], sc2[b], sh2[b], g2[2], g2[b], final_g])
                mvs[(b, q)] = mv10
                maps.append({"x": _c(xr), "yT": yc, "wout": w_out, "mv": mv10, "wq": peer_wq[layer], "skT": skT,
                             "ident": ident})
        rDa = _run(ncDa, maps)
        acc = [None] * 8
        for ch in range(4):
            first, lastc = ch == 0, ch == 3
            ncDb = _prog(("Db", NT, first, lastc), lambda: build_Db(NT, CTXT, last, 16, first, lastc))
            maps = []
            for ci in range(8):
                m = {"h2T": rDa[ci]["h2T"], "G": _c(rDa[ci]["G"][:, ch * 4096:(ch + 1) * 4096]),
                     "UTb": _c(UTb[layer][ch * 16:(ch + 1) * 16]), "Vb": _c(Vbf[layer][ch * 4096:(ch + 1) * 4096]),
                     "ident": ident}
                if not first:
                    m["acc_in"] = acc[ci]
                if lastc:
                    m["x1"] = rDa[ci]["x1"]
                    m["mv"] = mvs[(ci // 4, ci % 4)]
                maps.append(m)
            rDb = _run(ncDb, maps)
            if not lastc:
                acc = [r["acc_out"] for r in rDb]
        if last:
            for b in range(2):
                for q in range(4):
                    out[b, 1024 * q:1024 * (q + 1)] = rDb[b * 4 + q]["xo"]
        else:
            xl = np.empty_like(x)
            xc = np.empty_like(ctx)
            for b in range(2):
                for q in range(4):
                    r_ = rDb[b * 4 + q]["xo"]
                    xc[b, 64 * q:64 * q + 64] = r_[0:64]
                    xl[b, 1024 * q:1024 * (q + 1)] = r_[128:]
    return out
```

```python
import numpy as np
import concourse.bass as bass
import concourse.mybir as mybir

F32 = mybir.dt.float32
BF16 = mybir.dt.bfloat16
AF = mybir.ActivationFunctionType
ALU = mybir.AluOpType
AX = mybir.AxisListType

SEM_LIMIT = 30000
import os
N_DMA_SEMS = int(os.environ.get("NDS", "48"))


class KB:
    def __init__(self, nc, stack):
        self.nc = nc
        self.stack = stack
        self.sem_stack = stack
        self.phase_stack = None
        self.eng = {"pe": nc.tensor, "dve": nc.vector, "act": nc.scalar,
                    "pool": nc.gpsimd, "sp": nc.sync}
        self.cnt = {}
        self.semkey = {}
        self.sems = {}
        self.nsem = 0
        for e in ("pe", "dve", "act", "pool"):
            self._new_epoch(e, 0)
        self.epoch = {e: 0 for e in ("pe", "dve", "act", "pool")}
        self.known = {e: {} for e in self.eng}
        self.last_w = {}
        self.readers = {}
        self.dma_sems = []
        for i in range(N_DMA_SEMS):
            s = self.sem_stack.enter_context(nc.semaphore(f"dq{i}"))
            key = ("dma", i)
            self.sems[key] = s
            self.dma_sems.append(key)
        self.dma_uses = {k: 0 for k in self.dma_sems}
        self.dma_rr = 0
        self.ninstr = 0
        self.out_tokens = []
        self.prog = {e: [] for e in self.eng}
        self.psum_keys = set()

    def _new_epoch(self, e, ep):
        key = (e, ep)
        s = self.sem_stack.enter_context(self.nc.semaphore(f"c_{e}_{ep}"))
        self.sems[key] = s
        self.semkey[e] = key
        self.cnt[e] = 0

    def _deps(self, R, W):
        deps = set()
        for b in R:
            t = self.last_w.get(b)
            if t is not None:
                deps.add(t)
        for b in W:
            t = self.last_w.get(b)
            if t is not None:
                deps.add(t)
            for t in self.readers.get(b, ()):
                deps.add(t)
        return deps

    def _record(self, tok, R, W):
        for b in R:
            self.readers.setdefault(b, []).append(tok)
        for b in W:
            self.last_w[b] = tok
            self.readers[b] = []

    def _emit_waits(self, x, deps):
        kn = self.known[x]
        best = {}
        for (key, val) in deps:
            if kn.get(key, 0) >= val:
                continue
            if key == self.semkey.get(x) and val > self.cnt[x]:
                continue
            if best.get(key, 0) < val:
                best[key] = val
        for key, val in best.items():
            self.prog[x].append(("w", self.sems[key], val))
            kn[key] = val
            self.ninstr += 1

    def op(self, x, meth, R=(), W=(), inc=True, **kw):
        import os
        if x == "pool" and os.environ.get("NOPOOL"):
            x = "dve"
        fn = (lambda e, meth=meth, kw=kw: getattr(e, meth)(**kw))
        pk = [b for b in R if b in self.psum_keys]
        if pk:
            R = [b for b in R if b not in self.psum_keys]
            W = list(W) + pk
        deps = self._deps(R, W)
        self._emit_waits(x, deps)
        if self.cnt[x] + 1 > SEM_LIMIT:
            self.epoch[x] += 1
            self._new_epoch(x, self.epoch[x])
        self.ninstr += 1
        key = self.semkey[x]
        if inc:
            self.cnt[x] += 1
            self.prog[x].append(("o", fn, self.sems[key], 1))
            tok = (key, self.cnt[x])
        else:
            self.prog[x].append(("o", fn, None, 0))
            tok = (key, self.cnt[x] + 1)
        self._record(tok, R, W)
        return tok

    def dma(self, out, in_, R=(), W=(), q="sp", is_output=False, **kw):
        key = self.dma_sems[self.dma_rr % N_DMA_SEMS]
        self.dma_rr += 1
        deps = self._deps(R, W)
        prev = self.dma_uses[key]
        if prev:
            deps.add((key, 16 * prev))
        self._emit_waits(q, deps)
        self.prog[q].append(("o", (lambda e, out=out, in_=in_, kw=kw: e.dma_start(out=out, in_=in_, **kw)),
                             self.sems[key], 16))
        self.ninstr += 1
        self.dma_uses[key] = prev + 1
        tok = (key, 16 * (prev + 1))
        self._record(tok, R, W)
        if is_output:
            self.out_tokens.append(tok)
        return tok

    def finish(self, q="sp"):
        self._emit_waits(q, set(self.out_tokens))
        deps = set()
        for e in ("pe", "dve", "act", "pool"):
            if self.cnt[e] > 0:
                deps.add((self.semkey[e], self.cnt[e]))
        for key, n in self.dma_uses.items():
            if n:
                deps.add((key, 16 * n))
        self._emit_waits(q, deps)
        self.emit()

    def begin_phase(self):
        from contextlib import ExitStack
        ps = ExitStack()
        ps.__enter__()
        if not hasattr(self, "_stk"):
            self._stk = []
        self._stk.append(self.stack)
        self.stack = ps

    def end_phase(self, q="sp"):
        deps = set()
        for key, n in self.dma_uses.items():
            if n:
                deps.add((key, 16 * n))
        for e in ("pe", "dve", "act", "pool"):
            if self.cnt[e] > 0:
                deps.add((self.semkey[e], self.cnt[e]))
        self._emit_waits(q, deps)
        self.emit()
        self.prog = {e: [] for e in self.eng}
        ps = self.stack
        self.stack = self._stk.pop()
        ps.__exit__(None, None, None)
        self.last_w = {b: t for b, t in self.last_w.items() if isinstance(b, str) and b.startswith("D:")}
        self.readers = {b: t for b, t in self.readers.items() if isinstance(b, str) and b.startswith("D:")}

    def emit(self):
        def runner(lst):
            def f(eng):
                for it in lst:
                    if it[0] == "w":
                        eng.wait_ge(it[1], it[2])
                    else:
                        ins = it[1](eng)
                        if it[2] is not None:
                            ins.then_inc(it[2], it[3])
            return f
        with self.nc.Block() as block:
            block.sync(runner(self.prog["sp"]))
            block.tensor(runner(self.prog["pe"]))
            block.vector(runner(self.prog["dve"]))
            block.scalar(runner(self.prog["act"]))
            block.gpsimd(runner(self.prog["pool"]))

    def sb(self, name, shape, dtype=F32):
        self.nsem += 1
        return self.stack.enter_context(self.nc.sbuf_tensor(f"{name}_{self.nsem}", list(shape), dtype))

    def ps(self, name, shape, dtype=F32):
        self.nsem += 1
        self.psum_keys.add(name)
        return self.stack.enter_context(self.nc.psum_tensor(f"{name}_{self.nsem}", list(shape), dtype))


import numpy as np
from contextlib import ExitStack
import concourse.bass as bass
import concourse.mybir as mybir
from concourse.bass_utils import run_bass_kernel_spmd
import ml_dtypes

BF = ml_dtypes.bfloat16
EPS = 1e-6
D = 4096


def new_nc():
    return bass.Bass("TRN2", target_bir_lowering=False)


def din(nc, name, shape, dt=F32):
    return nc.dram_tensor(name, list(shape), dt, kind="ExternalInput").ap()


def dout(nc, name, shape, dt=F32):
    return nc.dram_tensor(name, list(shape), dt, kind="ExternalOutput").ap()


def dscr(nc, name, shape, dt=F32):
    return nc.dram_tensor(name, list(shape), dt, kind="Internal").ap()


class NormT:
    def __init__(self, k, ident, nbuf=2, pfx="n"):
        self.k = k
        self.ident = ident
        self.pfx = pfx
        self.xt = [k.sb(f"{pfx}xt{i}", [128, D]) for i in range(nbuf)]
        self.tmp = k.sb(f"{pfx}tmp", [128, D])
        self.hb = [k.sb(f"{pfx}hb{i}", [128, D], BF16) for i in range(nbuf)]
        self.ss = [k.sb(f"{pfx}ss{i}", [128, 2]) for i in range(nbuf)]
        self.pst = [k.ps(f"{pfx}pst{i}", [128, 1024], BF16) for i in range(2)]
        self.n = 0
        self.nps = 0

    def tile(self, x_rows, xkey_R, scale_bc, shift_bc, scale_key, hT, hT_key):
        k = self.k
        i = self.n % len(self.xt)
        self.n += 1
        p = self.pfx
        xt, hb, ss = self.xt[i], self.hb[i], self.ss[i]
        k.dma(xt[:], x_rows, R=xkey_R, W=[f"{p}xt{i}"])
        k.op("act", "activation", out=self.tmp[:], in_=xt[:], func=AF.Square, accum_out=ss[:, 0:1],
             R=[f"{p}xt{i}"], W=[f"{p}tmp", f"{p}ss{i}"])
        k.op("dve", "tensor_scalar", out=ss[:, 1:2], in0=ss[:, 0:1], scalar1=1.0 / D, scalar2=EPS,
             op0=ALU.mult, op1=ALU.add, R=[f"{p}ss{i}"], W=[f"{p}ss{i}"])
        k.op("act", "activation", out=ss[:, 1:2], in_=ss[:, 1:2], func=AF.Sqrt, R=[f"{p}ss{i}"], W=[f"{p}ss{i}"])
        k.op("dve", "reciprocal", out=ss[:, 1:2], in_=ss[:, 1:2], R=[f"{p}ss{i}"], W=[f"{p}ss{i}"])
        k.op("dve", "scalar_tensor_tensor", out=self.tmp[:], in0=xt[:], scalar=ss[:, 1:2], in1=scale_bc,
             op0=ALU.mult, op1=ALU.mult, R=[f"{p}xt{i}", f"{p}ss{i}", scale_key], W=[f"{p}tmp"])
        k.op("pool", "tensor_tensor", out=hb[:], in0=self.tmp[:], in1=shift_bc, op=ALU.add,
             R=[f"{p}tmp", scale_key], W=[f"{p}hb{i}"])
        for b in range(4):
            j = self.nps % 2
            self.nps += 1
            for q in range(8):
                kk = b * 8 + q
                k.op("pe", "transpose", out=self.pst[j][:, q * 128:(q + 1) * 128],
                     in_=hb[:, kk * 128:(kk + 1) * 128], identity=self.ident,
                     R=[f"{p}hb{i}", "ident"], W=[f"{p}pst{j}"], inc=(q == 7))
            eng = "act" if b % 2 == 0 else "dve"
            meth = "copy" if eng == "act" else "tensor_copy"
            k.op(eng, meth, out=hT[:, b * 8:(b + 1) * 8, :],
                 in_=self.pst[j][:].rearrange("p (a b) -> p a b", a=8),
                 R=[f"{p}pst{j}"], W=[hT_key])


def load_bc(k, dst, vec_ap, key):
    k.dma(dst, vec_ap.partition_broadcast(128), W=[key])


def build_B(NT=9):
    nc = new_nc()
    x = din(nc, "x", [NT * 128, D])
    mv = din(nc, "mv", [5, D])
    idn = din(nc, "ident", [128, 128], BF16)
    hT = dout(nc, "hT", [D, NT * 128], BF16)
    with ExitStack() as st:
        k = KB(nc, st)
        ident = k.sb("ident_sb", [128, 128], BF16)
        k.dma(ident[:], idn, W=["ident"])
        mods = emit_mods(k, mv)
        nt = NormT(k, ident[:])
        hts = [k.sb(f"hT{i}", [128, 32, 128], BF16) for i in range(2)]
        for t in range(NT):
            sc, sh = mods[0] if t == 0 else mods[1]
            j = t % 2
            nt.tile(x[t * 128:(t + 1) * 128, :], [], sc[:], sh[:], "mods", hts[j], f"hT{j}")
            k.dma(hT[:, t * 128:(t + 1) * 128].rearrange("(a p) t -> p a t", p=128), hts[j][:],
                  R=[f"hT{j}"], W=["hTout"], is_output=True)
        k.finish()
    return nc


def emit_mods(k, mv, pfx="m"):
    g = k.sb(pfx + "g", [128, D])
    tiles = [k.sb(f"{pfx}{i}", [128, D]) for i in range(4)]
    load_bc(k, g[:], mv[0:1, :], "mods_g")
    for i in range(4):
        load_bc(k, tiles[i][:], mv[1 + i:2 + i, :], f"mods_r{i}")
    for i in (0, 2):
        k.op("dve", "scalar_tensor_tensor", out=tiles[i][:], in0=tiles[i][:], scalar=1.0, in1=g[:],
             op0=ALU.add, op1=ALU.mult, R=["mods_g", f"mods_r{i}"], W=[f"mods_r{i}", "mods"])
    k.op("dve", "tensor_copy", out=g[:, 0:1], in_=tiles[1][:, 0:1], R=["mods_r1", "mods_r3", "mods_g"], W=["mods", "mods_g"])
    return [(tiles[0], tiles[1]), (tiles[2], tiles[3])]


def ref_B(x, mv):
    xf = x.astype(np.float64)
    y = xf / np.sqrt((xf * xf).mean(-1, keepdims=True) + EPS) * mv[0]
    out = np.empty_like(y)
    out[:128] = y[:128] * (1 + mv[1]) + mv[2]
    out[128:] = y[128:] * (1 + mv[3]) + mv[4]
    return out.T


C_Q, C_K, C_V, C_QB, C_KB, C_VB, C_OB, C_GT, C_N = 0, 512, 640, 768, 1024, 1280, 1792, 2304, 2312
ATT_SCALE = 128 ** -0.5


def emit_linear_tm(k, hT, W, out, T, N, out_key, pfx="l"):
    wbs = [k.sb(f"{pfx}wb{i}", [128, 32, 512], BF16) for i in range(2)]
    stg = [k.sb(f"{pfx}stg{i}", [128, 4, 512]) for i in range(3)]
    hb = [k.sb(f"{pfx}hb{i}", [128, 32, 512], BF16) for i in range(2)]
    osb = [k.sb(f"{pfx}o{i}", [128, 512]) for i in range(3)]
    ps = [k.ps(f"{pfx}ps{i}", [128, 512]) for i in range(3)]
    ns = nh = no = 0
    blocks = [(c0, min(512, N - c0)) for c0 in range(0, N, 512)]

    def load_block(bi):
        nonlocal ns
        c0, nw = blocks[bi]
        wb = wbs[bi % 2]
        for q in range(8):
            j = ns % 3
            ns += 1
            k.dma(stg[j][:, :, :nw], W[q * 512:(q + 1) * 512, c0:c0 + nw].rearrange("(a p) n -> p a n", p=128),
                  W=[f"{pfx}stg{j}"])
            if q % 2 == 0:
                k.op("dve", "tensor_copy", out=wb[:, q * 4:(q + 1) * 4, :nw], in_=stg[j][:, :, :nw],
                     R=[f"{pfx}stg{j}"], W=[f"{pfx}wb{bi % 2}"])
            else:
                k.op("act", "copy", out=wb[:, q * 4:(q + 1) * 4, :nw], in_=stg[j][:, :, :nw],
                     R=[f"{pfx}stg{j}"], W=[f"{pfx}wb{bi % 2}"])

    load_block(0)
    for bi, (c0, nw) in enumerate(blocks):
        wb = wbs[bi % 2]
        wkey = f"{pfx}wb{bi % 2}"
        first_tile = True
        for t0 in range(0, T, 512):
            tw = min(512, T - t0)
            jh = nh % 2
            nh += 1
            k.dma(hb[jh][:, :, :tw], hT[:, t0:t0 + tw].rearrange("(a p) t -> p a t", p=128), W=[f"{pfx}hb{jh}"])
            for tt in range(0, tw, 128):
                jo = no % 3
                no += 1
                for kk in range(32):
                    k.op("pe", "matmul", out=ps[jo][:, :nw], lhsT=hb[jh][:, kk, tt:tt + 128], rhs=wb[:, kk, :nw],
                         start=(kk == 0), stop=(kk == 31), R=[f"{pfx}hb{jh}", wkey], W=[f"{pfx}ps{jo}"],
                         inc=(kk == 31))
                if first_tile and bi + 1 < len(blocks):
                    load_block(bi + 1)
                    first_tile = False
                if jo % 2 == 0:
                    k.op("act", "copy", out=osb[jo][:, :nw], in_=ps[jo][:, :nw], R=[f"{pfx}ps{jo}"], W=[f"{pfx}o{jo}"])
                else:
                    k.op("dve", "tensor_copy", out=osb[jo][:, :nw], in_=ps[jo][:, :nw], R=[f"{pfx}ps{jo}"],
                         W=[f"{pfx}o{jo}"])
                k.dma(out[t0 + tt:t0 + tt + 128, c0:c0 + nw], osb[jo][:, :nw], R=[f"{pfx}o{jo}"], W=[out_key])


def build_C0(CT=2, LT=32, stop_after=3, part="all"):
    NTt = CT + LT
    T = NTt * 128
    nc = new_nc()
    hT = din(nc, "hT", [D, T], BF16)
    W = din(nc, "W", [D, C_N])
    cst = din(nc, "cst", [128, 5, 128])
    idn = din(nc, "ident", [128, 128], BF16)
    qkg = din(nc, "qkg", [2, 128])
    gb = din(nc, "gb", [1, 8])
    hng = din(nc, "hng", [2, 256])
    cs_t = din(nc, "rope", [LT * 128, 128])
    yT = dout(nc, "yT", [1024, T], BF16)
    if part == "a":
        Pm = dout(nc, "Pm", [T, C_N])
    elif part == "b":
        Pm = din(nc, "Pm", [T, C_N])
    else:
        Pm = dscr(nc, "Pm", [T, C_N])
    with ExitStack() as st:
        k = KB(nc, st)
        import os
        if part != "b":
            k.begin_phase()
            emit_linear_tm(k, hT, W, Pm, T, C_N, "D:Pm")
            k.end_phase()
        if stop_after < 1.5 or part == "a":
            return nc
        k.begin_phase()
        ident = k.sb("ident_sb", [128, 128], BF16)
        k.dma(ident[:], idn, W=["ident"])
        ones = k.sb("ones", [128, 128], BF16)
        k.op("dve", "memset", ap=ones[:], constant=1.0, W=["ones"])
        g5 = k.sb("g5", [128, 5, 128])
        for j in range(5):
            k.dma(g5[:, j, :], qkg[(0 if j < 4 else 1):(1 if j < 4 else 2), :].partition_broadcast(128), W=["g5"])
        QT = k.sb("QT", [128, 4, T], BF16)
        KT = k.sb("KT", [128, T], BF16)
        Vs = k.sb("Vs", [128, NTt, 128], BF16)
        xin = [k.sb(f"xin{i}", [128, 768]) for i in range(2)]
        cs = [k.sb(f"cs{i}", [128, 128]) for i in range(2)]
        sq = k.sb("sq", [128, 5, 128])
        xn = k.sb("xn", [128, 5, 128])
        ta = k.sb("ta", [128, 5, 64])
        tb = k.sb("tb", [128, 5, 64])
        xo = [k.sb(f"xo{i}", [128, 5, 128], BF16) for i in range(2)]
        st5 = k.sb("st5", [128, 10])
        pst = [k.ps(f"pst{i}", [128, 1024], BF16) for i in range(2)]
        for t in range(NTt):
            i = t % 2
            k.dma(xin[i][:], Pm[t * 128:(t + 1) * 128, 0:768], R=["D:Pm"], W=[f"xin{i}"])
            xv = xin[i][:, 0:640].rearrange("p (h d) -> p h d", h=5)
            if int(os.environ.get("PREP_CUT", "9")) < 1:
                continue
            k.op("pool", "tensor_tensor", out=sq[:], in0=xv, in1=xv, op=ALU.mult, R=[f"xin{i}"], W=["sq"])
            k.op("dve", "tensor_reduce", out=st5[:, 0:5], in_=sq[:], op=ALU.add, axis=AX.X, R=["sq"], W=["st5"])
            k.op("dve", "tensor_scalar", out=st5[:, 5:10], in0=st5[:, 0:5], scalar1=1.0 / 128, scalar2=EPS,
                 op0=ALU.mult, op1=ALU.add, R=["st5"], W=["st5"])
            k.op("act", "activation", out=st5[:, 5:10], in_=st5[:, 5:10], func=AF.Sqrt, R=["st5"], W=["st5"])
            k.op("dve", "reciprocal", out=st5[:, 5:10], in_=st5[:, 5:10], R=["st5"], W=["st5"])
            k.op("dve", "tensor_tensor", out=xn[:], in0=xv, in1=st5[:, 5:10].unsqueeze(2).to_broadcast([128, 5, 128]),
                 op=ALU.mult, R=[f"xin{i}", "st5"], W=["xn"])
            if t >= CT:
                k.dma(cs[i][:], cs_t[(t - CT) * 128:(t - CT + 1) * 128, :], W=[f"cs{i}"])
                k.op("pool", "tensor_tensor", out=xn[:], in0=xn[:], in1=g5[:], op=ALU.mult, R=["xn", "g5"], W=["xn"])
                cosb = cs[i][:, 0:64].unsqueeze(1).to_broadcast([128, 5, 64])
                sinb = cs[i][:, 64:128].unsqueeze(1).to_broadcast([128, 5, 64])
                x1, x2 = xn[:, :, 0:64], xn[:, :, 64:128]
                k.op("dve", "tensor_tensor", out=ta[:], in0=x1, in1=cosb, op=ALU.mult, R=["xn", f"cs{i}"], W=["ta"])
                k.op("dve", "tensor_tensor", out=tb[:], in0=x2, in1=sinb, op=ALU.mult, R=["xn", f"cs{i}"], W=["tb"])
                k.op("dve", "tensor_tensor", out=xo[i][:, :, 0:64], in0=ta[:], in1=tb[:], op=ALU.subtract,
                     R=["ta", "tb"], W=[f"xo{i}"])
                k.op("dve", "tensor_tensor", out=ta[:], in0=x1, in1=sinb, op=ALU.mult, R=["xn", f"cs{i}"], W=["ta"])
                k.op("dve", "tensor_tensor", out=tb[:], in0=x2, in1=cosb, op=ALU.mult, R=["xn", f"cs{i}"], W=["tb"])
                k.op("pool", "tensor_tensor", out=xo[i][:, :, 64:128], in0=ta[:], in1=tb[:], op=ALU.add,
                     R=["ta", "tb"], W=[f"xo{i}"])
            else:
                k.op("pool", "tensor_tensor", out=xo[i][:], in0=xn[:], in1=g5[:], op=ALU.mult, R=["xn", "g5"],
                     W=[f"xo{i}"])
            if int(os.environ.get("PREP_CUT", "9")) < 3:
                continue
            for j in range(5):
                k.op("pe", "transpose", out=pst[i][:, j * 128:(j + 1) * 128], in_=xo[i][:, j, :], identity=ident[:],
                     R=[f"xo{i}", "ident"], W=[f"pst{i}"], inc=(j == 4))
            if int(os.environ.get("PREP_CUT", "9")) < 5:
                continue
            k.op("act", "copy", out=QT[:, :, t * 128:(t + 1) * 128],
                 in_=pst[i][:, 0:512].rearrange("p (h d) -> p h d", h=4), R=[f"pst{i}"], W=["QT"])
            k.op("dve", "tensor_copy", out=KT[:, t * 128:(t + 1) * 128], in_=pst[i][:, 512:640], R=[f"pst{i}"], W=["KT"])
            k.op("act", "copy", out=Vs[:, t, :], in_=xin[i][:, 640:768], R=[f"xin{i}"], W=["Vs"])
        if stop_after < 1.7:
            k.end_phase()
            return nc
        E = [k.sb(f"E{i}", [128, 512], BF16) for i in range(3)]
        pS = [k.ps(f"pS{i}", [128, 512]) for i in range(2)]
        pO = [k.ps(f"pO{i}", [128, 512]) for i in range(2)]
        pD = [k.ps(f"pD{i}", [128, 512]) for i in range(2)]
        rc = k.sb("rc", [128, 512])
        yo = [k.sb(f"yo{i}", [128, 512], BF16) for i in range(2)]
        nE = nS = nb = 0
        for j in range(4):
            blocks = [(0, CT * 128, range(0, CT))]
            for q0 in range(CT * 128, T, 512):
                blocks.append((q0, min(512, T - q0), range(0, NTt)))
            for (q0, qw, kts) in blocks:
                ib = nb % 2
                nb += 1
                kts = list(kts)

                def SA(kt):
                    nonlocal nS, nE
                    iS = nS % 2
                    nS += 1
                    iE = nE % 3
                    nE += 1
                    k.op("pe", "matmul", out=pS[iS][:, :qw], lhsT=KT[:, kt * 128:(kt + 1) * 128],
                         rhs=QT[:, j, q0:q0 + qw], start=True, stop=True, R=["KT", "QT"], W=[f"pS{iS}"])
                    k.op("act", "activation", out=E[iE][:, :qw], in_=pS[iS][:, :qw], func=AF.Exp, scale=ATT_SCALE,
                         R=[f"pS{iS}"], W=[f"E{iE}"])
                    return iE

                pend = SA(kts[0])
                for n_, kt in enumerate(kts):
                    nxt = SA(kts[n_ + 1]) if n_ + 1 < len(kts) else None
                    iE = pend
                    last = (n_ == len(kts) - 1)
                    k.op("pe", "matmul", out=pO[ib][:, :qw], lhsT=Vs[:, kt, :], rhs=E[iE][:, :qw], start=(n_ == 0),
                         stop=last, R=["Vs", f"E{iE}"], W=[f"pO{ib}"], inc=last)
                    k.op("pe", "matmul", out=pD[ib][:, :qw], lhsT=ones[:], rhs=E[iE][:, :qw], start=(n_ == 0),
                         stop=last, R=["ones", f"E{iE}"], W=[f"pD{ib}"], inc=last)
                    pend = nxt
                k.op("dve", "reciprocal", out=rc[:, :qw], in_=pD[ib][:, :qw], R=[f"pD{ib}"], W=["rc"])
                k.op("dve", "tensor_tensor", out=yo[ib][:, :qw], in0=pO[ib][:, :qw], in1=rc[:, :qw], op=ALU.mult,
                     R=[f"pO{ib}", "rc"], W=[f"yo{ib}"])
                k.dma(yT[j * 128:(j + 1) * 128, q0:q0 + qw], yo[ib][:, :qw], R=[f"yo{ib}"], W=["D:yT"], is_output=True)
        k.end_phase()
        if stop_after < 3:
            return nc
        k.begin_phase()
        ident = k.sb("ident_sb", [128, 128], BF16)
        k.dma(ident[:], idn, W=["ident"])
        cst_sb = k.sb("cst_sb", [128, 5, 128])
        k.dma(cst_sb[:], cst, W=["cst"])
        hgb = k.sb("hgb", [128, 2, 256])
        for h in range(2):
            k.dma(hgb[:, h, :], hng[h:h + 1, :].partition_broadcast(128), W=["hgb"])
        gbb = k.sb("gbb", [128, 8])
        k.dma(gbb[:], gb.partition_broadcast(128), W=["gbb"])
        Gt = k.sb("Gt", [128, NTt, 8])
        k.dma(Gt[:], Pm[:, C_GT:C_GT + 8].rearrange("(n p) c -> p n c", p=128), R=["D:Pm"], W=["Gt"])
        k.op("dve", "tensor_tensor", out=Gt[:], in0=Gt[:], in1=gbb[:].unsqueeze(1).to_broadcast([128, NTt, 8]),
             op=ALU.add, R=["Gt", "gbb"], W=["Gt"])
        LF = k.sb("LF", [128, 2, NTt, 2])
        for d_ in range(2):
            zc = Gt[:, :, 2 + 4 * d_:4 + 4 * d_]
            k.op("act", "activation", out=LF[:, d_, :, :], in_=zc, func=AF.Exp, scale=-1.0, R=["Gt"], W=["LF"])
        k.op("act", "activation", out=LF[:], in_=LF[:], func=AF.Ln, bias=1.0, R=["LF"], W=["LF"])
        k.op("dve", "tensor_scalar", out=LF[:], in0=LF[:], scalar1=-1.0, scalar2=None, op0=ALU.mult, R=["LF"], W=["LF"])
        pg = k.ps("pg", [128, 512])
        NG = NTt * 2
        for d_ in range(2):
            rhs = LF[:, d_, :, :].rearrange("p n h -> p (n h)")
            k.op("pe", "matmul", out=pg[:, d_ * NG:(d_ + 1) * NG], lhsT=cst_sb[:, d_, :], rhs=rhs, start=True, stop=True,
                 R=["cst", "LF"], W=["pg"])
            k.op("pe", "matmul", out=pg[:, (2 + d_) * NG:(3 + d_) * NG], lhsT=cst_sb[:, 2, :], rhs=rhs, start=True,
                 stop=True, R=["cst", "LF"], W=["pg"])
        Aa = k.sb("Aa", [128, 2, NTt, 2])
        Cc = k.sb("Cc", [128, 2, NTt, 2])
        EB = k.sb("EB", [128, 2, NTt, 2])
        k.op("act", "activation", out=Aa[:].rearrange("p d n h -> p (d n h)"), in_=pg[:, 0:2 * NG], func=AF.Exp,
             R=["pg"], W=["Aa"])
        k.op("act", "activation", out=EB[:].rearrange("p d n h -> p (d n h)"), in_=pg[:, 2 * NG:4 * NG], func=AF.Exp,
             R=["pg"], W=["EB"])
        for d_ in range(2):
            k.op("dve", "tensor_tensor", out=Cc[:, d_, :, :], in0=Gt[:, :, 4 * d_:4 * d_ + 2],
                 in1=pg[:, d_ * NG:(d_ + 1) * NG].rearrange("p (n h) -> p n h", h=2), op=ALU.subtract,
                 R=["Gt", "pg"], W=["Cc"])
        lnsc = k.sb("lnsc", [128, 1])
        k.op("dve", "memset", ap=lnsc[:], constant=float(-0.5 * np.log(128.0)), W=["lnsc"])
        k.op("act", "activation", out=Cc[:], in_=Cc[:], func=AF.Exp, bias=lnsc[:, 0:1], R=["Cc", "lnsc"], W=["Cc"])
        stg = k.sb("stg", [128, NTt, 256])
        Hs = k.sb("Hs", [128, NTt, 256])
        hsc = [k.sb(f"hsc{i}", [128, 4]) for i in range(2)]
        pst = [k.ps(f"pst{i}", [128, 1024], BF16) for i in range(1)]
        pP = [k.ps(f"pP{i}", [128, 512]) for i in range(2)]
        pN = [k.ps(f"pN{i}", [128, 512]) for i in range(2)]
        pC = [k.ps(f"pC{i}", [128, 512]) for i in range(2)]
        npst = 0

        def transpose_all(src, dst, src_key, dst_key, ncol_tiles=1):
            nonlocal npst
            for a in range(ncol_tiles):
                for c0 in range(0, NTt, 8):
                    cn = min(8, NTt - c0)
                    i = npst % len(pst)
                    npst += 1
                    for c in range(cn):
                        k.op("pe", "transpose", out=pst[i][:, c * 128:(c + 1) * 128],
                             in_=src[:, c0 + c, a * 128:(a + 1) * 128], identity=ident[:],
                             R=[src_key, "ident"], W=[f"pst{i}"], inc=(c == cn - 1))
                    dv = dst[:, c0 * 128:(c0 + cn) * 128] if ncol_tiles == 1 else dst[:, a, c0 * 128:(c0 + cn) * 128]
                    if (c0 // 8) % 2 == 0:
                        k.op("act", "copy", out=dv, in_=pst[i][:, :cn * 128], R=[f"pst{i}"], W=[dst_key])
                    else:
                        k.op("dve", "tensor_copy", out=dv, in_=pst[i][:, :cn * 128], R=[f"pst{i}"], W=[dst_key])

        for hh in range(2):
            k.begin_phase()
            qb = k.sb("qb", [128, NTt, 128], BF16)
            kf = [k.sb(f"kf{d_}", [128, NTt, 128], BF16) for d_ in range(2)]
            qT = k.sb("qT", [128, T], BF16)
            kT = [k.sb(f"kT{d_}", [128, T], BF16) for d_ in range(2)]
            vt = k.sb("vt", [128, NTt, 257], BF16)
            Cst = [k.sb(f"Cst{i}", [128, 257]) for i in range(2)]
            Cb = [k.sb(f"Cb{i}", [128, 257], BF16) for i in range(2)]
            PTm = [k.sb(f"PTm{i}", [128, 128], BF16) for i in range(2)]
            k.dma(stg[:, :, 0:128], Pm[:, C_QB + 128 * hh:C_QB + 128 * hh + 128].rearrange("(n p) c -> p n c", p=128),
                  R=["D:Pm"], W=["stg"])
            k.op("act", "copy", out=qb[:], in_=stg[:, :, 0:128], R=["stg"], W=["qb"])
            transpose_all(qb, qT, "qb", "qT")
            k.dma(stg[:, :, 0:128], Pm[:, C_KB + 128 * hh:C_KB + 128 * hh + 128].rearrange("(n p) c -> p n c", p=128),
                  R=["D:Pm"], W=["stg"])
            for d_ in range(2):
                k.op("dve", "tensor_tensor", out=kf[d_][:], in0=stg[:, :, 0:128],
                     in1=Cc[:, d_, :, hh:hh + 1].to_broadcast([128, NTt, 128]), op=ALU.mult,
                     R=["stg", "Cc"], W=[f"kf{d_}"])
                transpose_all(kf[d_], kT[d_], f"kf{d_}", f"kT{d_}")
            k.dma(stg[:], Pm[:, C_VB + 256 * hh:C_VB + 256 * hh + 256].rearrange("(n p) c -> p n c", p=128),
                  R=["D:Pm"], W=["stg"])
            k.op("act", "copy", out=vt[:, :, 0:256], in_=stg[:], R=["stg"], W=["vt"])
            k.op("dve", "memset", ap=vt[:, :, 256:257], constant=1.0, W=["vt"])
            orders = [list(range(NTt)), list(range(CT - 1, -1, -1)) + list(range(NTt - 1, CT - 1, -1))]
            k.op("dve", "memset", ap=Hs[:], constant=0.0, W=[f"Hs{c}" for c in range(NTt)])
            for d_ in range(2):
                k.op("dve", "memset", ap=Cst[d_][:], constant=0.0, W=[f"Cst{d_}"])
                k.op("dve", "memset", ap=Cb[d_][:], constant=0.0, W=[f"Cb{d_}"])
            for step in range(NTt):
                for d_ in range(2):
                    c = orders[d_][step]
                    i = d_
                    cols = slice(c * 128, (c + 1) * 128)
                    k.op("pe", "matmul", out=pP[i][:, 0:128], lhsT=kT[d_][:, cols], rhs=qT[:, cols], start=True, stop=True,
                         R=[f"kT{d_}", "qT"], W=[f"pP{i}"])
                    k.op("dve", "tensor_tensor", out=PTm[i][:], in0=pP[i][:, 0:128], in1=cst_sb[:, d_, :], op=ALU.mult,
                         R=[f"pP{i}", "cst"], W=[f"PTm{i}"])
                    k.op("pe", "matmul", out=pN[i][:, 0:257], lhsT=qT[:, cols], rhs=Cb[d_][:], start=True, stop=False,
                         R=["qT", f"Cb{d_}"], W=[f"pN{i}"], inc=False)
                    k.op("pe", "matmul", out=pN[i][:, 0:257], lhsT=PTm[i][:], rhs=vt[:, c, :], start=False, stop=True,
                         R=[f"PTm{i}", "vt"], W=[f"pN{i}"])
                    k.op("pe", "matmul", out=pC[d_][:, 0:257], lhsT=kf[d_][:, c, :], rhs=vt[:, c, :], start=True, stop=True,
                         R=[f"kf{d_}", "vt"], W=[f"pC{d_}"])
                    eb = EB[:, d_, c, hh:hh + 1]
                    k.op("dve", "tensor_scalar", out=Cst[d_][:], in0=Cst[d_][:], scalar1=eb, scalar2=None, op0=ALU.mult,
                         R=[f"Cst{d_}", "EB"], W=[f"Cst{d_}"])
                    k.op("dve", "scalar_tensor_tensor", out=Cst[d_][:], in0=pC[d_][:, 0:257], scalar=eb, in1=Cst[d_][:],
                         op0=ALU.mult, op1=ALU.add, R=[f"pC{d_}", "EB", f"Cst{d_}"], W=[f"Cst{d_}"])
                    k.op("act", "copy", out=Cb[d_][:], in_=Cst[d_][:], R=[f"Cst{d_}"], W=[f"Cb{d_}"])
                    a_ = Aa[:, d_, c, hh:hh + 1]
                    hs_ = hsc[d_]
                    hk = f"hsc{d_}"
                    k.op("dve", "tensor_scalar", out=hs_[:, 0:1], in0=pN[i][:, 256:257], scalar1=a_, scalar2=None,
                         op0=ALU.mult, R=[f"pN{i}", "Aa"], W=[hk])
                    k.op("dve", "tensor_scalar", out=hs_[:, 1:2], in0=hs_[:, 0:1], scalar1=-1.0, scalar2=None,
                         op0=ALU.mult, R=[hk], W=[hk])
                    k.op("dve", "scalar_tensor_tensor", out=hs_[:, 1:2], in0=hs_[:, 1:2], scalar=1.0, in1=hs_[:, 0:1],
                         op0=ALU.max, op1=ALU.max, R=[hk], W=[hk])
                    k.op("dve", "reciprocal", out=hs_[:, 2:3], in_=hs_[:, 1:2], R=[hk], W=[hk])
                    k.op("dve", "tensor_tensor", out=hs_[:, 3:4], in0=hs_[:, 2:3], in1=a_, op=ALU.mult,
                         R=[hk, "Aa"], W=[hk])
                    k.op("dve", "scalar_tensor_tensor", out=Hs[:, c, :], in0=pN[i][:, 0:256], scalar=hs_[:, 3:4],
                         in1=Hs[:, c, :], op0=ALU.mult, op1=ALU.add, R=[f"pN{i}", hk, f"Hs{c}"], W=[f"Hs{c}"])
            k.end_phase()
            k.begin_phase()
            Yb = k.sb("Yb", [128, NTt, 256], BF16)
            yTo = k.sb("yTo", [128, 2, T], BF16)
            rst = k.sb("rst", [128, 2, NTt])
            HK = [f"Hs{c_}" for c_ in range(NTt)]
            k.dma(stg[:], Pm[:, C_OB + 256 * hh:C_OB + 256 * hh + 256].rearrange("(n p) c -> p n c", p=128),
                  R=["D:Pm"], W=["stg"])
            k.op("act", "activation", out=stg[:], in_=stg[:], func=AF.Sigmoid, R=["stg"], W=["stg"])
            k.op("pool", "tensor_tensor", out=Yb[:], in0=Hs[:], in1=Hs[:], op=ALU.mult, R=HK, W=["Yb"])
            k.op("dve", "tensor_reduce", out=rst[:, 0, :], in_=Yb[:], op=ALU.add, axis=AX.X, R=["Yb"], W=["rst"])
            k.op("dve", "tensor_scalar", out=rst[:, 1, :], in0=rst[:, 0, :], scalar1=1.0 / 256, scalar2=EPS,
                 op0=ALU.mult, op1=ALU.add, R=["rst"], W=["rst"])
            k.op("act", "activation", out=rst[:, 1, :], in_=rst[:, 1, :], func=AF.Sqrt, R=["rst"], W=["rst"])
            k.op("dve", "reciprocal", out=rst[:, 1, :], in_=rst[:, 1, :], R=["rst"], W=["rst"])
            k.op("dve", "tensor_tensor", out=Hs[:], in0=Hs[:], in1=rst[:, 1, :].unsqueeze(2).to_broadcast([128, NTt, 256]),
                 op=ALU.mult, R=HK + ["rst"], W=HK)
            k.op("dve", "tensor_tensor", out=Hs[:], in0=Hs[:], in1=hgb[:, hh, :].unsqueeze(1).to_broadcast([128, NTt, 256]),
                 op=ALU.mult, R=HK + ["hgb"], W=HK)
            k.op("dve", "tensor_tensor", out=Yb[:], in0=Hs[:], in1=stg[:], op=ALU.mult, R=HK + ["stg"], W=["Yb"])
            transpose_all(Yb, yTo, "Yb", "yTo", ncol_tiles=2)
            for a in range(2):
                k.dma(yT[512 + hh * 256 + a * 128:512 + hh * 256 + (a + 1) * 128, :], yTo[:, a, :], R=["yTo"],
                      W=["D:yT"], is_output=True)
            k.end_phase()
        k.end_phase()
    return nc


NH1 = 8


def na_bias_table(rpb_h, rows):
    out = np.full((128, 8, 4, 64), -30000.0, np.float32)
    q = np.arange(64)
    cs = np.clip(q - 8, 0, 48)
    for di in range(8):
        for a in range(4):
            for rr2 in range(2):
                rr = 2 * a + rr2
                roff = 7 - di + rr
                for kc in range(64):
                    valid = (kc >= cs) & (kc < cs + 16)
                    coff = np.clip(kc - q + 15, 0, 30)
                    out[rr2 * 64 + kc, di, a, :] = np.where(valid, rpb_h[roff, coff], -30000.0)
    return out


def build_C1(CT=2, LT=32, nheads=NH1):
    NTt = CT + LT
    T = NTt * 128
    L0 = CT * 128
    rows = LT * 2
    NW = 3 * 128 * nheads
    nc = new_nc()
    hT = din(nc, "hT", [D, T], BF16)
    W = din(nc, "W", [D, NW])
    idn = din(nc, "ident", [128, 128], BF16)
    Bt = din(nc, "Bt", [nheads, 128, 8 * 4 * 64])
    yT = dout(nc, "yT", [nheads * 128, LT * 128], BF16)
    Pm = dscr(nc, "Pm", [T, NW])
    with ExitStack() as st:
        k = KB(nc, st)
        k.begin_phase()
        emit_linear_tm(k, hT, W, Pm, T, NW, "D:Pm")
        k.end_phase()
        k.begin_phase()
        ident = k.sb("ident_sb", [128, 128], BF16)
        k.dma(ident[:], idn, W=["ident"])
        ones = k.sb("ones", [128, 128], BF16)
        k.op("dve", "memset", ap=ones[:], constant=1.0, W=["ones"])
        stg = k.sb("stg", [128, NTt, 128])
        xb = k.sb("xb", [128, NTt, 128], BF16)
        QT = k.sb("QT", [128, T], BF16)
        KT = k.sb("KT", [128, T], BF16)
        Va = k.sb("Va", [128, NTt, 128], BF16)
        Vb = k.sb("Vb", [128, NTt, 128], BF16)
        Mt = k.sb("Mt", [128, 8, 256])
        E = [k.sb(f"E{i}", [128, 64 * (4 + CT)], BF16) for i in range(3)]
        rc = k.sb("rc", [128, 512])
        yo = [k.sb(f"yo{i}", [128, 512], BF16) for i in range(2)]
        pst = [k.ps(f"pst{i}", [128, 1024], BF16) for i in range(2)]
        pS = [k.ps(f"pS{i}", [128, 512]) for i in range(2)]
        pO = [k.ps(f"pO{i}", [128, 512]) for i in range(2)]
        pD = [k.ps(f"pD{i}", [128, 512]) for i in range(2)]
        npst = [0]

        def transpose_all(src, dst, src_key, dst_key):
            for c0 in range(0, NTt, 8):
                cn = min(8, NTt - c0)
                i = npst[0] % 2
                npst[0] += 1
                for c in range(cn):
                    k.op("pe", "transpose", out=pst[i][:, c * 128:(c + 1) * 128], in_=src[:, c0 + c, :],
                         identity=ident[:], R=[src_key, "ident"], W=[f"pst{i}"], inc=(c == cn - 1))
                dv = dst[:, c0 * 128:(c0 + cn) * 128]
                if (c0 // 8) % 2 == 0:
                    k.op("act", "copy", out=dv, in_=pst[i][:, :cn * 128], R=[f"pst{i}"], W=[dst_key])
                else:
                    k.op("dve", "tensor_copy", out=dv, in_=pst[i][:, :cn * 128], R=[f"pst{i}"], W=[dst_key])

        nE = nS = nG = 0
        NA_ = 4 + CT
        for h in range(nheads):
            for (col, dst, dkey) in ((h * 128, QT, "QT"), ((nheads + h) * 128, KT, "KT")):
                k.dma(stg[:], Pm[:, col:col + 128].rearrange("(n p) c -> p n c", p=128), R=["D:Pm"], W=["stg"])
                k.op("act", "copy", out=xb[:], in_=stg[:], R=["stg"], W=["xb"])
                transpose_all(xb, dst, "xb", dkey)
            vcol = (2 * nheads + h) * 128
            k.dma(stg[:], Pm[:, vcol:vcol + 128].rearrange("(n p) c -> p n c", p=128), R=["D:Pm"], W=["stg"])
            k.op("act", "copy", out=Va[:], in_=stg[:], R=["stg"], W=["Va"])
            k.dma(stg[:, 0:NTt - 1, :], Pm[64:64 + (NTt - 1) * 128, vcol:vcol + 128].rearrange("(n p) c -> p n c", p=128),
                  R=["D:Pm"], W=["stg"])
            k.op("dve", "tensor_copy", out=Vb[:, 0:NTt - 1, :], in_=stg[:, 0:NTt - 1, :], R=["stg"], W=["Vb"])
            k.dma(Mt[:].rearrange("p d c -> p (d c)"), Bt[h], W=["Mt"])
            k.op("act", "activation", out=Mt[:], in_=Mt[:], func=AF.Exp, R=["Mt"], W=["Mt"])
            def SA(r):
                nonlocal nS, nE
                r0 = min(max(r - 4, 0), rows - 8)
                di = r - r0
                iS = nS % 2
                nS += 1
                iE = nE % 3
                nE += 1
                qs = slice(L0 + 64 * r, L0 + 64 * r + 64)
                for a in range(NA_):
                    ks = (L0 + 64 * r0 + 128 * a) if a < 4 else 128 * (a - 4)
                    k.op("pe", "matmul", out=pS[iS][:, a * 64:(a + 1) * 64], lhsT=KT[:, ks:ks + 128], rhs=QT[:, qs],
                         start=True, stop=True, R=["KT", "QT"], W=[f"pS{iS}"], inc=(a == NA_ - 1))
                k.op("act", "activation", out=E[iE][:], in_=pS[iS][:, 0:64 * NA_], func=AF.Exp, scale=ATT_SCALE,
                     R=[f"pS{iS}"], W=[f"E{iE}"])
                k.op("dve", "tensor_tensor", out=E[iE][:, 0:256], in0=E[iE][:, 0:256], in1=Mt[:, di, :], op=ALU.mult,
                     R=[f"E{iE}", "Mt"], W=[f"E{iE}"])
                return iE

            pend = SA(0)
            for r in range(rows):
                nxt = SA(r + 1) if r + 1 < rows else None
                iE = pend
                pend = nxt
                r0 = min(max(r - 4, 0), rows - 8)
                g8, rg = r // 8, r % 8
                ig = nG % 2
                for a in range(NA_):
                    if a < 4:
                        t64 = CT * 2 + r0 + 2 * a
                        vt = Va[:, t64 // 2, :] if t64 % 2 == 0 else Vb[:, (t64 - 1) // 2, :]
                        vkey = "Va" if t64 % 2 == 0 else "Vb"
                    else:
                        vt, vkey = Va[:, a - 4, :], "Va"
                    last = (a == NA_ - 1)
                    k.op("pe", "matmul", out=pO[ig][:, rg * 64:(rg + 1) * 64], lhsT=vt, rhs=E[iE][:, a * 64:(a + 1) * 64],
                         start=(a == 0), stop=last, R=[vkey, f"E{iE}"], W=[f"pO{ig}"], inc=last)
                    k.op("pe", "matmul", out=pD[ig][:, rg * 64:(rg + 1) * 64], lhsT=ones[:], rhs=E[iE][:, a * 64:(a + 1) * 64],
                         start=(a == 0), stop=last, R=["ones", f"E{iE}"], W=[f"pD{ig}"], inc=last)
                if rg == 7:
                    nG += 1
                    k.op("dve", "reciprocal", out=rc[:], in_=pD[ig][:], R=[f"pD{ig}"], W=["rc"])
                    k.op("dve", "tensor_tensor", out=yo[ig][:], in0=pO[ig][:], in1=rc[:], op=ALU.mult,
                         R=[f"pO{ig}", "rc"], W=[f"yo{ig}"])
                    k.dma(yT[h * 128:(h + 1) * 128, g8 * 512:(g8 + 1) * 512], yo[ig][:], R=[f"yo{ig}"], W=["D:yT"],
                          is_output=True)
        k.end_phase()
    return nc


NEG_INF = -1.0e30


def build_D(NT=9, ctx_tiles=1, final=False, stop_after=9, neg=64, cut=9, part="all"):
    R = NT * 128
    if part == "a":
        stop_after = 5
    nc = new_nc()
    x = din(nc, "x", [R, D])
    yT = din(nc, "yT", [D, R], BF16)
    wout = din(nc, "wout", [D, D])
    mv = din(nc, "mv", [10, D])
    wq = din(nc, "wq", [D, 2048])
    skT = din(nc, "skT", [128, 2, 128])
    if stop_after >= 6:
        UTb = din(nc, "UTb", [neg, 128, 32, 256], BF16)
        Vb = din(nc, "Vb", [neg * 256, D], BF16)
    idn = din(nc, "ident", [128, 128], BF16)
    xo = dout(nc, "xo", [R, D]) if part != "a" else None
    if part == "a":
        stop_after = 5
        x1 = dout(nc, "x1", [R, D])
        h2T = dout(nc, "h2T", [D, R], BF16)
        G = dout(nc, "G", [R, 16384], BF16)
    else:
        x1 = dscr(nc, "x1", [R, D])
        h2T = dscr(nc, "h2T", [D, R], BF16)
        G = dscr(nc, "G", [R, 16384], BF16)
    S = dscr(nc, "S", [R, 2048])
    x2 = dscr(nc, "x2", [R, D]) if final else xo
    typ = lambda t: 0 if t < ctx_tiles else 1
    with ExitStack() as st:
        k = KB(nc, st)

        def load_wblock(wb, wkey, stg, Wd, c0, nw, ns):
            for q in range(8):
                j = ns[0] % 3
                ns[0] += 1
                k.dma(stg[j][:, :, :nw], Wd[q * 512:(q + 1) * 512, c0:c0 + nw].rearrange("(a p) n -> p a n", p=128),
                      W=[f"stg{j}"])
                if q % 2 == 0:
                    k.op("dve", "tensor_copy", out=wb[:, q * 4:(q + 1) * 4, :nw], in_=stg[j][:, :, :nw],
                         R=[f"stg{j}"], W=[wkey])
                else:
                    k.op("act", "copy", out=wb[:, q * 4:(q + 1) * 4, :nw], in_=stg[j][:, :, :nw],
                         R=[f"stg{j}"], W=[wkey])

        k.begin_phase()
        yTs = k.sb("yTs", [128, 32, R], BF16)
        k.dma(yTs[:], yT.rearrange("(a p) t -> p a t", p=128), W=["yTs"])
        wbs = [k.sb(f"wb{i}", [128, 32, 512], BF16) for i in range(2)]
        stg = [k.sb(f"stg{i}", [128, 4, 512]) for i in range(3)]
        g1s = k.sb("g1s", [128, 2, 512])
        xs = [k.sb(f"xs{i}", [128, 512]) for i in range(3)]
        tmp = [k.sb(f"tmp{i}", [128, 512]) for i in range(3)]
        ps = [k.ps(f"ps{i}", [128, 512]) for i in range(3)]
        ns = [0]
        n = 0
        load_wblock(wbs[0], "wb0", stg, wout, 0, 512, ns)
        for nb in range(8):
            c0 = nb * 512
            wb, wkey = wbs[nb % 2], f"wb{nb % 2}"
            for ty in range(2):
                k.dma(g1s[:, ty, :], mv[ty:ty + 1, c0:c0 + 512].partition_broadcast(128), W=["g1s"])
            for t in range(NT):
                i = n % 3
                n += 1
                for kk in range(32):
                    k.op("pe", "matmul", out=ps[i][:], lhsT=yTs[:, kk, t * 128:(t + 1) * 128], rhs=wb[:, kk, :],
                         start=(kk == 0), stop=(kk == 31), R=["yTs", wkey], W=[f"ps{i}"], inc=(kk == 31))
                if t == 0 and nb + 1 < 8:
                    load_wblock(wbs[(nb + 1) % 2], f"wb{(nb + 1) % 2}", stg, wout, c0 + 512, 512, ns)
                k.dma(xs[i][:], x[t * 128:(t + 1) * 128, c0:c0 + 512], W=[f"xs{i}"])
                k.op("dve", "tensor_tensor", out=tmp[i][:], in0=ps[i][:], in1=g1s[:, typ(t), :], op=ALU.mult,
                     R=[f"ps{i}", "g1s"], W=[f"tmp{i}"])
                k.op("pool", "tensor_tensor", out=tmp[i][:], in0=tmp[i][:], in1=xs[i][:], op=ALU.add,
                     R=[f"tmp{i}", f"xs{i}"], W=[f"tmp{i}"])
                k.dma(x1[t * 128:(t + 1) * 128, c0:c0 + 512], tmp[i][:], R=[f"tmp{i}"], W=["D:x1"])
        k.end_phase()
        if stop_after < 2:
            return nc
        k.begin_phase()
        ident = k.sb("ident_sb", [128, 128], BF16)
        k.dma(ident[:], idn, W=["ident"])
        mods = emit_mods(k, mv[2:7, :])
        ntm = NormT(k, ident[:])
        hts = [k.sb(f"hT{i}", [128, 32, 128], BF16) for i in range(2)]
        for t in range(NT):
            sc, sh = mods[typ(t)]
            j = t % 2
            ntm.tile(x1[t * 128:(t + 1) * 128, :], ["D:x1"], sc[:], sh[:], "mods", hts[j], f"hT{j}")
            k.dma(h2T[:, t * 128:(t + 1) * 128].rearrange("(a p) t -> p a t", p=128), hts[j][:],
                  R=[f"hT{j}"], W=["D:h2T"])
        k.end_phase()
        if stop_after < 3:
            return nc
        k.begin_phase()
        h2s = k.sb("h2s", [128, 32, R], BF16)
        k.dma(h2s[:], h2T.rearrange("(a p) t -> p a t", p=128), R=["D:h2T"], W=["h2s"])
        sk = k.sb("sk", [128, 2, 128])
        k.dma(sk[:], skT, W=["sk"])
        wbs = [k.sb(f"wb{i}", [128, 32, 512], BF16) for i in range(2)]
        stg = [k.sb(f"stg{i}", [128, 4, 512]) for i in range(3)]
        qn = k.sb("qn", [128, 4, R])
        so = [k.sb(f"so{i}", [128, 512]) for i in range(2)]
        ps = [k.ps(f"ps{i}", [128, 512]) for i in range(3)]
        p2 = [k.ps(f"p2{i}", [128, 512]) for i in range(2)]
        n = n2 = 0
        load_wblock(wbs[0], "wb0", stg, wq, 0, 512, ns)
        for nb in range(4):
            wb, wkey = wbs[nb % 2], f"wb{nb % 2}"
            for j in range(4):
                for t0 in range(0, R, 512):
                    tw = min(512, R - t0)
                    i = n % 3
                    n += 1
                    for kk in range(32):
                        k.op("pe", "matmul", out=ps[i][:, :tw], lhsT=wb[:, kk, j * 128:(j + 1) * 128],
                             rhs=h2s[:, kk, t0:t0 + tw], start=(kk == 0), stop=(kk == 31), R=[wkey, "h2s"],
                             W=[f"ps{i}"], inc=(kk == 31))
                    if j == 0 and t0 == 0 and nb + 1 < 4:
                        load_wblock(wbs[(nb + 1) % 2], f"wb{(nb + 1) % 2}", stg, wq, (nb + 1) * 512, 512, ns)
                    if n % 2 == 0:
                        k.op("act", "copy", out=qn[:, j, t0:t0 + tw], in_=ps[i][:, :tw], R=[f"ps{i}"], W=["qn"])
                    else:
                        k.op("dve", "tensor_copy", out=qn[:, j, t0:t0 + tw], in_=ps[i][:, :tw], R=[f"ps{i}"], W=["qn"])
            for t in range(NT):
                i = n2 % 2
                n2 += 1
                for j in range(4):
                    k.op("pe", "matmul", out=p2[i][:, j * 128:(j + 1) * 128], lhsT=qn[:, j, t * 128:(t + 1) * 128],
                         rhs=sk[:, j % 2, :], start=True, stop=True, R=["qn", "sk"], W=[f"p2{i}"], inc=(j == 3))
                k.op("dve", "tensor_copy", out=so[i][:], in_=p2[i][:], R=[f"p2{i}"], W=[f"so{i}"])
                k.dma(S[t * 128:(t + 1) * 128, nb * 512:(nb + 1) * 512], so[i][:], R=[f"so{i}"], W=["D:S"])
        k.end_phase()
        if stop_after < 4:
            return nc
        k.begin_phase()
        St = [k.sb(f"St{i}", [128, 16, 128]) for i in range(2)]
        V16 = k.sb("V16", [128, 16, 16])
        scr = k.sb("scr", [128, 256])
        cand = k.sb("cand", [128, 8, 256])
        T16 = k.sb("T16", [128, 8, 16])
        E16 = k.sb("E16", [128, 8, 16])
        zz = k.sb("zz", [128, 16])
        ident = k.sb("ident_sb", [128, 128], BF16)
        k.dma(ident[:], idn, W=["ident"])
        e1 = k.sb("e1", [128, 8, 128])
        e2 = k.sb("e2", [128, 8, 128])
        th = k.sb("th", [128, 8])
        Eg = [k.sb(f"Eg{i}", [128, 16, 128]) for i in range(2)]
        Fb = [k.sb(f"Fb{i}", [128, 2048], BF16) for i in range(2)]
        Gb = [k.sb(f"Gb{i}", [128, 4096], BF16) for i in range(2)]
        pG = [k.ps(f"pG{i}", [128, 2048]) for i in range(2)]
        nd = ngb = 0
        for t in range(NT):
            s_ = St[t % 2]
            sk_ = f"St{t % 2}"
            k.dma(s_[:], S[t * 128:(t + 1) * 128, :].rearrange("p (b c) -> p b c", b=16), R=["D:S"], W=[sk_])
            for b in range(16):
                k.op("dve", "max", out=V16[:, b, 0:8], in_=s_[:, b, :], R=[sk_], W=["V16"])
                k.op("dve", "match_replace", out=scr[:, 0:128], in_to_replace=V16[:, b, 0:8], in_values=s_[:, b, :],
                     imm_value=NEG_INF, R=[sk_, "V16"], W=["scr"])
                k.op("dve", "max", out=V16[:, b, 8:16], in_=scr[:, 0:128], R=["scr"], W=["V16"])
            Vv = V16[:].rearrange("p (h two) r -> p h two r", two=2)
            k.op("dve", "tensor_tensor", out=cand[:].rearrange("p h (a b) -> p h a b", a=16),
                 in0=Vv[:, :, 0, :].unsqueeze(3).to_broadcast([128, 8, 16, 16]),
                 in1=Vv[:, :, 1, :].unsqueeze(2).to_broadcast([128, 8, 16, 16]), op=ALU.add, R=["V16"], W=["cand"])
            for h in range(8):
                k.op("dve", "max", out=T16[:, h, 0:8], in_=cand[:, h, :], R=["cand"], W=["T16"])
                k.op("dve", "match_replace", out=scr[:], in_to_replace=T16[:, h, 0:8], in_values=cand[:, h, :],
                     imm_value=NEG_INF, R=["cand", "T16"], W=["scr"])
                k.op("dve", "max", out=T16[:, h, 8:16], in_=scr[:], R=["scr"], W=["T16"])
            k.op("act", "activation", out=E16[:], in_=T16[:], func=AF.Exp, R=["T16"], W=["E16"])
            k.op("dve", "tensor_reduce", out=zz[:, 0:8], in_=E16[:], op=ALU.add, axis=AX.X, R=["E16"], W=["zz"])
            k.op("act", "activation", out=zz[:, 8:16], in_=zz[:, 0:8], func=AF.Ln, R=["zz"], W=["zz"])
            k.op("dve", "tensor_scalar", out=zz[:, 8:16], in0=zz[:, 8:16], scalar1=-1.0, scalar2=None, op0=ALU.mult,
                 R=["zz"], W=["zz"])
            Sv = s_[:].rearrange("p (h two) c -> p h two c", two=2)
            for h in range(8):
                k.op("act", "activation", out=e1[:, h, :], in_=Sv[:, h, 0, :], func=AF.Exp, bias=zz[:, 8 + h:9 + h],
                     R=[sk_, "zz"], W=["e1"])
            k.op("act", "activation", out=e2[:], in_=Sv[:, :, 1, :], func=AF.Exp, R=[sk_], W=["e2"])
            k.op("dve", "scalar_tensor_tensor", out=th[:], in0=T16[:, :, 15], scalar=-1.0e-4, in1=zz[:, 8:16],
                 op0=ALU.add, op1=ALU.add, R=["T16", "zz"], W=["th"])
            k.op("act", "activation", out=th[:], in_=th[:], func=AF.Exp, R=["th"], W=["th"])
            for oi in range(8):
                ig = ngb % 2
                ngb += 1
                for h in range(8):
                    i = nd % 2
                    nd += 1
                    if nd % 16 in (1, 3, 5, 7, 9, 11, 13):
                        for ii in range(16):
                            k.op("act", "activation", out=Eg[i][:, ii, :], in_=e2[:, h, :], func=AF.Copy,
                                 scale=e1[:, h, oi * 16 + ii:oi * 16 + ii + 1], R=["e1", "e2"], W=[f"Eg{i}"],
                                 inc=(ii == 15))
                    else:
                        k.op("pool", "tensor_tensor", out=Eg[i][:],
                             in0=e1[:, h, oi * 16:(oi + 1) * 16].unsqueeze(2).to_broadcast([128, 16, 128]),
                             in1=e2[:, h, :].unsqueeze(1).to_broadcast([128, 16, 128]), op=ALU.mult,
                             R=["e1", "e2"], W=[f"Eg{i}"])
                    egf = Eg[i][:].rearrange("p a b -> p (a b)")
                    k.op("dve", "scalar_tensor_tensor", out=Fb[i][:], in0=egf, scalar=th[:, h:h + 1], in1=egf,
                         op0=ALU.is_ge, op1=ALU.mult, R=[f"Eg{i}", "th"], W=[f"Fb{i}"])
                    for blk in range(4):
                        k.op("pe", "matmul", out=pG[ig][:, blk * 512:(blk + 1) * 512], lhsT=ident[:],
                             rhs=Fb[i][:, blk * 512:(blk + 1) * 512], start=(h == 0), stop=(h == 7),
                             R=[f"Fb{i}", "ident"], W=[f"pG{ig}"], inc=(blk == 3))
                gbi = (t * 4 + oi // 2) % 2
                k.op("act", "copy", out=Gb[gbi][:, (oi % 2) * 2048:(oi % 2 + 1) * 2048], in_=pG[ig][:],
                     R=[f"pG{ig}"], W=[f"Gb{gbi}"])
                if oi % 2 == 1:
                    qi = oi // 2
                    k.dma(G[t * 128:(t + 1) * 128, qi * 4096:(qi + 1) * 4096], Gb[gbi][:], R=[f"Gb{gbi}"], W=["D:G"])
        k.end_phase()
        if stop_after < 6:
            return nc
        k.begin_phase()
        ident = k.sb("ident_sb", [128, 128], BF16)
        k.dma(ident[:], idn, W=["ident"])
        h2b = k.sb("h2b", [128, 32, 384], BF16)
        acc = k.sb("acc", [128, 3, D])
        UTs = [k.sb(f"UTs{i}", [128, 32, 256], BF16) for i in range(2)]
        Vs = [k.sb(f"Vs{i}", [128, 2, D], BF16) for i in range(2)]
        Gs = [k.sb(f"Gs{i}", [128, 3, 256], BF16) for i in range(2)]
        Wg = [k.sb(f"Wg{i}", [128, 256]) for i in range(2)]
        Wb = [k.sb(f"Wb{i}", [128, 256], BF16) for i in range(2)]
        WT = [k.sb(f"WT{i}", [128, 2, 128], BF16) for i in range(2)]
        xs = [k.sb(f"xs{i}", [128, 512]) for i in range(2)]
        g2s = [k.sb(f"g2s{i}", [128, 512]) for i in range(2)]
        tm = [k.sb(f"tm{i}", [128, 512]) for i in range(2)]
        pA = [k.ps(f"pA{i}", [128, 512]) for i in range(2)]
        pT = k.ps("pT", [128, 1024], BF16)
        po = [k.ps(f"po{i}", [128, 1024]) for i in range(2)]
        nA = npo = ne = 0
        for tb0 in range(0, NT, 3):
            tiles = list(range(tb0, min(tb0 + 3, NT)))
            nt_ = len(tiles)
            r0, r1 = tb0 * 128, (tb0 + nt_) * 128
            k.dma(h2b[:, :, :nt_ * 128], h2T[:, r0:r1].rearrange("(a p) t -> p a t", p=128), R=["D:h2T"], W=["h2b"])
            for eg in range(neg):
                j = eg % 2
                k.dma(UTs[j][:], UTb[eg], W=[f"UTs{j}"])
                k.dma(Vs[j][:], Vb[eg * 256:(eg + 1) * 256, :].rearrange("(c p) d -> p c d", p=128), W=[f"Vs{j}"])
                k.dma(Gs[j][:, :nt_, :], G[r0:r1, eg * 256:(eg + 1) * 256].rearrange("(n p) c -> p n c", p=128),
                      R=["D:G"], W=[f"Gs{j}"])
                for ti in range(nt_):
                    i = nA % 2
                    nA += 1
                    for kk in range(32):
                        k.op("pe", "matmul", out=pA[i][:, 0:256], lhsT=h2b[:, kk, ti * 128:(ti + 1) * 128],
                             rhs=UTs[j][:, kk, :], start=(kk == 0), stop=(kk == 31), R=["h2b", f"UTs{j}"],
                             W=[f"pA{i}"], inc=(kk == 31))
                    k.op("act", "activation", out=Wg[i][:], in_=pA[i][:, 0:256], func=AF.Gelu_apprx_tanh,
                         R=[f"pA{i}"], W=[f"Wg{i}"])
                    if cut < 2:
                        continue
                    k.op("pool", "tensor_tensor", out=Wb[i][:], in0=Wg[i][:], in1=Gs[j][:, ti, :], op=ALU.mult,
                         R=[f"Wg{i}", f"Gs{j}"], W=[f"Wb{i}"])
                    if cut < 3:
                        continue
                    for c in range(2):
                        k.op("pe", "transpose", out=pT[:, c * 128:(c + 1) * 128], in_=Wb[i][:, c * 128:(c + 1) * 128],
                             identity=ident[:], R=[f"Wb{i}", "ident"], W=["pT"], inc=(c == 1))
                    k.op("act", "copy", out=WT[i][:], in_=pT[:, 0:256].rearrange("p (c t) -> p c t", c=2),
                         R=["pT"], W=[f"WT{i}"])
                    if cut < 4:
                        continue
                    for ch in range(4):
                        ip = npo % 2
                        npo += 1
                        for db in range(2):
                            for c in range(2):
                                d0 = ch * 1024 + db * 512
                                k.op("pe", "matmul", out=po[ip][:, db * 512:(db + 1) * 512], lhsT=WT[i][:, c, :],
                                     rhs=Vs[j][:, c, d0:d0 + 512], start=(c == 0), stop=(c == 1),
                                     R=[f"WT{i}", f"Vs{j}"], W=[f"po{ip}"], inc=(db == 1 and c == 1))
                        a_ = acc[:, ti, ch * 1024:(ch + 1) * 1024]
                        if cut < 5:
                            continue
                        if eg == 0:
                            k.op("dve", "tensor_copy", out=a_, in_=po[ip][:], R=[f"po{ip}"], W=["acc"])
                        else:
                            k.op("dve", "tensor_tensor", out=a_, in0=po[ip][:], in1=a_, op=ALU.add,
                                 R=[f"po{ip}", "acc"], W=["acc"])
            for ti, t in enumerate(tiles):
                for db in range(8):
                    i = ne % 2
                    ne += 1
                    c0 = db * 512
                    k.dma(xs[i][:], x1[t * 128:(t + 1) * 128, c0:c0 + 512], R=["D:x1"], W=[f"xs{i}"])
                    k.dma(g2s[i][:], mv[7 + typ(t):8 + typ(t), c0:c0 + 512].partition_broadcast(128), W=[f"g2s{i}"])
                    k.op("dve", "tensor_tensor", out=tm[i][:], in0=acc[:, ti, c0:c0 + 512], in1=g2s[i][:], op=ALU.mult,
                         R=["acc", f"g2s{i}"], W=[f"tm{i}"])
                    k.op("pool", "tensor_tensor", out=tm[i][:], in0=tm[i][:], in1=xs[i][:], op=ALU.add,
                         R=[f"tm{i}", f"xs{i}"], W=[f"tm{i}"])
                    k.dma(x2[t * 128:(t + 1) * 128, c0:c0 + 512], tm[i][:], R=[f"tm{i}"], W=["D:x2"],
                          is_output=(not final))
        k.end_phase()
        if final:
            k.begin_phase()
            fg = k.sb("fg", [128, D])
            k.dma(fg[:], mv[9:10, :].partition_broadcast(128), W=["fg"])
            xt = [k.sb(f"xt{i}", [128, D]) for i in range(2)]
            jk = k.sb("jk", [128, D])
            ss = [k.sb(f"ss{i}", [128, 2]) for i in range(2)]
            for t in range(NT):
                i = t % 2
                k.dma(xt[i][:], x2[t * 128:(t + 1) * 128, :], R=["D:x2"], W=[f"xt{i}"])
                k.op("act", "activation", out=jk[:], in_=xt[i][:], func=AF.Square, accum_out=ss[i][:, 0:1],
                     R=[f"xt{i}"], W=["jk", f"ss{i}"])
                k.op("dve", "tensor_scalar", out=ss[i][:, 1:2], in0=ss[i][:, 0:1], scalar1=1.0 / D, scalar2=EPS,
                     op0=ALU.mult, op1=ALU.add, R=[f"ss{i}"], W=[f"ss{i}"])
                k.op("act", "activation", out=ss[i][:, 1:2], in_=ss[i][:, 1:2], func=AF.Sqrt, R=[f"ss{i}"], W=[f"ss{i}"])
                k.op("dve", "reciprocal", out=ss[i][:, 1:2], in_=ss[i][:, 1:2], R=[f"ss{i}"], W=[f"ss{i}"])
                k.op("dve", "scalar_tensor_tensor", out=xt[i][:], in0=xt[i][:], scalar=ss[i][:, 1:2], in1=fg[:],
                     op0=ALU.mult, op1=ALU.mult, R=[f"xt{i}", f"ss{i}", "fg"], W=[f"xt{i}"])
                k.dma(xo[t * 128:(t + 1) * 128, :], xt[i][:], R=[f"xt{i}"], W=["D:xo"], is_output=True)
            k.end_phase()
    return nc


def build_Db(NT=9, ctx_tiles=1, final=False, neg=16, first=True, last=True):
    R = NT * 128
    nc = new_nc()
    h2T = din(nc, "h2T", [D, R], BF16)
    G = din(nc, "G", [R, neg * 256], BF16)
    UTb = din(nc, "UTb", [neg, 128, 32, 256], BF16)
    Vb = din(nc, "Vb", [neg * 256, D], BF16)
    idn = din(nc, "ident", [128, 128], BF16)
    acc_in = None if first else din(nc, "acc_in", [R, D])
    if last:
        x1 = din(nc, "x1", [R, D])
        mv = din(nc, "mv", [10, D])
        xo = dout(nc, "xo", [R, D])
        x2 = dscr(nc, "x2", [R, D]) if final else xo
    else:
        acc_out = dout(nc, "acc_out", [R, D])
    typ = lambda t: 0 if t < ctx_tiles else 1
    with ExitStack() as st:
        k = KB(nc, st)
        k.begin_phase()
        ident = k.sb("ident_sb", [128, 128], BF16)
        k.dma(ident[:], idn, W=["ident"])
        h2b = k.sb("h2b", [128, 32, 384], BF16)
        acc = k.sb("acc", [128, 3, D])
        UTs = [k.sb(f"UTs{i}", [128, 32, 256], BF16) for i in range(2)]
        Vs = [k.sb(f"Vs{i}", [128, 2, D], BF16) for i in range(2)]
        Gs = [k.sb(f"Gs{i}", [128, 3, 256], BF16) for i in range(2)]
        Wg = [k.sb(f"Wg{i}", [128, 256]) for i in range(2)]
        Wb = [k.sb(f"Wb{i}", [128, 256], BF16) for i in range(2)]
        WT = [k.sb(f"WT{i}", [128, 2, 128], BF16) for i in range(2)]
        xs = [k.sb(f"xs{i}", [128, 512]) for i in range(2)]
        g2s = [k.sb(f"g2s{i}", [128, 512]) for i in range(2)]
        tm = [k.sb(f"tm{i}", [128, 512]) for i in range(2)]
        pA = [k.ps(f"pA{i}", [128, 512]) for i in range(2)]
        pT = k.ps("pT", [128, 1024], BF16)
        po = [k.ps(f"po{i}", [128, 1024]) for i in range(2)]
        nA = npo = ne = 0
        for tb0 in range(0, NT, 3):
            tiles = list(range(tb0, min(tb0 + 3, NT)))
            nt_ = len(tiles)
            r0, r1 = tb0 * 128, (tb0 + nt_) * 128
            k.dma(h2b[:, :, :nt_ * 128], h2T[:, r0:r1].rearrange("(a p) t -> p a t", p=128), W=["h2b"])
            if not first:
                k.dma(acc[:, :nt_, :], acc_in[r0:r1, :].rearrange("(n p) d -> p n d", p=128), W=["acc"])
            units = [(eg, ti) for eg in range(neg) for ti in range(nt_)]

            def S1(eg, ti):
                nonlocal nA
                j = eg % 2
                if ti == 0:
                    k.dma(UTs[j][:], UTb[eg], W=[f"UTs{j}"])
                    k.dma(Vs[j][:], Vb[eg * 256:(eg + 1) * 256, :].rearrange("(c p) d -> p c d", p=128), W=[f"Vs{j}"])
                    k.dma(Gs[j][:, :nt_, :], G[r0:r1, eg * 256:(eg + 1) * 256].rearrange("(n p) c -> p n c", p=128),
                          W=[f"Gs{j}"])
                i = nA % 2
                nA += 1
                for kk in range(32):
                    k.op("pe", "matmul", out=pA[i][:, 0:256], lhsT=h2b[:, kk, ti * 128:(ti + 1) * 128],
                         rhs=UTs[j][:, kk, :], start=(kk == 0), stop=(kk == 31), R=["h2b", f"UTs{j}"],
                         W=[f"pA{i}"], inc=(kk == 31))
                k.op("act", "activation", out=Wg[i][:], in_=pA[i][:, 0:256], func=AF.Gelu_apprx_tanh,
                     R=[f"pA{i}"], W=[f"Wg{i}"])
                k.op("pool", "tensor_tensor", out=Wb[i][:], in0=Wg[i][:], in1=Gs[j][:, ti, :], op=ALU.mult,
                     R=[f"Wg{i}", f"Gs{j}"], W=[f"Wb{i}"])
                return i

            def S2(eg, ti, i):
                nonlocal npo
                j = eg % 2
                for c in range(2):
                    k.op("pe", "transpose", out=pT[:, c * 128:(c + 1) * 128], in_=Wb[i][:, c * 128:(c + 1) * 128],
                         identity=ident[:], R=[f"Wb{i}", "ident"], W=["pT"], inc=(c == 1))
                k.op("act", "copy", out=WT[i][:], in_=pT[:, 0:256].rearrange("p (c t) -> p c t", c=2),
                     R=["pT"], W=[f"WT{i}"])
                for ch in range(4):
                    ip = npo % 2
                    npo += 1
                    for db in range(2):
                        for c in range(2):
                            d0 = ch * 1024 + db * 512
                            k.op("pe", "matmul", out=po[ip][:, db * 512:(db + 1) * 512], lhsT=WT[i][:, c, :],
                                 rhs=Vs[j][:, c, d0:d0 + 512], start=(c == 0), stop=(c == 1),
                                 R=[f"WT{i}", f"Vs{j}"], W=[f"po{ip}"], inc=(db == 1 and c == 1))
                    a_ = acc[:, ti, ch * 1024:(ch + 1) * 1024]
                    if eg == 0 and first:
                        k.op("dve", "tensor_copy", out=a_, in_=po[ip][:], R=[f"po{ip}"], W=["acc"])
                    else:
                        k.op("dve", "tensor_tensor", out=a_, in0=po[ip][:], in1=a_, op=ALU.add,
                             R=[f"po{ip}", "acc"], W=["acc"])

            pend = S1(*units[0])
            for ui, (eg, ti) in enumerate(units):
                nxt = S1(*units[ui + 1]) if ui + 1 < len(units) else None
                S2(eg, ti, pend)
                pend = nxt
            if not last:
                k.dma(acc_out[r0:r1, :].rearrange("(n p) d -> p n d", p=128), acc[:, :nt_, :], R=["acc"],
                      W=["D:acc_out"], is_output=True)
                continue
            for ti, t in enumerate(tiles):
                for db in range(8):
                    i = ne % 2
                    ne += 1
                    c0 = db * 512
                    k.dma(xs[i][:], x1[t * 128:(t + 1) * 128, c0:c0 + 512], W=[f"xs{i}"])
                    k.dma(g2s[i][:], mv[7 + typ(t):8 + typ(t), c0:c0 + 512].partition_broadcast(128), W=[f"g2s{i}"])
                    k.op("dve", "tensor_tensor", out=tm[i][:], in0=acc[:, ti, c0:c0 + 512], in1=g2s[i][:], op=ALU.mult,
                         R=["acc", f"g2s{i}"], W=[f"tm{i}"])
                    k.op("pool", "tensor_tensor", out=tm[i][:], in0=tm[i][:], in1=xs[i][:], op=ALU.add,
                         R=[f"tm{i}", f"xs{i}"], W=[f"tm{i}"])
                    k.dma(x2[t * 128:(t + 1) * 128, c0:c0 + 512], tm[i][:], R=[f"tm{i}"], W=["D:x2"],
                          is_output=(not final))
        k.end_phase()
        if last and final:
            k.begin_phase()
            fg = k.sb("fg", [128, D])
            k.dma(fg[:], mv[9:10, :].partition_broadcast(128), W=["fg"])
            xt = [k.sb(f"xt{i}", [128, D]) for i in range(2)]
            jk = k.sb("jk", [128, D])
            ss = [k.sb(f"ss{i}", [128, 2]) for i in range(2)]
            for t in range(NT):
                i = t % 2
                k.dma(xt[i][:], x2[t * 128:(t + 1) * 128, :], R=["D:x2"], W=[f"xt{i}"])
                k.op("act", "activation", out=jk[:], in_=xt[i][:], func=AF.Square, accum_out=ss[i][:, 0:1],
                     R=[f"xt{i}"], W=["jk", f"ss{i}"])
                k.op("dve", "tensor_scalar", out=ss[i][:, 1:2], in0=ss[i][:, 0:1], scalar1=1.0 / D, scalar2=EPS,
                     op0=ALU.mult, op1=ALU.add, R=[f"ss{i}"], W=[f"ss{i}"])
                k.op("act", "activation", out=ss[i][:, 1:2], in_=ss[i][:, 1:2], func=AF.Sqrt, R=[f"ss{i}"], W=[f"ss{i}"])
                k.op("dve", "reciprocal", out=ss[i][:, 1:2], in_=ss[i][:, 1:2], R=[f"ss{i}"], W=[f"ss{i}"])
                k.op("dve", "scalar_tensor_tensor", out=xt[i][:], in0=xt[i][:], scalar=ss[i][:, 1:2], in1=fg[:],
                     op0=ALU.mult, op1=ALU.mult, R=[f"xt{i}", f"ss{i}", "fg"], W=[f"xt{i}"])
                k.dma(xo[t * 128:(t + 1) * 128, :], xt[i][:], R=[f"xt{i}"], W=["D:xo"], is_output=True)
            k.end_phase()
    return nc


def build_E(NEG=8):
    nc = new_nc()
    U = din(nc, "U", [NEG * 256, D])
    V = din(nc, "V", [NEG * 256, D])
    idf = din(nc, "identf", [128, 128])
    UTb = dout(nc, "UTb", [NEG, 128, 32, 256], BF16)
    Vb = dout(nc, "Vb", [NEG * 256, D], BF16)
    with ExitStack() as st:
        k = KB(nc, st)
        k.begin_phase()
        ident = k.sb("identf_sb", [128, 128])
        k.dma(ident[:], idf, W=["ident"])
        ut = [k.sb(f"ut{i}", [128, D]) for i in range(3)]
        vb = [k.sb(f"vb{i}", [128, D], BF16) for i in range(2)]
        uo = [k.sb(f"uo{i}", [128, 32, 256], BF16) for i in range(2)]
        pt = [k.ps(f"pt{i}", [128, 512]) for i in range(4)]
        nu = nv = npt = 0
        for g in range(NEG):
            io = g % 2
            for c in range(2):
                iu = nu % 3
                nu += 1
                r0 = g * 256 + c * 128
                k.dma(ut[iu][:], U[r0:r0 + 128, :], W=[f"ut{iu}"])
                for k4 in range(8):
                    ip = npt % 4
                    npt += 1
                    for q in range(4):
                        kk = k4 * 4 + q
                        k.op("pe", "transpose", out=pt[ip][:, q * 128:(q + 1) * 128], in_=ut[iu][:, kk * 128:(kk + 1) * 128],
                             identity=ident[:], R=[f"ut{iu}", "ident"], W=[f"pt{ip}"], inc=(q == 3))
                    src = pt[ip][:].rearrange("p (a b) -> p a b", a=4)
                    dst = uo[io][:, k4 * 4:(k4 + 1) * 4, c * 128:(c + 1) * 128]
                    if k4 % 2 == 0:
                        k.op("act", "copy", out=dst, in_=src, R=[f"pt{ip}"], W=[f"uo{io}"])
                    else:
                        k.op("dve", "tensor_copy", out=dst, in_=src, R=[f"pt{ip}"], W=[f"uo{io}"])
                iu2 = nu % 3
                nu += 1
                iv = nv % 2
                nv += 1
                k.dma(ut[iu2][:], V[r0:r0 + 128, :], W=[f"ut{iu2}"])
                k.op("pool", "tensor_copy", out=vb[iv][:], in_=ut[iu2][:], R=[f"ut{iu2}"], W=[f"vb{iv}"])
                k.dma(Vb[r0:r0 + 128, :], vb[iv][:], R=[f"vb{iv}"], W=["D:Vb"], is_output=True)
            k.dma(UTb[g], uo[io][:], R=[f"uo{io}"], W=["D:UTb"], is_output=True)
        k.end_phase()
    return nc


def build_mod(NL=2, NC=3072):
    nc = bass.Bass("TRN2", target_bir_lowering=False)
    cT = nc.dram_tensor("cT", [128, 32, 3], F32, kind="ExternalInput").ap()
    w = nc.dram_tensor("w", [NL, 4096, NC], F32, kind="ExternalInput").ap()
    b = nc.dram_tensor("b", [NL, 1, NC], F32, kind="ExternalInput").ap()
    m = nc.dram_tensor("m", [NL, 3, NC], F32, kind="ExternalOutput").ap()
    with ExitStack() as st:
        k = KB(nc, st)
        c_sb = k.sb("c_sb", [128, 32, 3])
        s_sb = k.sb("s_sb", [128, 32, 3])
        wb = [k.sb(f"wb{i}", [128, 32, 512]) for i in range(2)]
        bias = k.sb("bias", [3, NL, NC])
        o_sb = [k.sb(f"o{i}", [3, 512]) for i in range(2)]
        ps = [k.ps(f"ps{i}", [128, 512]) for i in range(2)]
        k.dma(c_sb[:], cT, W=["c"])
        for l in range(NL):
            k.dma(bias[:, l, :], b[l].partition_broadcast(3), W=["bias"])
        k.op("act", "activation", out=s_sb[:], in_=c_sb[:], func=AF.Silu, R=["c"], W=["s"])
        it = 0
        for l in range(NL):
            for cb in range(NC // 512):
                j = it % 2
                k.dma(wb[j][:], w[l, :, cb * 512:(cb + 1) * 512].rearrange("(k p) n -> p k n", p=128),
                      W=[f"wb{j}"])
                for kk in range(32):
                    k.op("pe", "matmul", out=ps[j][0:3, :], lhsT=s_sb[:, kk, :], rhs=wb[j][:, kk, :],
                         start=(kk == 0), stop=(kk == 31),
                         R=["s", f"wb{j}"], W=[f"ps{j}"], inc=(kk == 31))
                k.op("dve", "tensor_tensor", out=o_sb[j][:], in0=ps[j][0:3, :],
                     in1=bias[:, l, cb * 512:(cb + 1) * 512], op=ALU.add,
                     R=[f"ps{j}", "bias"], W=[f"o{j}"])
                k.dma(m[l, :, cb * 512:(cb + 1) * 512], o_sb[j][:], R=[f"o{j}"], W=["m"], is_output=True)
                it += 1
        k.finish()
        print("instrs", k.ninstr)
    return nc


_PROGS = {}


def _prog(key, fn):
    if key not in _PROGS:
        _PROGS[key] = fn()
    return _PROGS[key]


def _run(nc, in_maps):
    return run_bass_kernel_spmd(nc, in_maps, core_ids=list(range(8))).results


def _c(a):
    return np.ascontiguousarray(a)


def kernel(x, c, ctx, c_ctx, mod_w, mod_b, norm_g, ev_w_in, ev_gate_b, ev_qk_g, ev_hnorm_g, ev_w_out,
           od_w_in, od_rpb, od_w_out, peer_wq, peer_subkeys, peer_u, peer_v, final_g):
    f32 = np.float32
    x, c, ctx, c_ctx = (np.asarray(a, f32) for a in (x, c, ctx, c_ctx))
    mod_w, mod_b, norm_g = (np.asarray(a, f32) for a in (mod_w, mod_b, norm_g))
    ev_w_in, ev_gate_b, ev_qk_g, ev_hnorm_g, ev_w_out = (np.asarray(a, f32) for a in
                                                         (ev_w_in, ev_gate_b, ev_qk_g, ev_hnorm_g, ev_w_out))
    od_w_in, od_rpb, od_w_out = (np.asarray(a, f32) for a in (od_w_in, od_rpb, od_w_out))
    peer_wq, peer_subkeys, peer_u, peer_v, final_g = (np.asarray(a, f32) for a in
                                                      (peer_wq, peer_subkeys, peer_u, peer_v, final_g))
    ident = np.eye(128, dtype=f32).astype(BF)
    identf = np.eye(128, dtype=f32)
    cst = np.zeros((128, 5, 128), f32)
    s_, t_ = np.meshgrid(np.arange(128), np.arange(128), indexing="ij")
    cst[:, 0, :] = (s_ <= t_)
    cst[:, 1, :] = (s_ >= t_)
    cst[:, 2, :] = 1.0
    tt = np.arange(4096)
    inv = (10000.0 ** (-np.arange(32, dtype=f32) / 32)).astype(f32)
    ang = np.concatenate([(tt // 64).astype(f32)[:, None] * inv, (tt % 64).astype(f32)[:, None] * inv], -1)
    rope = np.concatenate([np.cos(ang), np.sin(ang)], -1).astype(f32)

    cvec = np.stack([c[0], c[1], c_ctx])
    cT = _c(cvec.reshape(3, 32, 128).transpose(2, 1, 0))
    ncA = _prog("A", lambda: build_mod(2, 3072))
    rA = _run(ncA, [{"cT": cT, "w": _c(mod_w[:, :, i * 3072:(i + 1) * 3072]),
                     "b": _c(mod_b[:, None, i * 3072:(i + 1) * 3072])} for i in range(8)])
    mod = np.concatenate([r["m"] for r in rA], axis=-1).reshape(2, 3, 6, 4096)

    ncE = _prog("E", lambda: build_E(8))
    UTb, Vbf = [], []
    for l in range(2):
        rE = _run(ncE, [{"U": _c(peer_u[l, i * 2048:(i + 1) * 2048]), "V": _c(peer_v[l, i * 2048:(i + 1) * 2048]),
                         "identf": identf} for i in range(8)])
        UTb.append(np.concatenate([r["UTb"] for r in rE], axis=0))
        Vbf.append(np.concatenate([r["Vb"] for r in rE], axis=0))

    xl = x
    xc = ctx
    zpad = np.zeros((64, 4096), f32)
    out = np.empty((2, 4096, 4096), f32)
    for layer in range(2):
        last = layer == 1
        sh1, sc1, g1, sh2, sc2, g2 = (mod[layer, :, i] for i in range(6))
        ncB = _prog("B", lambda: build_B(9))
        rows = {}
        maps = []
        for b in range(2):
            for q in range(4):
                r_ = np.concatenate([xc[b, 64 * q:64 * q + 64], zpad, xl[b, 1024 * q:1024 * (q + 1)]], axis=0)
                rows[(b, q)] = r_
                mv5 = np.stack([norm_g[layer, 0], sc1[2], sh1[2], sc1[b], sh1[b]])
                maps.append({"x": r_, "mv": mv5, "ident": ident})
        rB = _run(ncB, maps)
        hTb = []
        for b in range(2):
            parts = [rB[b * 4 + q]["hT"][:, 0:64] for q in range(4)] + [rB[b * 4 + q]["hT"][:, 128:] for q in range(4)]
            hTb.append(_c(np.concatenate(parts, axis=1)))
        if layer == 0:
            ncC = _prog("C0", lambda: build_C0(2, 32))
            w = ev_w_in[0]
            maps = []
            for b in range(2):
                for g in range(4):
                    gcols = [9216 + o + 2 * g + hh for o in (0, 8, 16, 24) for hh in (0, 1)]
                    Wc = np.concatenate([w[:, 512 * g:512 * g + 512], w[:, 2048 + 128 * g:2048 + 128 * g + 128],
                                         w[:, 2560 + 128 * g:2560 + 128 * g + 128],
                                         w[:, 3072 + 256 * g:3072 + 256 * g + 256],
                                         w[:, 4096 + 256 * g:4096 + 256 * g + 256],
                                         w[:, 5120 + 512 * g:5120 + 512 * g + 512],
                                         w[:, 7168 + 512 * g:7168 + 512 * g + 512], w[:, gcols]], axis=1)
                    gbv = ev_gate_b[0][[o + 2 * g + hh for o in (0, 8, 16, 24) for hh in (0, 1)]][None]
                    maps.append({"hT": hTb[b], "W": _c(Wc), "cst": cst, "ident": ident, "qkg": _c(ev_qk_g[0]),
                                 "gb": _c(gbv), "hng": _c(ev_hnorm_g[0].reshape(8, 256)[2 * g:2 * g + 2]),
                                 "rope": rope})
            rC = _run(ncC, maps)
            yTb = []
            for b in range(2):
                y = np.empty((4096, 4352), BF)
                for g in range(4):
                    p = rC[b * 4 + g]["yT"]
                    y[512 * g:512 * g + 512] = p[0:512]
                    y[2048 + 512 * g:2048 + 512 * g + 512] = p[512:1024]
                yTb.append(y)
        else:
            ncC = _prog("C1", lambda: build_C1(2, 32, 8))
            w = od_w_in[0]
            maps = []
            for b in range(2):
                for g in range(4):
                    Wc = np.concatenate([w[:, 1024 * g:1024 * (g + 1)], w[:, 4096 + 1024 * g:4096 + 1024 * (g + 1)],
                                         w[:, 8192 + 1024 * g:8192 + 1024 * (g + 1)]], axis=1)
                    Bt = np.stack([na_bias_table(od_rpb[0][8 * g + h], 64).reshape(128, -1) for h in range(8)])
                    maps.append({"hT": hTb[b], "W": _c(Wc), "ident": ident, "Bt": _c(Bt)})
            rC = _run(ncC, maps)
            yTb = []
            for b in range(2):
                y = np.empty((4096, 4352), BF)
                y[:, :256] = 0
                for g in range(4):
                    y[1024 * g:1024 * (g + 1), 256:] = rC[b * 4 + g]["yT"]
                yTb.append(y)
        NT = 8 if last else 9
        CTXT = 0 if last else 1
        w_out = (od_w_out if last else ev_w_out)[0]
        skT = _c(peer_subkeys[layer].transpose(2, 0, 1))
        ncDa = _prog(("Da", NT), lambda: build_D(NT, CTXT, part="a"))
        maps = []
        mvs = {}
        for b in range(2):
            for q in range(4):
                lat = yTb[b][:, 256 + 1024 * q:256 + 1024 * (q + 1)]
                if last:
                    xr = rows[(b, q)][128:]
                    yc = _c(lat)
                else:
                    xr = rows[(b, q)]
                    yc = _c(np.concatenate([yTb[b][:, 64 * q:64 * q + 64], np.zeros((4096, 64), BF), lat], axis=1))
                mv10 = np.stack([g1[2], g1[b], norm_g[layer, 1], sc2[2], sh2[2], sc2[b], sh2[b], g2[2], g2[b], final_g])
                mvs[(b, q)] = mv10
                maps.append({"x": _c(xr), "yT": yc, "wout": w_out, "mv": mv10, "wq": peer_wq[layer], "skT": skT,
                             "ident": ident})
        rDa = _run(ncDa, maps)
        acc = [None] * 8
        for ch in range(4):
            first, lastc = ch == 0, ch == 3
            ncDb = _prog(("Db", NT, first, lastc), lambda: build_Db(NT, CTXT, last, 16, first, lastc))
            maps = []
            for ci in range(8):
                m = {"h2T": rDa[ci]["h2T"], "G": _c(rDa[ci]["G"][:, ch * 4096:(ch + 1) * 4096]),
                     "UTb": _c(UTb[layer][ch * 16:(ch + 1) * 16]), "Vb": _c(Vbf[layer][ch * 4096:(ch + 1) * 4096]),
                     "ident": ident}
                if not first:
                    m["acc_in"] = acc[ci]
                if lastc:
                    m["x1"] = rDa[ci]["x1"]
                    m["mv"] = mvs[(ci // 4, ci % 4)]
                maps.append(m)
            rDb = _run(ncDb, maps)
            if not lastc:
                acc = [r["acc_out"] for r in rDb]
        if last:
            for b in range(2):
                for q in range(4):
                    out[b, 1024 * q:1024 * (q + 1)] = rDb[b * 4 + q]["xo"]
        else:
            xl = np.empty_like(x)
            xc = np.empty_like(ctx)
            for b in range(2):
                for q in range(4):
                    r_ = rDb[b * 4 + q]["xo"]
                    xc[b, 64 * q:64 * q + 64] = r_[0:64]
                    xl[b, 1024 * q:1024 * (q + 1)] = r_[128:]
    return out
```
